# Optimizing a Trainium2 kernel written in Bass

```python
import jax
import jax.numpy as jnp
from jax import lax
import numpy as np


D_MODEL = 1024
BATCH = 8
SEQ = 8192
DEPTH = 2

N_EVEN = (DEPTH + 1) // 2
N_ODD = DEPTH // 2

GLA_HEADS = 4
GLA_DK = D_MODEL // 2 // GLA_HEADS
GLA_DV = D_MODEL // GLA_HEADS
GLA_KEY_WIDTH = GLA_HEADS * GLA_DK
GLA_VALUE_WIDTH = GLA_HEADS * GLA_DV
GLA_GATE_RANK = 16
GLA_GATE_TAU = 16.0
GLA_CHUNK = 64
GLA_SPLITS = tuple(int(s) for s in np.cumsum([GLA_KEY_WIDTH, GLA_KEY_WIDTH, GLA_VALUE_WIDTH, GLA_VALUE_WIDTH, GLA_GATE_RANK]))
GLA_IN_WIDTH = 2 * GLA_KEY_WIDTH + 2 * GLA_VALUE_WIDTH + 2 * GLA_GATE_RANK

N_Q_HEADS = 16
N_KV_HEADS = 4
HEAD_DIM = 64
GROUP = N_Q_HEADS // N_KV_HEADS
WINDOW = 128
ATT_BLOCK = 128
SWA_QKV_WIDTH = (N_Q_HEADS + 2 * N_KV_HEADS) * HEAD_DIM

D_FF = 2816
N_EXPERTS = 8
TOP_K = 2

NORM_EPS = 1e-5

kernel_name = 'hybrid_gla_swa_moe_encoder'


def rms_norm(x, gain):
    xf = x.astype(jnp.float32)
    y = xf * lax.rsqrt(jnp.mean(xf * xf, axis=-1, keepdims=True) + NORM_EPS)
    return (y * gain.astype(jnp.float32)).astype(x.dtype)


def alibi_slopes(n_heads):
    return jnp.asarray(np.power(2.0, -8.0 * np.arange(1, n_heads + 1) / n_heads).astype(np.float32))


def gla_chunked(q, k, v, log_a, inclusive):
    B, H, S, dk = q.shape
    dv = v.shape[-1]
    C = GLA_CHUNK
    n = S // C
    q = q.reshape(B, H, n, C, dk)
    k = k.reshape(B, H, n, C, dk)
    v = v.reshape(B, H, n, C, dv)
    b = jnp.cumsum(log_a.reshape(B, H, n, C, dk), axis=3)
    b_ref = b[:, :, :, C // 2:C // 2 + 1]
    qe = q * jnp.exp(b - b_ref)
    ke = k * jnp.exp(b_ref - b)
    scores = jnp.einsum('bhnik,bhnjk->bhnij', qe, ke)
    mask = jnp.tril(jnp.ones((C, C), dtype=bool), 0 if inclusive else -1)
    scores = jnp.where(mask, scores, 0.0)
    o_intra = jnp.einsum('bhnij,bhnjv->bhniv', scores, v)
    b_last = b[:, :, :, -1:]
    chunk_update = jnp.einsum('bhnjk,bhnjv->bhnkv', k * jnp.exp(b_last - b), v)
    chunk_decay = jnp.exp(b_last[:, :, :, 0])

    def step(state, inp):
        decay, upd = inp
        return state * decay[..., None] + upd, state

    init = jnp.zeros((B, H, dk, dv), jnp.float32)
    _, prev = lax.scan(step, init, (jnp.moveaxis(chunk_decay, 2, 0), jnp.moveaxis(chunk_update, 2, 0)))
    prev = jnp.moveaxis(prev, 0, 2)
    o_inter = jnp.einsum('bhnik,bhnkv->bhniv', q * jnp.exp(b), prev)
    return (o_intra + o_inter).reshape(B, H, S, dv)


def gla_mixer(h, w_in, wg_f, bg_f, wg_b, bg_b, head_gain, w_out):
    B, S, _ = h.shape
    proj = (h @ w_in).astype(jnp.float32)
    q, k, v, r, lr_f, lr_b = jnp.split(proj, GLA_SPLITS, axis=-1)

    def heads(t, d):
        return t.reshape(B, S, GLA_HEADS, d).transpose(0, 2, 1, 3)

    q = heads(q, GLA_DK) * (GLA_DK ** -0.5)
    k = heads(k, GLA_DK)
    v = heads(v, GLA_DV)
    log_a_f = heads(jax.nn.log_sigmoid(lr_f @ wg_f.astype(jnp.float32) + bg_f.astype(jnp.float32)) / GLA_GATE_TAU, GLA_DK)
    log_a_b = heads(jax.nn.log_sigmoid(lr_b @ wg_b.astype(jnp.float32) + bg_b.astype(jnp.float32)) / GLA_GATE_TAU, GLA_DK)
    o_f = gla_chunked(q, k, v, log_a_f, True)
    flip = lambda t: jnp.flip(t, axis=2)
    o_b = flip(gla_chunked(flip(q), flip(k), flip(v), flip(log_a_b), False))
    o = (o_f + o_b).transpose(0, 2, 1, 3)
    o = o * lax.rsqrt(jnp.mean(o * o, axis=-1, keepdims=True) + NORM_EPS)
    o = o * head_gain.astype(jnp.float32).reshape(GLA_HEADS, GLA_DV)
    o = o.reshape(B, S, GLA_VALUE_WIDTH) * jax.nn.silu(r)
    return o.astype(h.dtype) @ w_out


def swa_mixer(h, w_qkv, b_qkv, sinks, w_out, b_out):
    B, S, _ = h.shape
    T = ATT_BLOCK
    n = S // T
    qkv = (h @ w_qkv + b_qkv).astype(jnp.float32)
    q, k, v = jnp.split(qkv, (N_Q_HEADS * HEAD_DIM, (N_Q_HEADS + N_KV_HEADS) * HEAD_DIM), axis=-1)
    q = q.reshape(B, n, T, N_KV_HEADS, GROUP, HEAD_DIM) * (HEAD_DIM ** -0.5)
    k = k.reshape(B, n, T, N_KV_HEADS, HEAD_DIM)
    v = v.reshape(B, n, T, N_KV_HEADS, HEAD_DIM)
    pad = ((0, 0), (1, 1), (0, 0), (0, 0), (0, 0))
    kp = jnp.pad(k, pad)
    vp = jnp.pad(v, pad)
    kw = jnp.concatenate([kp[:, :-2], kp[:, 1:-1], kp[:, 2:]], axis=2)
    vw = jnp.concatenate([vp[:, :-2], vp[:, 1:-1], vp[:, 2:]], axis=2)
    scores = jnp.einsum('bnqhgd,bnkhd->bnhgqk', q, kw)
    q_pos = jnp.arange(n)[:, None] * T + jnp.arange(T)[None, :]
    k_pos = (jnp.arange(n)[:, None] - 1) * T + jnp.arange(3 * T)[None, :]
    dist = jnp.abs(q_pos[:, :, None] - k_pos[:, None, :])
    valid = (dist <= WINDOW) & (k_pos >= 0)[:, None, :] & (k_pos < S)[:, None, :]
    slopes = alibi_slopes(N_Q_HEADS).reshape(N_KV_HEADS, GROUP)
    bias = jnp.where(valid[:, None, None], -slopes[None, :, :, None, None] * dist[:, None, None].astype(jnp.float32), -jnp.inf)
    scores = scores + bias[None]
    sink = sinks.astype(jnp.float32).reshape(N_KV_HEADS, GROUP)[None, None, :, :, None, None]
    m = jnp.maximum(jnp.max(scores, axis=-1, keepdims=True), sink)
    p = jnp.exp(scores - m)
    denom = jnp.sum(p, axis=-1, keepdims=True) + jnp.exp(sink - m)
    out = jnp.einsum('bnhgqk,bnkhd->bnqhgd', p / denom, vw)
    out = out.reshape(B, S, N_Q_HEADS * HEAD_DIM).astype(h.dtype)
    return out @ w_out + b_out


def swiglu(h, w_gate, w_up, w_down):
    return (jax.nn.silu(h @ w_gate) * (h @ w_up)) @ w_down


def moe_swiglu(h, router, w_gate, w_up, w_down):
    B, S, D = h.shape
    t = h.reshape(B * S, D)
    logits = (t @ router).astype(jnp.float32)
    top_val, top_idx = lax.top_k(logits, TOP_K)
    top_w = jax.nn.softmax(top_val, axis=-1)
    combine = jnp.sum(jax.nn.one_hot(top_idx, N_EXPERTS, dtype=jnp.float32) * top_w[..., None], axis=1)
    out = jnp.zeros((B * S, D), jnp.float32)
    for e in range(N_EXPERTS):
        out = out + combine[:, e:e + 1] * swiglu(t, w_gate[e], w_up[e], w_down[e]).astype(jnp.float32)
    return out.astype(h.dtype).reshape(B, S, D)


def _normal(key, shape, scale):
    return jax.random.normal(key, shape, jnp.float32) * scale


def setup_inputs(seed: int = 0) -> dict:
    key = jax.random.key(seed)
    ks = jax.random.split(key, 24)
    D, F, E = D_MODEL, D_FF, N_EXPERTS
    return {
        'x': _normal(ks[0], (BATCH, SEQ, D), 1.0),
        'mix_norm': 1.0 + _normal(ks[1], (DEPTH, D), 0.02),
        'ffn_norm': 1.0 + _normal(ks[2], (DEPTH, D), 0.02),
        'gla_in_proj': _normal(ks[3], (N_EVEN, D, GLA_IN_WIDTH), D ** -0.5),
        'gla_gate_w_fwd': _normal(ks[4], (N_EVEN, GLA_GATE_RANK, GLA_KEY_WIDTH), GLA_GATE_RANK ** -0.5),
        'gla_gate_b_fwd': _normal(ks[5], (N_EVEN, GLA_KEY_WIDTH), 0.1),
        'gla_gate_w_bwd': _normal(ks[6], (N_EVEN, GLA_GATE_RANK, GLA_KEY_WIDTH), GLA_GATE_RANK ** -0.5),
        'gla_gate_b_bwd': _normal(ks[7], (N_EVEN, GLA_KEY_WIDTH), 0.1),
        'gla_head_norm': 1.0 + _normal(ks[8], (N_EVEN, GLA_VALUE_WIDTH), 0.02),
        'gla_out_proj': _normal(ks[9], (N_EVEN, GLA_VALUE_WIDTH, D), GLA_VALUE_WIDTH ** -0.5),
        'swa_qkv_proj': _normal(ks[10], (N_ODD, D, SWA_QKV_WIDTH), D ** -0.5),
        'swa_qkv_bias': _normal(ks[11], (N_ODD, SWA_QKV_WIDTH), 0.02),
        'swa_sinks': _normal(ks[12], (N_ODD, N_Q_HEADS), 0.5),
        'swa_out_proj': _normal(ks[13], (N_ODD, N_Q_HEADS * HEAD_DIM, D), (N_Q_HEADS * HEAD_DIM) ** -0.5),
        'swa_out_bias': _normal(ks[14], (N_ODD, D), 0.02),
        'dense_w_gate': _normal(ks[15], (N_EVEN, D, F), D ** -0.5),
        'dense_w_up': _normal(ks[16], (N_EVEN, D, F), D ** -0.5),
        'dense_w_down': _normal(ks[17], (N_EVEN, F, D), F ** -0.5),
        'moe_router': _normal(ks[18], (N_ODD, D, E), D ** -0.5),
        'moe_w_gate': _normal(ks[19], (N_ODD, E, D, F), D ** -0.5),
        'moe_w_up': _normal(ks[20], (N_ODD, E, D, F), D ** -0.5),
        'moe_w_down': _normal(ks[21], (N_ODD, E, F, D), F ** -0.5),
        'final_norm': 1.0 + _normal(ks[22], (D,), 0.02),
    }


def reference(x, mix_norm, ffn_norm, gla_in_proj, gla_gate_w_fwd, gla_gate_b_fwd, gla_gate_w_bwd, gla_gate_b_bwd,
              gla_head_norm, gla_out_proj, swa_qkv_proj, swa_qkv_bias, swa_sinks, swa_out_proj, swa_out_bias,
              dense_w_gate, dense_w_up, dense_w_down, moe_router, moe_w_gate, moe_w_up, moe_w_down, final_norm):
    h = x
    for i in range(DEPTH):
        j = i // 2
        hn = rms_norm(h, mix_norm[i])
        if i % 2 == 0:
            h = h + gla_mixer(hn, gla_in_proj[j], gla_gate_w_fwd[j], gla_gate_b_fwd[j], gla_gate_w_bwd[j],
                              gla_gate_b_bwd[j], gla_head_norm[j], gla_out_proj[j])
        else:
            h = h + swa_mixer(hn, swa_qkv_proj[j], swa_qkv_bias[j], swa_sinks[j], swa_out_proj[j], swa_out_bias[j])
        hn = rms_norm(h, ffn_norm[i])
        if i % 2 == 0:
            h = h + swiglu(hn, dense_w_gate[j], dense_w_up[j], dense_w_down[j])
        else:
            h = h + moe_swiglu(hn, moe_router[j], moe_w_gate[j], moe_w_up[j], moe_w_down[j])
    return rms_norm(h, final_norm)
```

```python
import contextlib
import numpy as np
import concourse.bass as bass
import concourse.mybir as mybir
from concourse.bass_utils import run_bass_kernel_spmd

F32 = mybir.dt.float32
BF16 = mybir.dt.bfloat16
I32 = mybir.dt.int32
ALU = mybir.AluOpType
AF = mybir.ActivationFunctionType
AX = mybir.AxisListType

D = 1024
DC = 8
FF = 2816
FH = 1408
NFB = 11
NE = 8
EPS = 1e-5


class Buf:
    __slots__ = ("t", "name", "lw", "rd", "rd_dma", "excl")

    registry = []

    def __init__(self, t, name):
        Buf.registry.append(self)
        self.t = t
        self.name = name
        self.excl = False
        self.lw = None
        self.rd = {}
        self.rd_dma = []

    def __getitem__(self, idx):
        return self.t[idx]


class Op:
    __slots__ = ("eng", "fn", "deps", "needed", "val", "dma_key", "idx")

    def __init__(self, eng, fn, dma_key):
        self.eng = eng
        self.fn = fn
        self.deps = []
        self.needed = False
        self.val = None
        self.dma_key = dma_key
        self.idx = None


class Sched:
    ENGS = ("pe", "act", "dve", "pool", "sp")

    _uid = 0

    def __init__(self, nc):
        Sched._uid += 1
        self.uid = Sched._uid
        self.nc = nc
        self.ops = {e: [] for e in self.ENGS}
        self.stack = contextlib.ExitStack()
        self.sems = {}
        self.dma_count = {}
        self.dma_ops = []
        self.sem_used = {}
        self.slot = {}
        self.base = {}

    sem_pool = {"sw": [], "hw": [], "eng": []}
    sem_stack = None
    sem_total = {}

    def sem(self, key, cls="eng"):
        if key not in self.sems:
            i = self.sem_used.get(cls, 0)
            self.sem_used[cls] = i + 1
            pool = Sched.sem_pool[cls]
            if i >= len(pool):
                pool.append(Sched.sem_stack.enter_context(self.nc.semaphore("sem_%s_%d" % (cls, i))))
            self.sems[key] = pool[i]
            self.slot[key] = (cls, i)
            self.base[key] = Sched.sem_total.get((cls, i), 0)
        return self.sems[key]

    def sbuf(self, name, shape, dtype):
        t = self.stack.enter_context(self.nc.sbuf_tensor("%s_u%d" % (name, self.uid), list(shape), dtype))
        return Buf(t, name)

    def psum(self, name, shape, dtype):
        t = self.stack.enter_context(self.nc.psum_tensor("%s_u%d" % (name, self.uid), list(shape), dtype))
        b = Buf(t, name)
        b.excl = True
        return b

    def dbuf(self, name):
        return Buf(None, name)

    def op(self, eng, fn, reads=(), writes=(), dma_key=None):
        o = Op(eng, fn, dma_key)
        deps = set()
        for b in reads:
            if b.lw is not None:
                deps.add(b.lw)
            if b.excl:
                for en, r in b.rd.items():
                    if en != eng:
                        deps.add(r)
        for b in writes:
            if b.lw is not None:
                deps.add(b.lw)
            for r in b.rd.values():
                deps.add(r)
            for r in b.rd_dma:
                deps.add(r)
        for d in deps:
            if d is o:
                continue
            if d.dma_key is None and d.eng == eng:
                if eng == "pe" or eng == "sp":
                    continue
                if not any((b.lw is d) for b in reads):
                    continue
            d.needed = True
            o.deps.append(d)
        for b in reads:
            if dma_key is not None:
                b.rd_dma.append(o)
            else:
                b.rd[eng] = o
        for b in writes:
            b.lw = o
            b.rd = {}
            b.rd_dma = []
        if dma_key is not None:
            self.sem(dma_key, "sw" if eng == "pool" else "hw")
            self.dma_count[dma_key] = self.dma_count.get(dma_key, 0) + 16
            o.val = self.base[dma_key] + self.dma_count[dma_key]
            o.needed = True
            self.dma_ops.append(o)
        self.ops[eng].append(o)
        return o

    def dma(self, eng, out_ap, in_ap, key, reads=(), writes=(), **kw):
        return self.op(eng, lambda e: e.dma_start(out=out_ap, in_=in_ap, **kw),
                       reads=reads, writes=writes, dma_key=key)

    def prepare(self):
        self.totals = {}
        for e in self.ENGS:
            c = 0
            if any(o.dma_key is None and o.needed for o in self.ops[e]):
                self.sem("E" + e)
                c0 = self.base["E" + e]
                for o in self.ops[e]:
                    if o.dma_key is None and o.needed:
                        c += 1
                        o.val = c0 + c
                self.totals[self.slot["E" + e]] = c0 + c
        for key, n in self.dma_count.items():
            self.totals[self.slot[key]] = self.base[key] + n
        fin = Op("sp", None, None)
        last = {}
        for o in self.dma_ops:
            last[o.dma_key] = o
        fin.deps = list(last.values())
        self.ops["sp"].append(fin)

    def run(self, e, eng):
        waited = {}
        for o in self.ops[e]:
            for d in o.deps:
                key = d.dma_key if d.dma_key is not None else "E" + d.eng
                if waited.get(key, 0) >= d.val:
                    continue
                waited[key] = d.val
                eng.wait_ge(self.sems[key], d.val)
            if o.fn is None:
                continue
            ins = o.fn(eng)
            if o.dma_key is not None:
                ins.then_inc(self.sems[o.dma_key], 16)
            elif o.needed:
                ins.then_inc(self.sems["E" + e], 1)

    def emit(self):
        nc = self.nc
        self.prepare()
        with nc.Block() as block:
            @block.tensor
            def _(eng):
                self.run("pe", eng)

            @block.scalar
            def _(eng):
                self.run("act", eng)

            @block.vector
            def _(eng):
                self.run("dve", eng)

            @block.gpsimd
            def _(eng):
                self.run("pool", eng)

            @block.sync
            def _(eng):
                self.run("sp", eng)
        Sched.sem_total.update(self.totals)
        self.stack.close()


def emit_either(nc, flag, regs, sa, sb):
    sa.prepare()
    sb.prepare()

    def body(e, eng):
        r = regs[e]
        eng.reg_load(r, flag[0:1, 0:1])
        with eng.If_eq(r, 1):
            sa.run(e, eng)
        with eng.Else():
            sb.run(e, eng)

    with nc.Block() as block:
        @block.tensor
        def _(eng):
            body("pe", eng)

        @block.scalar
        def _(eng):
            body("act", eng)

        @block.vector
        def _(eng):
            body("dve", eng)

        @block.gpsimd
        def _(eng):
            body("pool", eng)

        @block.sync
        def _(eng):
            body("sp", eng)
    sa.stack.close()
    sb.stack.close()


class Ctx:
    pass


def bcast_row(ap_row, nparts=128):
    return ap_row.partition_broadcast(nparts)


def emit_consts(cx):
    s = cx.s
    cx.ident = s.sbuf("ident", [128, 128], BF16)
    cx.identf = s.sbuf("identf", [128, 128], F32)
    s.op("pool", lambda e: e.memset(cx.identf[:], 1.0), writes=[cx.identf])
    s.op("pool", lambda e: e.affine_select(out=cx.identf[:], in_=cx.identf[:], pattern=[[-1, 128]],
                                           compare_op=ALU.is_equal, fill=0.0, base=0,
                                           channel_multiplier=1),
         reads=[cx.identf], writes=[cx.identf])
    s.op("dve", lambda e: e.tensor_copy(out=cx.ident[:], in_=cx.identf[:]),
         reads=[cx.identf], writes=[cx.ident])
    cx.epsc = s.sbuf("epsc", [128, 1], F32)
    s.op("pool", lambda e: e.memset(cx.epsc[:], EPS), writes=[cx.epsc])


def emit_norm_T(cx, x, gain_bc, hn, bank, hnT, ssq, rstd, junk, xdeps=None, evac_eng="act"):
    s = cx.s
    xd = [x] if xdeps is None else list(xdeps)
    psT = Bview(bank)
    s.op("act", lambda e: e.activation(out=junk[:], in_=x[:], func=AF.Square, scale=1.0 / 32.0,
                                       accum_out=ssq[:]),
         reads=xd, writes=[junk, ssq])
    s.op("act", lambda e: e.activation(out=rstd[:], in_=ssq[:], func=AF.Ln, bias=cx.epsc[:, 0:1]),
         reads=[ssq, cx.epsc], writes=[rstd])
    s.op("act", lambda e: e.activation(out=rstd[:], in_=rstd[:], func=AF.Exp, scale=-0.5),
         reads=[rstd], writes=[rstd])
    s.op("dve", lambda e: e.scalar_tensor_tensor(out=hn[:], in0=x[:], scalar=rstd[:, 0:1], in1=gain_bc[:],
                                                 op0=ALU.mult, op1=ALU.mult),
         reads=xd + [rstd, gain_bc], writes=[hn])
    for c in range(DC):
        s.op("pe", lambda e, c=c: e.transpose(out=psT[:, c * 128:(c + 1) * 128],
                                              in_=hn[:, c * 128:(c + 1) * 128], identity=cx.ident[:]),
             reads=[hn, cx.ident], writes=[bank])
    if evac_eng == "act":
        s.op("act", lambda e: e.copy(out=hnT[:].rearrange("p c t -> p (c t)"), in_=psT[:]),
             reads=[bank], writes=[hnT])
    else:
        s.op("dve", lambda e: e.tensor_copy(out=hnT[:].rearrange("p c t -> p (c t)"), in_=psT[:]),
             reads=[bank], writes=[hnT])


class Bview:
    def __init__(self, b):
        self.b = b

    def __getitem__(self, idx):
        return self.b.t[:].bitcast(BF16)[idx]


def sub(parent, name):
    return Buf(parent.t, name)


def emit_psum(cx):
    cx.ps = [cx.s.psum("ps%d" % i, [128, 512], F32) for i in range(8)]


class WSet:
    pass


def alloc_wset(cx, k):
    s = cx.s
    w = WSet()
    w.wg = s.sbuf("wg%d" % k, [128, DC, FH], BF16)
    w.wu = s.sbuf("wu%d" % k, [128, DC, FH], BF16)
    w.wd = s.sbuf("wd%d" % k, [128, NFB, D], BF16)
    w.wg_c = [sub(w.wg, "wg%d_%d" % (k, c)) for c in range(DC)]
    w.wu_c = [sub(w.wu, "wu%d_%d" % (k, c)) for c in range(DC)]
    w.wd_c = [sub(w.wd, "wd%d_%d" % (k, c)) for c in range(NFB)]
    return w


def ffn_weight_loads(cx, w, Wg, Wu, Wd, half):
    s = cx.s
    out = []
    f0 = half * FH
    for c in range(DC):
        out.append(lambda c=c: s.dma("pool", w.wg.t[:, c, :], Wg[c * 128:(c + 1) * 128, f0:f0 + FH],
                                     w.wg_c[c].name, writes=[w.wg_c[c]]))
        out.append(lambda c=c: s.dma("pool", w.wu.t[:, c, :], Wu[c * 128:(c + 1) * 128, f0:f0 + FH],
                                     w.wu_c[c].name, writes=[w.wu_c[c]]))
    for fb in range(NFB):
        out.append(lambda fb=fb: s.dma("pool", w.wd.t[:, fb, :], Wd[f0 + fb * 128:f0 + (fb + 1) * 128, :],
                                       w.wd_c[fb].name, writes=[w.wd_c[fb]]))
    return out


def alloc_ffn_bufs(cx, TB):
    s = cx.s
    cx.TB = TB
    cx.hnTb = [s.sbuf("hnTb%d" % i, [128, DC, TB], BF16) for i in range(2)]
    cx.hT = [s.sbuf("hT%d" % i, [128, NFB, TB], BF16) for i in range(2)]
    cx.sg = [s.sbuf("sg%d" % i, [128, TB], BF16) for i in range(2)]
    cx.stg = [s.sbuf("stg%d" % i, [128, D], F32) for i in range(3)]
    cx.stgA = [sub(b, b.name + "A") for b in cx.stg]
    cx.stgB = [sub(b, b.name + "B") for b in cx.stg]


def emit_ffn_pass(cx, S, hnT_ap, hnT_db, w, cw, cw_col, acc_ap, acc_db, pre=(), ctr=None,
                  loader=None, sinker=None, prefetch=None):
    s = cx.s
    TB = cx.TB
    nblk = S // TB
    ntt = TB // 128
    hview = hnT_ap.rearrange("(c p) t -> p c t", p=128) if hnT_ap is not None else None
    pre = list(pre)
    per_blk = (len(pre) + nblk - 1) // nblk if pre else 0
    if ctr is None:
        ctr = {"blk": 0, "tile": 0, "fb": 0}

    def gu(b):
        k = ctr["blk"] + b
        hb = cx.hnTb[k % 2]
        if loader is not None:
            loader(b, hb)
        else:
            s.dma("sp", hb[:], hview[:, :, b * TB:(b + 1) * TB], hb.name,
                  reads=[hnT_db[b * ntt + i] for i in range(ntt)], writes=[hb])
        hT = cx.hT[k % 2]
        for fb in range(NFB):
            q = ctr["fb"]
            ctr["fb"] += 1
            pg = cx.ps[(2 * q) % 4]
            pu = cx.ps[(2 * q + 1) % 4]
            sg = cx.sg[q % 2]
            for c in range(DC):
                s.op("pe", lambda e, c=c, pg=pg, fb=fb: e.matmul(
                    pg[:, :TB], lhsT=w.wg.t[:, c, fb * 128:(fb + 1) * 128], rhs=hb[:, c, :],
                    start=(c == 0), stop=(c == DC - 1)), reads=[w.wg_c[c], hb], writes=[pg])
            for c in range(DC):
                s.op("pe", lambda e, c=c, pu=pu, fb=fb: e.matmul(
                    pu[:, :TB], lhsT=w.wu.t[:, c, fb * 128:(fb + 1) * 128], rhs=hb[:, c, :],
                    start=(c == 0), stop=(c == DC - 1)), reads=[w.wu_c[c], hb], writes=[pu])
            s.op("act", lambda e, pg=pg, sg=sg: e.activation(out=sg[:], in_=pg[:, :TB], func=AF.Silu),
                 reads=[pg], writes=[sg])
            s.op("dve", lambda e, pu=pu, sg=sg, fb=fb: e.tensor_tensor(
                out=hT[:, fb, :], in0=pu[:, :TB], in1=sg[:], op=ALU.mult),
                reads=[pu, sg], writes=[hT])

    def down(b):
        k = ctr["blk"] + b
        hT = cx.hT[k % 2]
        for tt in range(ntt):
            tile = b * ntt + tt
            q = ctr["tile"]
            ctr["tile"] += 1
            pd = (cx.ps[4 + (q % 2) * 2], cx.ps[5 + (q % 2) * 2])
            for half in range(2):
                for fb in range(NFB):
                    s.op("pe", lambda e, half=half, fb=fb, tt=tt, pd=pd: e.matmul(
                        pd[half][:], lhsT=hT[:, fb, tt * 128:(tt + 1) * 128],
                        rhs=w.wd.t[:, fb, half * 512:(half + 1) * 512],
                        start=(fb == 0), stop=(fb == NFB - 1)),
                        reads=[hT, w.wd_c[fb]], writes=[pd[half]])
            j = q % 3
            stg, sa, sb_ = cx.stg[j], cx.stgA[j], cx.stgB[j]
            if sinker is not None:
                sinker(tile, pd, stg, sa, sb_)
                continue
            if cw is None:
                s.op("act", lambda e, stg=stg, pd=pd: e.copy(out=stg[:, 0:512], in_=pd[0][:]),
                     reads=[pd[0]], writes=[sa])
                s.op("dve", lambda e, stg=stg, pd=pd: e.tensor_copy(out=stg[:, 512:1024], in_=pd[1][:]),
                     reads=[pd[1]], writes=[sb_])
            else:
                col = cw_col(tile)
                s.op("act", lambda e, stg=stg, col=col, pd=pd: e.activation(
                    out=stg[:, 0:512], in_=pd[0][:], func=AF.Copy, scale=cw[:, col:col + 1]),
                    reads=[pd[0], cw], writes=[sa])
                s.op("dve", lambda e, stg=stg, col=col, pd=pd: e.tensor_scalar(
                    out=stg[:, 512:1024], in0=pd[1][:], scalar1=cw[:, col:col + 1], scalar2=None,
                    op0=ALU.mult), reads=[pd[1], cw], writes=[sb_])
            s.dma("pool", acc_ap[tile * 128:(tile + 1) * 128, :], stg[:], stg.name,
                  reads=[sa, sb_, acc_db[tile]], writes=[acc_db[tile]], accum_op=ALU.add)

    if prefetch is not None:
        prefetch(0)
        if nblk > 1:
            prefetch(1)
    gu(0)
    for b in range(nblk):
        if prefetch is not None and b + 2 < nblk:
            prefetch(b + 2)
        if b + 1 < nblk:
            gu(b + 1)
        down(b)
        for _ in range(per_blk):
            if pre:
                pre.pop(0)()
    while pre:
        pre.pop(0)()
    ctr["blk"] += nblk
    return ctr


GQ, GK, GV, GR, GLF, GLB = 0, 512, 1024, 2048, 3072, 3088
GW = 3104
NH = 4
import os as _os
_GSTOP = int(_os.environ.get('GSTOP', '-1'))


def tri(cx, name, val, pattern, cm, cmp, dtype=F32, ncols=128):
    s = cx.s
    b = s.sbuf(name, [128, ncols], F32)
    s.op("pool", lambda e: e.memset(b[:], val), writes=[b])
    s.op("pool", lambda e: e.affine_select(out=b[:, 0:128], in_=b[:, 0:128], pattern=[[pattern, 128]],
                                           compare_op=cmp, fill=0.0, base=0, channel_multiplier=cm),
         reads=[b], writes=[b])
    return b


def alloc_gla(cx, P):
    s = cx.s
    g = cx.g = Ctx()
    c16 = -1.0 / 16.0
    g.Rf = s.sbuf("Rf", [128, 257], F32)
    g.Rb = s.sbuf("Rb", [128, 257], F32)
    uf = tri(cx, "Uf", c16, 1, -1, ALU.is_ge)
    ub = tri(cx, "Ub", c16, -1, 1, ALU.is_ge)
    for R, U, ref in ((g.Rf, uf, 64), (g.Rb, ub, 63)):
        s.op("pool", lambda e, R=R: e.memset(R[:, 256:257], c16), writes=[R])
        s.op("dve", lambda e, R=R, U=U: e.tensor_copy(out=R[:, 0:128], in_=U[:]), reads=[U, R], writes=[R])
        s.op("dve", lambda e, R=R, U=U, ref=ref: e.tensor_scalar(
            out=R[:, 128:256], in0=U[:], scalar1=U[:, ref:ref + 1], scalar2=None, op0=ALU.subtract),
            reads=[U, R], writes=[R])
    g.Mkf = tri(cx, "Mkf", c16, -1, 1, ALU.is_gt)
    g.Mkb = tri(cx, "Mkb", c16, 1, -1, ALU.is_gt)
    g.maskf = tri(cx, "maskf", 1.0, 1, -1, ALU.is_ge)
    g.maskb = tri(cx, "maskb", 1.0, -1, 1, ALU.is_gt)
    g.Win = s.sbuf("Win", [128, DC, GW], BF16)
    g.Win_c = [sub(g.Win, "Win_%d" % c) for c in range(DC)]
    for c in range(DC):
        s.dma("pool", g.Win.t[:, c, :], P["gla_in"][c * 128:(c + 1) * 128, :], g.Win_c[c].name,
              writes=[g.Win_c[c]])
    g.Wout = s.sbuf("Wout", [128, DC, D], BF16)
    g.Wout_c = [sub(g.Wout, "Wout_%d" % c) for c in range(DC)]
    for c in range(DC):
        s.dma("pool", g.Wout.t[:, c, :], P["gla_out"][c * 128:(c + 1) * 128, :], g.Wout_c[c].name,
              writes=[g.Wout_c[c]])
    g.wga = []
    for d, (wk, bk) in enumerate((("gw_f", "gb_f"), ("gw_b", "gb_b"))):
        wa = s.sbuf("wga%d" % d, [17, 512], F32)
        wa1 = sub(wa, "wga%d_b" % d)
        s.dma("sp", wa[0:16, :], P[wk], wa.name, writes=[wa])
        s.dma("sp", wa[16:17, :], P[bk].rearrange("(o n) -> o n", o=1), wa1.name, writes=[wa1])
        g.wga.append((wa, wa1))
    g.gain1 = s.sbuf("gain1", [128, D], F32)
    s.dma("sp", g.gain1[:], bcast_row(P["mix_norm0"]), "gain1", writes=[g.gain1])
    g.gain2 = s.sbuf("gain2", [128, D], F32)
    s.dma("sp", g.gain2[:], bcast_row(P["ffn_norm0"]), "gain2", writes=[g.gain2])
    g.hgain = s.sbuf("hgain", [128, D], F32)
    s.dma("sp", g.hgain[:], bcast_row(P["gla_hn"]), "hgain", writes=[g.hgain])
    g.one_c = s.sbuf("one_c", [128, 1], F32)
    s.op("pool", lambda e: e.memset(g.one_c[:], 1.0), writes=[g.one_c])
    g.eps256 = s.sbuf("eps256", [128, 1], F32)
    s.op("pool", lambda e: e.memset(g.eps256[:], EPS), writes=[g.eps256])
    g.S = [[s.sbuf("gS%d_%d" % (d, h), [128, 256], F32) for h in range(NH)] for d in range(2)]
    for d in range(2):
        for h in range(NH):
            s.op("pool", lambda e, b=g.S[d][h]: e.memset(b[:], 0.0), writes=[g.S[d][h]])
    g.Sbf = [s.sbuf("gSbf%d" % d, [128, NH, 256], BF16) for d in range(2)]
    g.Sbf_h = [[sub(g.Sbf[d], "gSbf%d_%d" % (d, h)) for h in range(NH)] for d in range(2)]
    for d in range(2):
        s.op("pool", lambda e, b=g.Sbf[d]: e.memset(b[:], 0.0), writes=[g.Sbf[d]] + g.Sbf_h[d])
    g.hn = s.sbuf("ghn", [128, D], BF16)
    g.hnT = s.sbuf("ghnT", [128, DC, 128], BF16)
    g.junk = s.sbuf("gjunk", [128, D], F32)
    g.ssq = s.sbuf("gssq", [128, 1], F32)
    g.rstd = s.sbuf("grstd", [128, 1], F32)
    g.rbf = s.sbuf("grbf", [128, D], BF16)
    g.lrT = []
    for d in range(2):
        b = s.sbuf("glrT%d" % d, [17, 128], F32)
        s.op("pool", lambda e, b=b: e.memset(b[:], 1.0), writes=[b])
        g.lrT.append(b)
    g.la = [s.sbuf("gla%d" % d, [128, 512], F32) for d in range(2)]
    g.Ekd = [s.sbuf("gEkd%d" % d, [128, 512], F32) for d in range(2)]
    g.Eall = [[s.sbuf("gEall%d_%d" % (d, h), [128, 257], F32) for h in range(NH)] for d in range(2)]
    g.E2 = [[s.sbuf("gE2%d_%d" % (d, h), [128, 128], F32) for h in range(NH)] for d in range(2)]
    g.ssq4 = s.sbuf("gssq4", [128, NH], F32)
    g.rstd4 = s.sbuf("grstd4", [128, NH], F32)
    g.og = s.sbuf("gog", [128, D], F32)
    g.og_h = [sub(g.og, "gog_%d" % h) for h in range(NH)]
    g.gated = s.sbuf("ggated", [128, D], BF16)
    g.gT = s.sbuf("ggT", [128, DC, 128], BF16)
    g.h1 = s.sbuf("gh1", [128, D], F32)
    g.h1A = sub(g.h1, "gh1A")
    g.h1B = sub(g.h1, "gh1B")
    g.hn2 = s.sbuf("ghn2", [128, D], BF16)
    g.hn2T = s.sbuf("ghn2T", [128, DC, 128], BF16)
    g.sets = []
    for k in range(2):
        w = Ctx()
        n = lambda nm: "%s_k%d" % (nm, k)
        w.x = s.sbuf(n("gx"), [128, D], F32)
        w.vbf = s.sbuf(n("gvbf"), [128, D], BF16)
        w.sig = s.sbuf(n("gsig"), [128, D], F32)
        w.er = w.sig
        w.kd = [s.sbuf(n("gkd%d" % d), [128, 512], BF16) for d in range(2)]
        w.qe = [[s.sbuf(n("gqe%d_%d" % (d, h)), [128, 128], BF16) for h in range(NH)] for d in range(2)]
        w.qb = [[s.sbuf(n("gqb%d_%d" % (d, h)), [128, 128], BF16) for h in range(NH)] for d in range(2)]
        w.ke = [[s.sbuf(n("gke%d_%d" % (d, h)), [128, 128], BF16) for h in range(NH)] for d in range(2)]
        w.PT = [[s.sbuf(n("gPT%d_%d" % (d, h)), [128, 128], BF16) for h in range(NH)] for d in range(2)]
        w.dec = [s.sbuf(n("gdec%d" % d), [128, NH], F32) for d in range(2)]
        w.Sb1 = s.sbuf(n("gSb1"), [128, NH, 256], BF16)
        g.sets.append(w)


class _GView:
    def __init__(self, base, ws):
        self._b = base
        self._w = ws

    def __getattr__(self, n):
        w = object.__getattribute__(self, "_w")
        if hasattr(w, n):
            return getattr(w, n)
        return getattr(object.__getattribute__(self, "_b"), n)


def emit_gla_tile(cx, t, full, x_ap, Sb_ap, Sb_db, h1_ap, h1_db, hnT_ap, hnT_db):
    s = cx.s
    g = _GView(cx.g, cx.g.sets[t % 2])
    ps = cx.ps
    W = g.Win.t
    rows = slice(t * 128, (t + 1) * 128)
    s.dma("sp", g.x[:], x_ap[rows, :], g.x.name, writes=[g.x])
    emit_norm_T(cx, g.x, g.gain1, g.hn, ps[0], g.hnT, g.ssq, g.rstd, g.junk)
    hnT = g.hnT
    dirs = (0, 1) if full else (1,)
    upd_dirs = (0,) if full else (1,)

    def proj_tok(bank, col0, n=512):
        for c in range(DC):
            s.op("pe", lambda e, c=c: e.matmul(bank[:, 0:n], lhsT=hnT[:, c, :], rhs=W[:, c, col0:col0 + n],
                                               start=(c == 0), stop=(c == DC - 1)),
                 reads=[hnT, g.Win_c[c]], writes=[bank])

    def proj_feat(bank, bcol, col0, m):
        for c in range(DC):
            s.op("pe", lambda e, c=c: e.matmul(bank[0:m, bcol:bcol + 128], lhsT=W[:, c, col0:col0 + m],
                                               rhs=hnT[:, c, :], start=(c == 0), stop=(c == DC - 1)),
                 reads=[hnT, g.Win_c[c]], writes=[bank])

    proj_tok(ps[1], GK)
    proj_tok(ps[2], GV)
    proj_tok(ps[3], GV + 512)
    if full:
        proj_tok(ps[4], GR)
        proj_tok(ps[5], GR + 512)
        for h in range(NH):
            proj_feat(ps[6], h * 128, GQ + h * 128, 128)
        for h in range(NH):
            proj_feat(ps[7], h * 128, GK + h * 128, 128)
    for d in dirs:
        proj_feat(ps[0], d * 128, GLF + 16 * d, 16)
    if _GSTOP == 0 and full:
        return
    s.op("act", lambda e: e.copy(out=g.vbf[:, 0:512], in_=ps[2][:]), reads=[ps[2]], writes=[g.vbf])
    s.op("act", lambda e: e.copy(out=g.vbf[:, 512:1024], in_=ps[3][:]), reads=[ps[3], g.vbf], writes=[g.vbf])
    if full:
        for hh in range(2):
            sl = slice(hh * 512, (hh + 1) * 512)
            s.op("act", lambda e, hh=hh, sl=sl: e.activation(out=g.er[:, sl], in_=ps[4 + hh][:], func=AF.Exp,
                                                             scale=-1.0),
                 reads=[ps[4 + hh], g.er], writes=[g.er])
            s.op("dve", lambda e, hh=hh, sl=sl: e.tensor_copy(out=g.rbf[:, sl], in_=ps[4 + hh][:]),
                 reads=[ps[4 + hh], g.rbf], writes=[g.rbf])
        s.op("act", lambda e: e.activation(out=g.er[:], in_=g.er[:], func=AF.Ln, bias=g.one_c[:, 0:1]),
             reads=[g.er, g.one_c], writes=[g.er])
        s.op("act", lambda e: e.activation(out=g.sig[:], in_=g.er[:], func=AF.Exp, scale=-1.0),
             reads=[g.er], writes=[g.sig])
        s.op("dve", lambda e: e.tensor_tensor(out=g.sig[:], in0=g.sig[:], in1=g.rbf[:], op=ALU.mult),
             reads=[g.sig, g.rbf], writes=[g.sig])
    for d in dirs:
        s.op("dve", lambda e, d=d: e.tensor_copy(out=g.lrT[d][0:16, :], in_=ps[0][0:16, d * 128:(d + 1) * 128]),
             reads=[ps[0], g.lrT[d]], writes=[g.lrT[d]])
    if _GSTOP == 1 and full:
        return
    for d in dirs:
        zb = ps[2 + d]
        s.op("pe", lambda e, d=d, zb=zb: e.matmul(zb[:], lhsT=g.lrT[d][:], rhs=g.wga[d][0][:], start=True, stop=True),
             reads=[g.lrT[d], g.wga[d][0], g.wga[d][1]], writes=[zb])
        s.op("act", lambda e, d=d, zb=zb: e.activation(out=g.la[d][:], in_=zb[:], func=AF.Exp, scale=-1.0),
             reads=[zb], writes=[g.la[d]])
        s.op("act", lambda e, d=d: e.activation(out=g.la[d][:], in_=g.la[d][:], func=AF.Ln, bias=g.one_c[:, 0:1]),
             reads=[g.la[d], g.one_c], writes=[g.la[d]])
    if _GSTOP == 2 and full:
        return
    for d in upd_dirs:
        Mk = g.Mkf if d == 0 else g.Mkb
        xb = ps[2 + d]
        s.op("pe", lambda e, d=d, Mk=Mk, xb=xb: e.matmul(xb[:], lhsT=Mk[:], rhs=g.la[d][:], start=True, stop=True),
             reads=[Mk, g.la[d]], writes=[xb])
        s.op("act", lambda e, d=d, xb=xb: e.activation(out=g.Ekd[d][:], in_=xb[:], func=AF.Exp),
             reads=[xb], writes=[g.Ekd[d]])
        s.op("dve", lambda e, d=d: e.tensor_tensor(out=g.kd[d][:], in0=ps[1][:], in1=g.Ekd[d][:], op=ALU.mult),
             reads=[ps[1], g.Ekd[d]], writes=[g.kd[d]])
    cnt = 0
    for d in dirs:
        R = g.Rf if d == 0 else g.Rb
        for h in range(NH):
            cb = ps[4 + (cnt % 2)]
            cnt += 1
            if full:
                s.op("pe", lambda e, d=d, h=h, cb=cb, R=R: e.matmul(
                    cb[:, 0:257], lhsT=g.la[d][:, h * 128:(h + 1) * 128], rhs=R[:], start=True, stop=True),
                    reads=[g.la[d], R], writes=[cb])
                s.op("act", lambda e, d=d, h=h, cb=cb: e.activation(out=g.Eall[d][h][:], in_=cb[:, 0:257], func=AF.Exp),
                     reads=[cb], writes=[g.Eall[d][h]])
                s.op("act", lambda e, d=d, h=h, cb=cb: e.activation(out=g.E2[d][h][:], in_=cb[:, 128:256], func=AF.Exp,
                                                                   scale=-1.0),
                     reads=[cb], writes=[g.E2[d][h]])
                s.op("dve", lambda e, d=d, h=h: e.tensor_copy(out=g.dec[d][:, h:h + 1], in_=g.Eall[d][h][:, 256:257]),
                     reads=[g.Eall[d][h], g.dec[d]], writes=[g.dec[d]])
            else:
                s.op("pe", lambda e, d=d, h=h, cb=cb, R=R: e.matmul(
                    cb[:, 0:1], lhsT=g.la[d][:, h * 128:(h + 1) * 128], rhs=R[:, 256:257], start=True, stop=True),
                    reads=[g.la[d], R], writes=[cb])
                s.op("act", lambda e, d=d, h=h, cb=cb: e.activation(out=g.dec[d][:, h:h + 1], in_=cb[:, 0:1], func=AF.Exp),
                     reads=[cb, g.dec[d]], writes=[g.dec[d]])
    if full:
        if _GSTOP == 3 and full:
            return
        sc = float(128 ** -0.5)
        for d in dirs:
            for h in range(NH):
                qps = ps[6][:, h * 128:(h + 1) * 128]
                kps = ps[7][:, h * 128:(h + 1) * 128]
                E = g.Eall[d][h]
                s.op("dve", lambda e, d=d, h=h, qps=qps, E=E: e.scalar_tensor_tensor(
                    out=g.qb[d][h][:], in0=qps, scalar=sc, in1=E[:, 0:128], op0=ALU.mult, op1=ALU.mult),
                    reads=[ps[6], E], writes=[g.qb[d][h]])
                s.op("dve", lambda e, d=d, h=h, qps=qps, E=E: e.scalar_tensor_tensor(
                    out=g.qe[d][h][:], in0=qps, scalar=sc, in1=E[:, 128:256], op0=ALU.mult, op1=ALU.mult),
                    reads=[ps[6], E], writes=[g.qe[d][h]])
                s.op("dve", lambda e, d=d, h=h, kps=kps: e.tensor_tensor(
                    out=g.ke[d][h][:], in0=kps, in1=g.E2[d][h][:], op=ALU.mult),
                    reads=[ps[7], g.E2[d][h]], writes=[g.ke[d][h]])
        s.dma("sp", g.Sb1[:].rearrange("p h v -> p (h v)"), Sb_ap[t], g.Sb1.name, reads=[Sb_db[t]], writes=[g.Sb1])
        if _GSTOP == 4 and full:
            return
        for d in dirs:
            mask = g.maskf if d == 0 else g.maskb
            sb_ = ps[2 + d]
            for h in range(NH):
                s.op("pe", lambda e, d=d, h=h, sb_=sb_: e.matmul(
                    sb_[:, h * 128:(h + 1) * 128], lhsT=g.ke[d][h][:], rhs=g.qe[d][h][:], start=True, stop=True),
                    reads=[g.ke[d][h], g.qe[d][h]], writes=[sb_])
            for h in range(NH):
                s.op("dve", lambda e, d=d, h=h, sb_=sb_, mask=mask: e.tensor_tensor(
                    out=g.PT[d][h][:], in0=sb_[:, h * 128:(h + 1) * 128], in1=mask[:], op=ALU.mult),
                    reads=[sb_, mask], writes=[g.PT[d][h]])
        if _GSTOP == 5 and full:
            return
        for h in range(NH):
            ob = ps[4 + h // 2]
            oc = slice((h % 2) * 256, (h % 2) * 256 + 256)
            vs = g.vbf[:, h * 256:(h + 1) * 256]
            s.op("pe", lambda e, h=h, ob=ob, oc=oc, vs=vs: e.matmul(ob[:, oc], lhsT=g.PT[0][h][:], rhs=vs,
                                                                   start=True, stop=False),
                 reads=[g.PT[0][h], g.vbf], writes=[ob])
            s.op("pe", lambda e, h=h, ob=ob, oc=oc, vs=vs: e.matmul(ob[:, oc], lhsT=g.PT[1][h][:], rhs=vs,
                                                                   start=False, stop=False),
                 reads=[g.PT[1][h], g.vbf], writes=[ob])
            s.op("pe", lambda e, h=h, ob=ob, oc=oc: e.matmul(ob[:, oc], lhsT=g.qb[0][h][:], rhs=g.Sbf[0][:, h, :],
                                                            start=False, stop=False),
                 reads=[g.qb[0][h], g.Sbf_h[0][h]], writes=[ob])
            s.op("pe", lambda e, h=h, ob=ob, oc=oc: e.matmul(ob[:, oc], lhsT=g.qb[1][h][:], rhs=g.Sb1[:, h, :],
                                                            start=False, stop=True),
                 reads=[g.qb[1][h], g.Sb1], writes=[ob])
        if _GSTOP == 6 and full:
            return
        for h in range(NH):
            ob = ps[4 + h // 2]
            oc = slice((h % 2) * 256, (h % 2) * 256 + 256)
            s.op("act", lambda e, h=h, ob=ob, oc=oc: e.activation(
                out=g.junk[:, h * 256:(h + 1) * 256], in_=ob[:, oc], func=AF.Square, scale=1.0 / 16.0,
                accum_out=g.ssq4[:, h:h + 1]), reads=[ob, g.junk, g.ssq4], writes=[g.junk, g.ssq4])
        s.op("act", lambda e: e.activation(out=g.rstd4[:], in_=g.ssq4[:], func=AF.Ln, bias=g.eps256[:, 0:1]),
             reads=[g.ssq4, g.eps256], writes=[g.rstd4])
        s.op("act", lambda e: e.activation(out=g.rstd4[:], in_=g.rstd4[:], func=AF.Exp, scale=-0.5),
             reads=[g.rstd4], writes=[g.rstd4])
        for h in range(NH):
            ob = ps[4 + h // 2]
            oc = slice((h % 2) * 256, (h % 2) * 256 + 256)
            hs = slice(h * 256, (h + 1) * 256)
            s.op("dve", lambda e, h=h, ob=ob, oc=oc, hs=hs: e.scalar_tensor_tensor(
                out=g.og[:, hs], in0=ob[:, oc], scalar=g.rstd4[:, h:h + 1], in1=g.hgain[:, hs],
                op0=ALU.mult, op1=ALU.mult), reads=[ob, g.rstd4, g.hgain], writes=[g.og_h[h]])
        s.op("dve", lambda e: e.tensor_tensor(out=g.gated[:], in0=g.og[:], in1=g.sig[:], op=ALU.mult),
             reads=g.og_h + [g.sig], writes=[g.gated])
        if _GSTOP == 7 and full:
            return
        psT = Bview(ps[0])
        for c in range(DC):
            s.op("pe", lambda e, c=c: e.transpose(out=psT[:, c * 128:(c + 1) * 128],
                                                  in_=g.gated[:, c * 128:(c + 1) * 128], identity=cx.ident[:]),
                 reads=[g.gated, cx.ident], writes=[ps[0]])
        s.op("act", lambda e: e.copy(out=g.gT[:].rearrange("p c t -> p (c t)"), in_=psT[:]),
             reads=[ps[0]], writes=[g.gT])
        for hh in range(2):
            yb = ps[2 + hh]
            for c in range(DC):
                s.op("pe", lambda e, c=c, hh=hh, yb=yb: e.matmul(
                    yb[:], lhsT=g.gT[:, c, :], rhs=g.Wout.t[:, c, hh * 512:(hh + 1) * 512],
                    start=(c == 0), stop=(c == DC - 1)), reads=[g.gT, g.Wout_c[c]], writes=[yb])
        s.op("dve", lambda e: e.tensor_tensor(out=g.h1[:, 0:512], in0=ps[2][:], in1=g.x[:, 0:512], op=ALU.add),
             reads=[ps[2], g.x], writes=[g.h1A])
        s.op("dve", lambda e: e.tensor_tensor(out=g.h1[:, 512:1024], in0=ps[3][:], in1=g.x[:, 512:1024], op=ALU.add),
             reads=[ps[3], g.x], writes=[g.h1B])
        s.dma("sp", h1_ap[rows, :], g.h1[:], "gh1", reads=[g.h1A, g.h1B], writes=[h1_db[t]])
        emit_norm_T(cx, g.h1, g.gain2, g.hn2, ps[0], g.hn2T, g.ssq, g.rstd, g.junk, xdeps=[g.h1A, g.h1B])
        s.dma("sp", hnT_ap.rearrange("(c p) t -> p c t", p=128)[:, :, rows], g.hn2T[:], "ghn2T",
              reads=[g.hn2T], writes=[hnT_db[t]])
    else:
        s.dma("sp", Sb_ap[t], g.Sbf[1][:].rearrange("p h v -> p (h v)"), "gSbf1", reads=g.Sbf_h[1], writes=[Sb_db[t]])
    if _GSTOP == 8 and full:
        return
    for d in upd_dirs:
        for h in range(NH):
            ub = ps[4 + h // 2] if full else ps[6 + h // 2]
            uc = slice((h % 2) * 256, (h % 2) * 256 + 256)
            s.op("pe", lambda e, d=d, h=h, ub=ub, uc=uc: e.matmul(
                ub[:, uc], lhsT=g.kd[d][:, h * 128:(h + 1) * 128], rhs=g.vbf[:, h * 256:(h + 1) * 256],
                start=True, stop=True), reads=[g.kd[d], g.vbf], writes=[ub])
            s.op("dve", lambda e, d=d, h=h, ub=ub, uc=uc: e.scalar_tensor_tensor(
                out=g.S[d][h][:], in0=g.S[d][h][:], scalar=g.dec[d][:, h:h + 1], in1=ub[:, uc],
                op0=ALU.mult, op1=ALU.add), reads=[g.S[d][h], g.dec[d], ub], writes=[g.S[d][h]])
            if (d == 0) or (not full):
                s.op("pool", lambda e, d=d, h=h: e.tensor_copy(out=g.Sbf[d][:, h, :], in_=g.S[d][h][:]),
                     reads=[g.S[d][h]], writes=[g.Sbf_h[d][h]])


NQH, NKV, HD = 16, 4, 64
_PH = int(_os.environ.get('PH', '5'))
NEG = -1.0e30


def alloc_swa(cx, P, NT):
    s = cx.s
    a = cx.a = Ctx()
    a.W = s.sbuf("aW", [128, DC, 1536], BF16)
    a.W_c = [sub(a.W, "aW_%d" % c) for c in range(DC)]
    for c in range(DC):
        s.dma("pool", a.W.t[:, c, :], P["swa_qkv"][c * 128:(c + 1) * 128, :], a.W_c[c].name, writes=[a.W_c[c]])
    a.Wo = s.sbuf("aWo", [128, DC, D], BF16)
    a.Wo_c = [sub(a.Wo, "aWo_%d" % c) for c in range(DC)]
    for c in range(DC):
        s.dma("pool", a.Wo.t[:, c, :], P["swa_out"][c * 128:(c + 1) * 128, :], a.Wo_c[c].name, writes=[a.Wo_c[c]])
    a.Wr = s.sbuf("aWr", [128, DC, NE], BF16)
    s.dma("pool", a.Wr[:], P["router"].rearrange("(c p) e -> p c e", p=128), "aWr", writes=[a.Wr])
    a.brow = s.sbuf("abrow", [1, 1536], F32)
    s.dma("sp", a.brow[:], P["swa_qkv_b"].rearrange("(o n) -> o n", o=1), "abrow", writes=[a.brow])
    a.ones = s.sbuf("aones", [1, 128], F32)
    s.op("pool", lambda e: e.memset(a.ones[:], 1.0), writes=[a.ones])
    a.bout = s.sbuf("about", [128, D], F32)
    s.dma("sp", a.bout[:], bcast_row(P["swa_out_b"]), "about", writes=[a.bout])
    a.sink = s.sbuf("asink", [128, NQH], F32)
    s.dma("sp", a.sink[:], bcast_row(P["sinks"]), "asink", writes=[a.sink])
    a.gain3 = s.sbuf("again3", [128, D], F32)
    s.dma("sp", a.gain3[:], bcast_row(P["mix_norm1"]), "again3", writes=[a.gain3])
    a.gain4 = s.sbuf("again4", [128, D], F32)
    s.dma("sp", a.gain4[:], bcast_row(P["ffn_norm1"]), "again4", writes=[a.gain4])
    a.dist = s.sbuf("adist", [128, 384], F32)
    a.disti = s.sbuf("adisti", [128, 384], I32)
    s.op("pool", lambda e: e.iota(a.disti[:], pattern=[[-1, 384]], base=128, channel_multiplier=1),
         writes=[a.disti])
    s.op("dve", lambda e: e.tensor_copy(out=a.dist[:], in_=a.disti[:]), reads=[a.disti], writes=[a.dist])
    a.ndist = s.sbuf("andist", [128, 384], F32)
    s.op("dve", lambda e: e.tensor_scalar(out=a.ndist[:], in0=a.dist[:], scalar1=-1.0, scalar2=None, op0=ALU.mult),
         reads=[a.dist], writes=[a.ndist])
    s.op("dve", lambda e: e.tensor_tensor(out=a.dist[:], in0=a.dist[:], in1=a.ndist[:], op=ALU.max),
         reads=[a.dist, a.ndist], writes=[a.dist])
    a.wmask = s.sbuf("awmask", [128, 384], F32)
    s.op("dve", lambda e: e.tensor_scalar(out=a.wmask[:], in0=a.dist[:], scalar1=128.0, scalar2=NEG,
                                          op0=ALU.is_gt, op1=ALU.mult), reads=[a.dist], writes=[a.wmask])
    a.bias = []
    for h in range(NQH):
        b = s.sbuf("abias%d" % h, [128, 384], F32)
        slope = float(np.float32(2.0 ** (-8.0 * (h + 1) / NQH)))
        s.op("dve", lambda e, b=b, slope=slope: e.scalar_tensor_tensor(
            out=b[:], in0=a.dist[:], scalar=-slope, in1=a.wmask[:], op0=ALU.mult, op1=ALU.add),
            reads=[a.dist, a.wmask], writes=[b])
        a.bias.append(b)
    a.x = [s.sbuf("ax%d" % i, [128, D], F32) for i in range(2)]
    a.qT = [s.sbuf("aqT%d" % i, [64, NQH, 128], BF16) for i in range(2)]
    a.kT = [s.sbuf("akT%d" % i, [64, NKV, 128], BF16) for i in range(4)]
    a.v = [s.sbuf("av%d" % i, [128, NKV * HD], BF16) for i in range(4)]
    a.hn = s.sbuf("ahn", [128, D], BF16)
    a.hnT = s.sbuf("ahnT", [128, DC, 128], BF16)
    a.junk = s.sbuf("ajunk", [128, D], F32)
    a.ssq = s.sbuf("assq", [128, 1], F32)
    a.rstd = s.sbuf("arstd", [128, 1], F32)
    a.sc = [s.sbuf("asc%d" % i, [128, 384], F32) for i in range(2)]
    a.p = [s.sbuf("ap%d" % i, [128, 384], BF16) for i in range(2)]
    a.pT = [s.sbuf("apT%d" % i, [128, 384], BF16) for i in range(2)]
    a.st = [s.sbuf("ast%d" % i, [128, 8], F32) for i in range(2)]
    a.sc4 = [s.sbuf("asc4%d" % i, [128, 4, 384], F32) for i in range(2)]
    a.p4 = [s.sbuf("ap4%d" % i, [128, 4, 384], BF16) for i in range(2)]
    a.pT4 = [s.sbuf("apT4%d" % i, [128, 4, 384], BF16) for i in range(2)]
    a.st4 = [s.sbuf("ast4%d" % i, [128, 24], F32) for i in range(2)]
    a.negsink = s.sbuf("anegsink", [128, NQH], F32)
    s.op("dve", lambda e: e.tensor_scalar(out=a.negsink[:], in0=a.sink[:], scalar1=-1.0, scalar2=None, op0=ALU.mult),
         reads=[a.sink], writes=[a.negsink])
    a.attn = s.sbuf("aattn", [128, D], BF16)
    a.attn_h = [sub(a.attn, "aattn_%d" % h) for h in range(NQH)]
    a.aT = s.sbuf("aaT", [128, DC, 128], BF16)
    a.h3 = s.sbuf("ah3", [128, D], F32)
    a.h3A = sub(a.h3, "ah3A")
    a.h3B = sub(a.h3, "ah3B")
    a.hn4 = s.sbuf("ahn4", [128, D], BF16)
    a.hn4T = s.sbuf("ahn4T", [128, DC, 128], BF16)
    a.rt = s.sbuf("art", [128, 8 * 8], F32)


def swa_produce(cx, t, h2_ap, h2_db):
    s = cx.s
    a = cx.a
    ps = cx.ps
    x = a.x[t % 2]
    s.dma("sp", x[:], h2_ap[t * 128:(t + 1) * 128, :], x.name, reads=[h2_db[t]], writes=[x])
    emit_norm_T(cx, x, a.gain3, a.hn, ps[0], a.hnT, a.ssq, a.rstd, a.junk)
    W = a.W.t
    qT = a.qT[t % 2]
    for b4 in range(4):
        bank = ps[1 + (b4 % 2)]
        for hh in range(4):
            h = b4 * 4 + hh
            oc = slice(hh * 128, (hh + 1) * 128)
            for c in range(DC):
                s.op("pe", lambda e, c=c, h=h, oc=oc, bank=bank: e.matmul(
                    bank[0:64, oc], lhsT=W[:, c, h * 64:(h + 1) * 64], rhs=a.hnT[:, c, :],
                    start=(c == 0), stop=False), reads=[a.hnT, a.W_c[c]], writes=[bank])
            s.op("pe", lambda e, h=h, oc=oc, bank=bank: e.matmul(
                bank[0:64, oc], lhsT=a.brow[0:1, h * 64:(h + 1) * 64], rhs=a.ones[0:1, :],
                start=False, stop=True), reads=[a.brow, a.ones], writes=[bank])
        s.op("act", lambda e, b4=b4, bank=bank: e.activation(
            out=qT[:, b4 * 4:(b4 + 1) * 4, :].rearrange("p h t -> p (h t)"), in_=bank[0:64, :], func=AF.Copy,
            scale=0.125), reads=[bank], writes=[qT])
    kT = a.kT[t % 4]
    bank = ps[3]
    for kv in range(NKV):
        oc = slice(kv * 128, (kv + 1) * 128)
        for c in range(DC):
            s.op("pe", lambda e, c=c, kv=kv, oc=oc, bank=bank: e.matmul(
                bank[0:64, oc], lhsT=W[:, c, 1024 + kv * 64:1024 + (kv + 1) * 64], rhs=a.hnT[:, c, :],
                start=(c == 0), stop=False), reads=[a.hnT, a.W_c[c]], writes=[bank])
        s.op("pe", lambda e, kv=kv, oc=oc, bank=bank: e.matmul(
            bank[0:64, oc], lhsT=a.brow[0:1, 1024 + kv * 64:1024 + (kv + 1) * 64], rhs=a.ones[0:1, :],
            start=False, stop=True), reads=[a.brow, a.ones], writes=[bank])
    s.op("dve", lambda e, bank=bank: e.tensor_copy(out=kT[:].rearrange("p h t -> p (h t)"), in_=bank[0:64, :]),
         reads=[bank], writes=[kT])
    v = a.v[t % 4]
    bank = ps[4]
    for c in range(DC):
        s.op("pe", lambda e, c=c, bank=bank: e.matmul(bank[:, 0:256], lhsT=a.hnT[:, c, :], rhs=W[:, c, 1280:1536],
                                           start=(c == 0), stop=False), reads=[a.hnT, a.W_c[c]], writes=[bank])
    s.op("pe", lambda e, bank=bank: e.matmul(bank[:, 0:256], lhsT=a.ones[0:1, :], rhs=a.brow[0:1, 1280:1536],
                                  start=False, stop=True), reads=[a.brow, a.ones], writes=[bank])
    s.op("act", lambda e, bank=bank: e.copy(out=v[:], in_=bank[:, 0:256]), reads=[bank], writes=[v])


def swa_attend(cx, t, NT, h3_ap, h3_db, hnT_ap, hnT_db, cw_all, hn4tm_ap=None):
    s = cx.s
    a = cx.a
    ps = cx.ps
    x = a.x[t % 2]
    qT = a.qT[t % 2]
    kts = [kt for kt in (t - 1, t, t + 1) if 0 <= kt < NT]
    c0 = (kts[0] - (t - 1)) * 128
    c1 = (kts[-1] - (t - 1) + 1) * 128
    for g4 in range(NKV):
        kv = g4
        j = g4 % 2
        p, pT, st = a.p4[j], a.pT4[j], a.st4[j]
        for hh in range(4):
            h = g4 * 4 + hh
            sb_ = ps[4 + hh]
            for i, kt in enumerate(kts):
                cc = (kt - (t - 1)) * 128
                s.op("pe", lambda e, h=h, kt=kt, cc=cc, sb_=sb_, kv=kv: e.matmul(
                    sb_[:, cc:cc + 128], lhsT=qT[:, h, :], rhs=a.kT[kt % 4][:, kv, :], start=True, stop=True),
                    reads=[qT, a.kT[kt % 4]], writes=[sb_])
        sc = a.sc4[j]
        for hh in range(4):
            h = g4 * 4 + hh
            s.op("dve", lambda e, h=h, hh=hh, sc=sc: e.tensor_tensor(
                out=sc[:, hh, c0:c1], in0=ps[4 + hh][:, c0:c1], in1=a.bias[h][:, c0:c1], op=ALU.add),
                reads=[ps[4 + hh], a.bias[h], sc], writes=[sc])
        for hh in range(4):
            s.op("dve", lambda e, hh=hh, st=st, sc=sc: e.reduce_max(out=st[:, hh:hh + 1], in_=sc[:, hh, c0:c1], axis=AX.X),
                 reads=[sc, st], writes=[st])
        s.op("dve", lambda e, st=st, g4=g4: e.scalar_tensor_tensor(
            out=st[:, 4:8], in0=st[:, 0:4], scalar=-1.0, in1=a.negsink[:, g4 * 4:(g4 + 1) * 4], op0=ALU.mult, op1=ALU.min),
            reads=[st, a.negsink], writes=[st])
        for hh in range(4):
            s.op("act", lambda e, hh=hh, p=p, st=st, sc=sc: e.activation(
                out=p[:, hh, c0:c1], in_=sc[:, hh, c0:c1], func=AF.Exp, bias=st[:, 4 + hh:5 + hh],
                accum_out=st[:, 8 + hh:9 + hh]), reads=[sc, st], writes=[p, st])
        s.op("dve", lambda e, st=st, g4=g4: e.tensor_tensor(out=st[:, 12:16], in0=a.sink[:, g4 * 4:(g4 + 1) * 4],
                                                            in1=st[:, 4:8], op=ALU.add), reads=[st, a.sink], writes=[st])
        s.op("act", lambda e, st=st: e.activation(out=st[:, 12:16], in_=st[:, 12:16], func=AF.Exp), reads=[st], writes=[st])
        s.op("dve", lambda e, st=st: e.tensor_tensor(out=st[:, 16:20], in0=st[:, 8:12], in1=st[:, 12:16], op=ALU.add),
             reads=[st], writes=[st])
        s.op("dve", lambda e, st=st: e.reciprocal(out=st[:, 20:24], in_=st[:, 16:20]), reads=[st], writes=[st])
        for half, tb in ((0, ps[0]), (1, ps[3])):
            tv = Bview(tb)
            for h2 in range(2):
                hh = half * 2 + h2
                for kt in kts:
                    cc = (kt - (t - 1)) * 128
                    s.op("pe", lambda e, cc=cc, hh=hh, h2=h2, p=p, tv=tv: e.transpose(
                        out=tv[:, h2 * 384 + cc:h2 * 384 + cc + 128], in_=p[:, hh, cc:cc + 128], identity=cx.ident[:]),
                        reads=[p, cx.ident], writes=[tb])
            full = (c0 == 0 and c1 == 384)
            segs = [(0, 768, None)] if full else [(h2 * 384 + c0, h2 * 384 + c1, h2) for h2 in range(2)]
            for (x0, x1, h2) in segs:
                if h2 is None:
                    dst = pT[:, half * 2:half * 2 + 2, :].rearrange("p h c -> p (h c)")
                else:
                    dst = pT[:, half * 2 + h2, c0:c1]
                if half == 0:
                    s.op("act", lambda e, dst=dst, tv=tv, x0=x0, x1=x1: e.copy(out=dst, in_=tv[:, x0:x1]),
                         reads=[tb], writes=[pT])
                else:
                    s.op("dve", lambda e, dst=dst, tv=tv, x0=x0, x1=x1: e.tensor_copy(out=dst, in_=tv[:, x0:x1]),
                         reads=[tb, pT], writes=[pT])
        for hh in range(4):
            h = g4 * 4 + hh
            ob = ps[1 + h // 8]
            oc = slice((h % 8) * 64, (h % 8) * 64 + 64)
            for i, kt in enumerate(kts):
                cc = (kt - (t - 1)) * 128
                s.op("pe", lambda e, kt=kt, cc=cc, kv=kv, i=i, ob=ob, oc=oc, pT=pT, hh=hh: e.matmul(
                    ob[:, oc], lhsT=pT[:, hh, cc:cc + 128], rhs=a.v[kt % 4][:, kv * 64:(kv + 1) * 64],
                    start=(i == 0), stop=(i == len(kts) - 1)), reads=[pT, a.v[kt % 4]], writes=[ob])
        for hh in range(4):
            h = g4 * 4 + hh
            ob = ps[1 + h // 8]
            oc = slice((h % 8) * 64, (h % 8) * 64 + 64)
            s.op("dve", lambda e, h=h, hh=hh, ob=ob, oc=oc, st=st: e.tensor_scalar(
                out=a.attn[:, h * 64:(h + 1) * 64], in0=ob[:, oc], scalar1=st[:, 20 + hh:21 + hh], scalar2=None,
                op0=ALU.mult), reads=[ob, st], writes=[a.attn_h[h]])
    tb = ps[0]
    tv = Bview(tb)
    for c in range(DC):
        s.op("pe", lambda e, c=c, tv=tv: e.transpose(out=tv[:, c * 128:(c + 1) * 128], in_=a.attn[:, c * 128:(c + 1) * 128],
                                              identity=cx.ident[:]), reads=a.attn_h + [cx.ident], writes=[tb])
    s.op("act", lambda e, tv=tv: e.copy(out=a.aT[:].rearrange("p c t -> p (c t)"), in_=tv[:]), reads=[tb], writes=[a.aT])
    for hh in range(2):
        yb = ps[3 + hh]
        for c in range(DC):
            s.op("pe", lambda e, c=c, hh=hh, yb=yb: e.matmul(
                yb[:], lhsT=a.aT[:, c, :], rhs=a.Wo.t[:, c, hh * 512:(hh + 1) * 512],
                start=(c == 0), stop=(c == DC - 1)), reads=[a.aT, a.Wo_c[c]], writes=[yb])
    for hh, hb in ((0, a.h3A), (1, a.h3B)):
        sl = slice(hh * 512, (hh + 1) * 512)
        s.op("dve", lambda e, hh=hh, sl=sl: e.tensor_tensor(out=a.h3[:, sl], in0=ps[3 + hh][:], in1=x[:, sl], op=ALU.add),
             reads=[ps[3 + hh], x], writes=[hb])
        s.op("pool", lambda e, sl=sl: e.tensor_tensor(out=a.h3[:, sl], in0=a.h3[:, sl], in1=a.bout[:, sl], op=ALU.add),
             reads=[hb, a.bout], writes=[hb])
    s.dma("sp", h3_ap[t * 128:(t + 1) * 128, :], a.h3[:], "ah3", reads=[a.h3A, a.h3B], writes=[h3_db[t]])
    emit_norm_T(cx, a.h3, a.gain4, a.hn4, ps[0], a.hn4T, a.ssq, a.rstd, a.junk, xdeps=[a.h3A, a.h3B])
    s.dma("sp", hnT_ap.rearrange("(c p) t -> p c t", p=128)[:, :, t * 128:(t + 1) * 128], a.hn4T[:], "ahn4T",
          reads=[a.hn4T], writes=[hnT_db[t]])
    if hn4tm_ap is not None:
        s.dma("sp", hn4tm_ap[t * 128:(t + 1) * 128, :], a.hn4[:], "ahn4tm", reads=[a.hn4])
    lb = ps[7]
    for c in range(DC):
        s.op("pe", lambda e, c=c: e.matmul(lb[:, 0:NE], lhsT=a.hn4T[:, c, :], rhs=a.Wr[:, c, :],
                                           start=(c == 0), stop=(c == DC - 1)), reads=[a.hn4T, a.Wr], writes=[lb])
    r = a.rt
    R = lambda i: r[:, i * 8:(i + 1) * 8]
    cwt = cw_all[:, t * NE:(t + 1) * NE]

    def dv(fn, rd=(), wr=()):
        s.op("dve", fn, reads=[a.rt] + list(rd), writes=[a.rt] + list(wr))
    dv(lambda e: e.tensor_copy(out=R(0), in_=lb[:, 0:NE]), rd=[lb])
    dv(lambda e: e.reduce_max(out=r[:, 56:57], in_=R(0), axis=AX.X))
    dv(lambda e: e.tensor_scalar(out=R(1), in0=R(0), scalar1=r[:, 56:57], scalar2=None, op0=ALU.is_equal))
    dv(lambda e: e.scalar_tensor_tensor(out=R(2), in0=R(1), scalar=NEG, in1=R(0), op0=ALU.mult, op1=ALU.add))
    dv(lambda e: e.reduce_max(out=r[:, 57:58], in_=R(2), axis=AX.X))
    dv(lambda e: e.tensor_scalar(out=R(3), in0=R(2), scalar1=r[:, 57:58], scalar2=None, op0=ALU.is_equal))
    dv(lambda e: e.tensor_tensor(out=r[:, 58:59], in0=r[:, 57:58], in1=r[:, 56:57], op=ALU.subtract))
    s.op("act", lambda e: e.activation(out=r[:, 59:60], in_=r[:, 58:59], func=AF.Exp), reads=[a.rt], writes=[a.rt])
    dv(lambda e: e.tensor_scalar(out=r[:, 60:61], in0=r[:, 59:60], scalar1=1.0, scalar2=None, op0=ALU.add))
    dv(lambda e: e.reciprocal(out=r[:, 61:62], in_=r[:, 60:61]))
    dv(lambda e: e.tensor_tensor(out=r[:, 62:63], in0=r[:, 59:60], in1=r[:, 61:62], op=ALU.mult))
    dv(lambda e: e.tensor_scalar(out=R(4), in0=R(1), scalar1=r[:, 61:62], scalar2=None, op0=ALU.mult))
    dv(lambda e: e.scalar_tensor_tensor(out=cwt, in0=R(3), scalar=r[:, 62:63], in1=R(4), op0=ALU.mult, op1=ALU.add),
       wr=[cw_all])


def new_phase(cx, nc):
    cx.s = Sched(nc)
    for b in cx.persist:
        b.lw = None
        b.rd = {}
        b.rd_dma = []
    return cx.s


def build_program(S, TB=256, sparse=True):
    NT = S // 128
    nc = bass.Bass("TRN2", target_bir_lowering=False)

    def din(name, shape):
        return nc.dram_tensor(name, list(shape), F32, kind="ExternalInput").ap()

    x = din("x", [S, D])
    P = {
        "mix_norm0": din("mix_norm0", [D]), "mix_norm1": din("mix_norm1", [D]),
        "ffn_norm0": din("ffn_norm0", [D]), "ffn_norm1": din("ffn_norm1", [D]),
        "gla_in": din("gla_in", [D, GW]), "gw_f": din("gw_f", [16, 512]), "gb_f": din("gb_f", [512]),
        "gw_b": din("gw_b", [16, 512]), "gb_b": din("gb_b", [512]), "gla_hn": din("gla_hn", [D]),
        "gla_out": din("gla_out", [D, D]),
        "swa_qkv": din("swa_qkv", [D, 1536]), "swa_qkv_b": din("swa_qkv_b", [1536]), "sinks": din("sinks", [NQH]),
        "swa_out": din("swa_out", [D, D]), "swa_out_b": din("swa_out_b", [D]),
        "dWg": din("dWg", [D, FF]), "dWu": din("dWu", [D, FF]), "dWd": din("dWd", [FF, D]),
        "router": din("router", [D, NE]),
        "mWg": din("mWg", [NE, D, FF]), "mWu": din("mWu", [NE, D, FF]), "mWd": din("mWd", [NE, FF, D]),
        "final_norm": din("final_norm", [D]),
    }
    out = nc.dram_tensor("out", [S, D], F32, kind="ExternalOutput").ap()
    Sb = nc.dram_tensor("scr_Sb", [NT, 128, NH * 256], BF16).ap()
    h1 = nc.dram_tensor("scr_h1", [S, D], F32).ap()
    h3 = nc.dram_tensor("scr_h3", [S + 128, D], F32).ap()
    hnT = nc.dram_tensor("scr_hnT", [D, S], BF16).ap()
    hnT2 = nc.dram_tensor("scr_hnT2", [D, S], BF16).ap()
    hn4tm = nc.dram_tensor("scr_hn4tm", [S + 128, D], BF16).ap()
    lst = nc.dram_tensor("scr_list", [NE * CAPR + NT * NE * 128, 2], I32).ap()

    cx = Ctx()
    pstack = contextlib.ExitStack()
    Sched.sem_pool = {"sw": [], "hw": [], "eng": []}
    Sched.sem_total = {}
    Sched.sem_stack = pstack
    cx.persist = []
    s = cx.s = Sched(nc)
    keep = s.stack
    s.stack = pstack
    emit_consts(cx)
    emit_psum(cx)
    cw_all = s.sbuf("cw_all", [128, NT * NE], F32)
    flag = s.sbuf("flag", [128, 1], I32)
    s.stack = keep
    regs = {}
    for en, eo in (("pe", nc.tensor), ("act", nc.scalar), ("dve", nc.vector), ("pool", nc.gpsimd), ("sp", nc.sync)):
        regs[en] = pstack.enter_context(eo.register("flagreg_" + en))
    cx.persist = [cx.ident, cx.identf, cx.epsc, cw_all, flag] + cx.ps
    cx.cw_buf = cw_all
    alloc_gla(cx, P)
    Sb_db = [s.dbuf("Sbdb%d" % t) for t in range(NT)]
    h1_db = [s.dbuf("h1db%d" % t) for t in range(NT)]
    hn_db = [s.dbuf("hndb%d" % t) for t in range(NT)]
    for t in reversed(range(NT)):
        emit_gla_tile(cx, t, False, x, Sb, Sb_db, h1, h1_db, hnT, hn_db)
    for t in range(NT):
        emit_gla_tile(cx, t, True, x, Sb, Sb_db, h1, h1_db, hnT, hn_db)
    s.emit()
    if _PH == 1:
        pstack.close()
        return nc
    s = new_phase(cx, nc)
    alloc_ffn_bufs(cx, TB)
    ws = [alloc_wset(cx, 0), alloc_wset(cx, 1)]
    db1 = [s.dbuf("p2a%d" % t) for t in range(NT)]
    db2 = [s.dbuf("p2b%d" % t) for t in range(NT)]
    for f in ffn_weight_loads(cx, ws[0], P["dWg"], P["dWu"], P["dWd"], 0):
        f()
    pre = ffn_weight_loads(cx, ws[1], P["dWg"], P["dWu"], P["dWd"], 1)
    ctr = emit_ffn_pass(cx, S, hnT, db1, ws[0], None, None, h1, db2, pre=pre)
    emit_ffn_pass(cx, S, hnT, db1, ws[1], None, None, h1, db2, ctr=ctr)
    s.emit()
    if _PH == 2:
        pstack.close()
        return nc
    s = new_phase(cx, nc)
    alloc_swa(cx, P, NT)
    d2 = [s.dbuf("p3a%d" % t) for t in range(NT)]
    d3 = [s.dbuf("p3b%d" % t) for t in range(NT)]
    d4 = [s.dbuf("p3c%d" % t) for t in range(NT)]
    swa_produce(cx, 0, h1, d2)
    for t in range(NT):
        if t + 1 < NT:
            swa_produce(cx, t + 1, h1, d2)
        swa_attend(cx, t, NT, h3, d3, hnT2, d4, cw_all, hn4tm_ap=(hn4tm if sparse else None))
    s.emit()
    if _PH == 3:
        pstack.close()
        return nc

    def moe_dense():
        s = cx.s
        db1 = [s.dbuf("p4a%d" % t) for t in range(NT)]
        db2 = [s.dbuf("p4b%d" % t) for t in range(NT)]
        for f in ffn_weight_loads(cx, ws[0], P["mWg"][0], P["mWu"][0], P["mWd"][0], 0):
            f()
        ctr = None
        for he in range(2 * NE):
            e_, half = he // 2, he % 2
            pre = ()
            if he + 1 < 2 * NE:
                e2, h2_ = (he + 1) // 2, (he + 1) % 2
                pre = ffn_weight_loads(cx, ws[(he + 1) % 2], P["mWg"][e2], P["mWu"][e2], P["mWd"][e2], h2_)
            ctr = emit_ffn_pass(cx, S, hnT2, db1, ws[he % 2], cw_all, (lambda t, e_=e_: t * NE + e_), h3, db2,
                                pre=pre, ctr=ctr)
        emit_final_norm(cx, S, h3, out, P["final_norm"], lambda t: [db2[t]])

    def moe_sparse():
        s = cx.s
        h3db = s.dbuf("h3db")
        for f in ffn_weight_loads(cx, ws[0], P["mWg"][0], P["mWu"][0], P["mWd"][0], 0):
            f()
        ctr = None
        for he in range(2 * NE):
            e_, half = he // 2, he % 2
            pre = ()
            if he + 1 < 2 * NE:
                e2, h2_ = (he + 1) // 2, (he + 1) % 2
                pre = ffn_weight_loads(cx, ws[(he + 1) % 2], P["mWg"][e2], P["mWu"][e2], P["mWd"][e2], h2_)
            ctr = emit_moe_sparse_pass(cx, S, e_, ws[he % 2], lst, hn4tm, h3, h3db, pre, ctr)
        emit_final_norm(cx, S, h3, out, P["final_norm"], lambda t: [h3db])

    if not sparse:
        s = new_phase(cx, nc)
        alloc_ffn_bufs(cx, TB)
        ws = [alloc_wset(cx, 0), alloc_wset(cx, 1)]
        cx.fst = [s.sbuf("fst%d" % i, [128, 2], F32) for i in range(3)]
        moe_dense()
        s.emit()
        pstack.close()
        return nc
    s = new_phase(cx, nc)
    emit_route(cx, S, cw_all, lst, flag)
    if _os.environ.get("DBG"):
        dbg = nc.dram_tensor("dbg", [128, NE + 2], F32, kind="ExternalOutput").ap()
        s.dma("sp", dbg[:, 0:NE], cx.r_base[:, NT * NE:NT * NE + NE], "r_dbg", reads=[cx.r_base])
        s.dma("sp", dbg[:, NE:NE + 2], cx.r_fl[:], "r_dbg2", reads=[cx.r_fl])
    zf = s.sbuf("r_zf", [128, D], F32)
    zb = s.sbuf("r_zb", [128, D], BF16)
    s.op("pool", lambda e: e.memset(zf[:], 0.0), writes=[zf])
    s.op("pool", lambda e: e.memset(zb[:], 0.0), writes=[zb])
    s.dma("sp", h3[S:S + 128, :], zf[:], "r_zf", reads=[zf])
    s.dma("sp", hn4tm[S:S + 128, :], zb[:], "r_zb", reads=[zb])
    s.emit()
    sa = new_phase(cx, nc)
    alloc_ffn_bufs(cx, TB)
    ws = [alloc_wset(cx, 0), alloc_wset(cx, 1)]
    g = cx.sp = Ctx()
    g.cnt = 0
    g.tile_idx = {}
    g.tile_gx = {}
    g.idx = [sa.sbuf("sp_idx%d" % i, [128, 2], I32) for i in range(8)]
    g.gx = [sa.sbuf("sp_gx%d" % i, [128, D], BF16) for i in range(6)]
    cx.fst = [sa.sbuf("fst%d" % i, [128, 2], F32) for i in range(3)]
    moe_sparse()
    for b in Buf.registry:
        b.lw = None
        b.rd = {}
        b.rd_dma = []
    sb_ = cx.s = Sched(nc)
    moe_dense()
    emit_either(nc, flag, regs, sa, sb_)
    pstack.close()
    return nc


def emit_final_norm(cx, S, h3, out, gain_ap, h3dep):
    s = cx.s
    NT = S // 128

    def fview(b):
        return b.t[:].rearrange("p a b -> p (a b)").bitcast(F32)[:, 0:D]
    fgB, fjB = cx.hnTb[1], cx.hnTb[0]
    s.dma("sp", fview(fgB), bcast_row(gain_ap), "fgain", writes=[fgB])
    for t in range(NT):
        i = t % 3
        xb, xd = cx.stg[i], [cx.stg[i], cx.stgA[i], cx.stgB[i]]
        ob = cx.hT[t % 2]
        st = cx.fst[i]
        s.dma("sp", xb[:], h3[t * 128:(t + 1) * 128, :], "fx%d" % i, reads=h3dep(t), writes=xd)
        s.op("act", lambda e, xb=xb, st=st: e.activation(out=fview(fjB), in_=xb[:], func=AF.Square, scale=1.0 / 32.0,
                                                         accum_out=st[:, 0:1]), reads=xd, writes=[fjB, st])
        s.op("act", lambda e, st=st: e.activation(out=st[:, 1:2], in_=st[:, 0:1], func=AF.Ln, bias=cx.epsc[:, 0:1]),
             reads=[st, cx.epsc], writes=[st])
        s.op("act", lambda e, st=st: e.activation(out=st[:, 1:2], in_=st[:, 1:2], func=AF.Exp, scale=-0.5),
             reads=[st], writes=[st])
        s.op("dve", lambda e, xb=xb, ob=ob, st=st: e.scalar_tensor_tensor(
            out=fview(ob), in0=xb[:], scalar=st[:, 1:2], in1=fview(fgB), op0=ALU.mult, op1=ALU.mult),
            reads=xd + [st, fgB], writes=[ob])
        s.dma("sp", out[t * 128:(t + 1) * 128, :], fview(ob), "fo%d" % (t % 2), reads=[ob])


PARAM_MAP = [
    ("mix_norm0", "mix_norm", 0), ("mix_norm1", "mix_norm", 1), ("ffn_norm0", "ffn_norm", 0), ("ffn_norm1", "ffn_norm", 1),
    ("gla_in", "gla_in_proj", 0), ("gw_f", "gla_gate_w_fwd", 0), ("gb_f", "gla_gate_b_fwd", 0),
    ("gw_b", "gla_gate_w_bwd", 0), ("gb_b", "gla_gate_b_bwd", 0), ("gla_hn", "gla_head_norm", 0),
    ("gla_out", "gla_out_proj", 0), ("swa_qkv", "swa_qkv_proj", 0), ("swa_qkv_b", "swa_qkv_bias", 0),
    ("sinks", "swa_sinks", 0), ("swa_out", "swa_out_proj", 0), ("swa_out_b", "swa_out_bias", 0),
    ("dWg", "dense_w_gate", 0), ("dWu", "dense_w_up", 0), ("dWd", "dense_w_down", 0), ("router", "moe_router", 0),
    ("mWg", "moe_w_gate", 0), ("mWu", "moe_w_up", 0), ("mWd", "moe_w_down", 0), ("final_norm", "final_norm", None),
]


def make_in_map(inputs, xs):
    m = {"x": np.ascontiguousarray(xs, dtype=np.float32)}
    for dst, src, idx in PARAM_MAP:
        v = np.asarray(inputs[src])
        if idx is not None:
            v = v[idx]
        m[dst] = np.ascontiguousarray(v, dtype=np.float32)
    return m


def kernel(**inputs):
    x = np.asarray(inputs["x"])
    B, S, _ = x.shape
    nc = build_program(S)
    base = make_in_map(inputs, x[0])
    in_maps = []
    for b in range(B):
        m = dict(base)
        m["x"] = np.ascontiguousarray(x[b], dtype=np.float32)
        in_maps.append(m)
    res = run_bass_kernel_spmd(nc, in_maps, core_ids=list(range(B)))
    return np.stack([np.asarray(r["out"]) for r in res.results], axis=0).astype(np.float32)


CAPT = 20
CAPR = CAPT * 128
BIGI = 1.0e6


def emit_route(cx, S, cw_all, list_ap, flag):
    s = cx.s
    NT = S // 128
    NC = NT * NE
    nb = (NC + 511) // 512
    m = s.sbuf("r_m", [128, NC], F32)
    mb = s.sbuf("r_mb", [128, NC], BF16)
    s.op("dve", lambda e: e.tensor_single_scalar(out=m[:], in_=cw_all[:], scalar=0.0, op=ALU.is_gt),
         reads=[cw_all], writes=[m])
    s.op("dve", lambda e: e.tensor_copy(out=mb[:], in_=m[:]), reads=[m], writes=[mb])
    slf = tri(cx, "r_slf", 1.0, 1, -1, ALU.is_gt)
    sl = s.sbuf("r_sl", [128, 128], BF16)
    s.op("dve", lambda e: e.tensor_copy(out=sl[:], in_=slf[:]), reads=[slf], writes=[sl])
    on = s.sbuf("r_on", [128, 128], BF16)
    s.op("pool", lambda e: e.memset(on[:], 1.0), writes=[on])
    within = s.sbuf("r_within", [128, NC], F32)
    tot = s.sbuf("r_tot", [128, NC], F32)
    for k in range(nb):
        cs = slice(k * 512, min(NC, (k + 1) * 512))
        n = cs.stop - cs.start
        s.op("pe", lambda e, cs=cs, n=n: e.matmul(cx.ps[0][:, 0:n], lhsT=sl[:], rhs=mb[:, cs], start=True, stop=True),
             reads=[sl, mb], writes=[cx.ps[0]])
        s.op("pe", lambda e, cs=cs, n=n: e.matmul(cx.ps[1][:, 0:n], lhsT=on[:], rhs=mb[:, cs], start=True, stop=True),
             reads=[on, mb], writes=[cx.ps[1]])
        s.op("act", lambda e, cs=cs, n=n: e.copy(out=within[:, cs], in_=cx.ps[0][:, 0:n]),
             reads=[cx.ps[0], within], writes=[within])
        s.op("dve", lambda e, cs=cs, n=n: e.tensor_copy(out=tot[:, cs], in_=cx.ps[1][:, 0:n]),
             reads=[cx.ps[1], tot], writes=[tot])
    base = s.sbuf("r_base", [128, NC + NE], F32)
    s.op("pool", lambda e: e.memset(base[:, 0:NE], 0.0), writes=[base])
    for t in range(NT):
        s.op("dve", lambda e, t=t: e.tensor_tensor(out=base[:, (t + 1) * NE:(t + 2) * NE], in0=base[:, t * NE:(t + 1) * NE],
                                                   in1=tot[:, t * NE:(t + 1) * NE], op=ALU.add),
             reads=[base, tot], writes=[base])
    fl = cx.r_fl = s.sbuf("r_fl", [128, 2], F32)
    cx.r_base = base
    s.op("dve", lambda e: e.reduce_max(out=fl[:, 0:1], in_=base[:, NC:NC + NE], axis=AX.X), reads=[base], writes=[fl])
    s.op("dve", lambda e: e.tensor_single_scalar(out=fl[:, 1:2], in_=fl[:, 0:1], scalar=(-1.0 if _os.environ.get('FORCEDENSE') else float(CAPR) + 0.5), op=ALU.is_lt),
         reads=[fl], writes=[fl])
    s.op("dve", lambda e: e.tensor_copy(out=flag[:], in_=fl[:, 1:2]), reads=[fl], writes=[flag])
    offi = s.sbuf("r_offi", [128, NC], I32)
    s.op("pool", lambda e: e.iota(offi[:].rearrange("p (t e) -> p t e", e=NE), pattern=[[0, NT], [CAPR, NE]], base=0,
                                  channel_multiplier=0), writes=[offi])
    dest = s.sbuf("r_dest", [128, NC], F32)
    s.op("dve", lambda e: e.tensor_copy(out=dest[:], in_=offi[:]), reads=[offi], writes=[dest])
    s.op("dve", lambda e: e.tensor_tensor(out=dest[:], in0=dest[:], in1=base[:, 0:NC], op=ALU.add),
         reads=[dest, base], writes=[dest])
    s.op("dve", lambda e: e.tensor_tensor(out=dest[:], in0=dest[:], in1=within[:], op=ALU.add),
         reads=[dest, within], writes=[dest])
    s.op("dve", lambda e: e.tensor_tensor(out=dest[:], in0=dest[:], in1=m[:], op=ALU.mult),
         reads=[dest, m], writes=[dest])
    nrow = NE * CAPR
    dumpi = s.sbuf("r_dumpi", [128, NC], I32)
    s.op("pool", lambda e: e.iota(dumpi[:], pattern=[[128, NC]], base=nrow, channel_multiplier=1), writes=[dumpi])
    tmp = s.sbuf("r_tmp", [128, NC], F32)
    s.op("dve", lambda e: e.tensor_copy(out=tmp[:], in_=dumpi[:]), reads=[dumpi], writes=[tmp])
    om = s.sbuf("r_om", [128, NC], F32)
    s.op("dve", lambda e: e.tensor_scalar(out=om[:], in0=m[:], scalar1=-1.0, scalar2=1.0, op0=ALU.mult, op1=ALU.add),
         reads=[m], writes=[om])
    s.op("dve", lambda e: e.tensor_tensor(out=tmp[:], in0=tmp[:], in1=om[:], op=ALU.mult),
         reads=[tmp, om], writes=[tmp])
    s.op("dve", lambda e: e.tensor_tensor(out=dest[:], in0=dest[:], in1=tmp[:], op=ALU.add),
         reads=[dest, tmp], writes=[dest])
    desti = s.sbuf("r_desti", [128, NC], I32)
    s.op("dve", lambda e: e.tensor_copy(out=desti[:], in_=dest[:]), reads=[dest], writes=[desti])
    src = s.sbuf("r_src", [128, NC, 2], I32)
    srcA = sub(src, "r_srcA")
    s.op("pool", lambda e: e.iota(src[:, :, 0].rearrange("p (t e) -> p t e", e=NE), pattern=[[128, NT], [0, NE]], base=0,
                                  channel_multiplier=1), writes=[src])
    s.op("dve", lambda e: e.tensor_copy(out=src[:, :, 1], in_=cw_all[:].bitcast(I32)), reads=[cw_all], writes=[srcA])
    K = nrow // 128
    ini = s.sbuf("r_ini", [128, K, 2], I32)
    iniA = sub(ini, "r_iniA")
    s.op("pool", lambda e: e.iota(ini[:, :, 0], pattern=[[1, K]], base=0, channel_multiplier=K), writes=[ini])
    s.op("dve", lambda e: e.tensor_single_scalar(out=ini[:, :, 0], in_=ini[:, :, 0], scalar=127, op=ALU.bitwise_and),
         reads=[ini], writes=[ini])
    s.op("dve", lambda e: e.tensor_single_scalar(out=ini[:, :, 0], in_=ini[:, :, 0], scalar=S, op=ALU.add),
         reads=[ini], writes=[ini])
    s.op("pool", lambda e: e.memset(ini[:, :, 1], 0), writes=[iniA])
    ldb = s.dbuf("r_listdb")
    s.dma("sp", list_ap[0:nrow, :].rearrange("(p k) c -> p k c", p=128), ini[:], "r_ini", reads=[ini, iniA], writes=[ldb])
    for col in range(NC):
        s.op("pool", lambda e, col=col: e.indirect_dma_start(
            out=list_ap[:, :], out_offset=bass.IndirectOffsetOnAxis(ap=desti[:, col:col + 1], axis=0),
            in_=src[:, col, :], in_offset=None),
            reads=[ldb, desti, src, srcA], writes=[], dma_key="r_scat")


def emit_moe_sparse_pass(cx, S, e_, w, list_ap, hn4tm_ap, h3_ap, h3db, pre, ctr):
    s = cx.s
    TB = cx.TB
    ntt = TB // 128
    g = cx.sp

    def prefetch(b):
        for i in range(ntt):
            j = b * ntt + i
            q = g.cnt
            g.cnt += 1
            idx = g.idx[q % 8]
            gx = g.gx[q % 6]
            g.tile_idx[j] = idx
            g.tile_gx[j] = (gx, q)
            r0 = e_ * CAPR + j * 128
            s.dma("sp", idx[:], list_ap[r0:r0 + 128, :], idx.name, writes=[idx])
            s.op("pool", lambda e, idx=idx, gx=gx: e.indirect_dma_start(
                out=gx[:, :], out_offset=None, in_=hn4tm_ap[:, :],
                in_offset=bass.IndirectOffsetOnAxis(ap=idx[:, 0:1], axis=0)),
                reads=[idx], writes=[gx], dma_key=gx.name)

    def loader(b, hb):
        for i in range(ntt):
            j = b * ntt + i
            gx, q = g.tile_gx[j]
            bank = cx.ps[4 + (q % 2) * 2]
            tv = Bview(bank)
            for c in range(DC):
                s.op("pe", lambda e, c=c, gx=gx, tv=tv: e.transpose(out=tv[:, c * 128:(c + 1) * 128],
                                                                    in_=gx[:, c * 128:(c + 1) * 128], identity=cx.ident[:]),
                     reads=[gx, cx.ident], writes=[bank])
            s.op("act", lambda e, i=i, tv=tv, hb=hb: e.copy(
                out=hb[:, :, i * 128:(i + 1) * 128], in_=tv[:].rearrange("p (c t) -> p c t", c=DC)),
                reads=[bank], writes=[hb])

    def sinker(tile, pd, stg, sa, sb_):
        idx = g.tile_idx[tile]
        cwa = idx[:, 1:2].bitcast(F32)
        s.op("act", lambda e, stg=stg, pd=pd, cwa=cwa: e.activation(
            out=stg[:, 0:512], in_=pd[0][:], func=AF.Copy, scale=cwa), reads=[pd[0], idx], writes=[sa])
        s.op("dve", lambda e, stg=stg, pd=pd, cwa=cwa: e.tensor_scalar(
            out=stg[:, 512:1024], in0=pd[1][:], scalar1=cwa, scalar2=None, op0=ALU.mult),
            reads=[pd[1], idx], writes=[sb_])
        s.op("pool", lambda e, stg=stg, idx=idx: e.indirect_dma_start(
            out=h3_ap[:, :], out_offset=bass.IndirectOffsetOnAxis(ap=idx[:, 0:1], axis=0), in_=stg[:, :],
            in_offset=None, compute_op=ALU.add),
            reads=[sa, sb_, idx, h3db], writes=[h3db], dma_key=stg.name)

    return emit_ffn_pass(cx, CAPR, None, None, w, None, None, None, None, pre=pre, ctr=ctr,
                         loader=loader, sinker=sinker, prefetch=prefetch)
```

```python
import contextlib
import numpy as np
import concourse.bass as bass
import concourse.mybir as mybir
from concourse.bass_utils import run_bass_kernel_spmd

F32 = mybir.dt.float32
BF16 = mybir.dt.bfloat16
I32 = mybir.dt.int32
ALU = mybir.AluOpType
AF = mybir.ActivationFunctionType
AX = mybir.AxisListType

D = 1024
DC = 8
FF = 2816
FH = 1408
NFB = 11
NE = 8
EPS = 1e-5


class Buf:
    __slots__ = ("t", "name", "lw", "rd", "rd_dma", "excl")

    registry = []

    def __init__(self, t, name):
        Buf.registry.append(self)
        self.t = t
        self.name = name
        self.excl = False
        self.lw = None
        self.rd = {}
        self.rd_dma = []

    def __getitem__(self, idx):
        return self.t[idx]


class Op:
    __slots__ = ("eng", "fn", "deps", "needed", "val", "dma_key", "idx")

    def __init__(self, eng, fn, dma_key):
        self.eng = eng
        self.fn = fn
        self.deps = []
        self.needed = False
        self.val = None
        self.dma_key = dma_key
        self.idx = None


class Sched:
    ENGS = ("pe", "act", "dve", "pool", "sp")

    _uid = 0

    def __init__(self, nc):
        Sched._uid += 1
        self.uid = Sched._uid
        self.nc = nc
        self.ops = {e: [] for e in self.ENGS}
        self.stack = contextlib.ExitStack()
        self.sems = {}
        self.dma_count = {}
        self.dma_ops = []
        self.sem_used = {}
        self.slot = {}
        self.base = {}

    sem_pool = {"sw": [], "hw": [], "eng": []}
    sem_stack = None
    sem_total = {}

    def sem(self, key, cls="eng"):
        if key not in self.sems:
            i = self.sem_used.get(cls, 0)
            self.sem_used[cls] = i + 1
            pool = Sched.sem_pool[cls]
            if i >= len(pool):
                pool.append(Sched.sem_stack.enter_context(self.nc.semaphore("sem_%s_%d" % (cls, i))))
            self.sems[key] = pool[i]
            self.slot[key] = (cls, i)
            self.base[key] = Sched.sem_total.get((cls, i), 0)
        return self.sems[key]

    def sbuf(self, name, shape, dtype):
        t = self.stack.enter_context(self.nc.sbuf_tensor("%s_u%d" % (name, self.uid), list(shape), dtype))
        return Buf(t, name)

    def psum(self, name, shape, dtype):
        t = self.stack.enter_context(self.nc.psum_tensor("%s_u%d" % (name, self.uid), list(shape), dtype))
        b = Buf(t, name)
        b.excl = True
        return b

    def dbuf(self, name):
        return Buf(None, name)

    def op(self, eng, fn, reads=(), writes=(), dma_key=None):
        o = Op(eng, fn, dma_key)
        deps = set()
        for b in reads:
            if b.lw is not None:
                deps.add(b.lw)
            if b.excl:
                for en, r in b.rd.items():
                    if en != eng:
                        deps.add(r)
        for b in writes:
            if b.lw is not None:
                deps.add(b.lw)
            for r in b.rd.values():
                deps.add(r)
            for r in b.rd_dma:
                deps.add(r)
        for d in deps:
            if d is o:
                continue
            if d.dma_key is None and d.eng == eng:
                if eng == "pe" or eng == "sp":
                    continue
                if not any((b.lw is d) for b in reads):
                    continue
            d.needed = True
            o.deps.append(d)
        for b in reads:
            if dma_key is not None:
                b.rd_dma.append(o)
            else:
                b.rd[eng] = o
        for b in writes:
            b.lw = o
            b.rd = {}
            b.rd_dma = []
        if dma_key is not None:
            self.sem(dma_key, "sw" if eng == "pool" else "hw")
            self.dma_count[dma_key] = self.dma_count.get(dma_key, 0) + 16
            o.val = self.base[dma_key] + self.dma_count[dma_key]
            o.needed = True
            self.dma_ops.append(o)
        self.ops[eng].append(o)
        return o

    def dma(self, eng, out_ap, in_ap, key, reads=(), writes=(), **kw):
        return self.op(eng, lambda e: e.dma_start(out=out_ap, in_=in_ap, **kw),
                       reads=reads, writes=writes, dma_key=key)

    def prepare(self):
        self.totals = {}
        for e in self.ENGS:
            c = 0
            if any(o.dma_key is None and o.needed for o in self.ops[e]):
                self.sem("E" + e)
                c0 = self.base["E" + e]
                for o in self.ops[e]:
                    if o.dma_key is None and o.needed:
                        c += 1
                        o.val = c0 + c
                self.totals[self.slot["E" + e]] = c0 + c
        for key, n in self.dma_count.items():
            self.totals[self.slot[key]] = self.base[key] + n
        fin = Op("sp", None, None)
        last = {}
        for o in self.dma_ops:
            last[o.dma_key] = o
        fin.deps = list(last.values())
        self.ops["sp"].append(fin)

    def run(self, e, eng):
        waited = {}
        for o in self.ops[e]:
            for d in o.deps:
                key = d.dma_key if d.dma_key is not None else "E" + d.eng
                if waited.get(key, 0) >= d.val:
                    continue
                waited[key] = d.val
                eng.wait_ge(self.sems[key], d.val)
            if o.fn is None:
                continue
            ins = o.fn(eng)
            if o.dma_key is not None:
                ins.then_inc(self.sems[o.dma_key], 16)
            elif o.needed:
                ins.then_inc(self.sems["E" + e], 1)

    def emit(self):
        nc = self.nc
        self.prepare()
        with nc.Block() as block:
            @block.tensor
            def _(eng):
                self.run("pe", eng)

            @block.scalar
            def _(eng):
                self.run("act", eng)

            @block.vector
            def _(eng):
                self.run("dve", eng)

            @block.gpsimd
            def _(eng):
                self.run("pool", eng)

            @block.sync
            def _(eng):
                self.run("sp", eng)
        Sched.sem_total.update(self.totals)
        self.stack.close()


def emit_either(nc, flag, regs, sa, sb):
    sa.prepare()
    sb.prepare()

    def body(e, eng):
        r = regs[e]
        eng.reg_load(r, flag[0:1, 0:1])
        with eng.If_eq(r, 1):
            sa.run(e, eng)
        with eng.Else():
            sb.run(e, eng)

    with nc.Block() as block:
        @block.tensor
        def _(eng):
            body("pe", eng)

        @block.scalar
        def _(eng):
            body("act", eng)

        @block.vector
        def _(eng):
            body("dve", eng)

        @block.gpsimd
        def _(eng):
            body("pool", eng)

        @block.sync
        def _(eng):
            body("sp", eng)
    sa.stack.close()
    sb.stack.close()


class Ctx:
    pass


def bcast_row(ap_row, nparts=128):
    return ap_row.partition_broadcast(nparts)


def emit_consts(cx):
    s = cx.s
    cx.ident = s.sbuf("ident", [128, 128], BF16)
    cx.identf = s.sbuf("identf", [128, 128], F32)
    s.op("pool", lambda e: e.memset(cx.identf[:], 1.0), writes=[cx.identf])
    s.op("pool", lambda e: e.affine_select(out=cx.identf[:], in_=cx.identf[:], pattern=[[-1, 128]],
                                           compare_op=ALU.is_equal, fill=0.0, base=0,
                                           channel_multiplier=1),
         reads=[cx.identf], writes=[cx.identf])
    s.op("dve", lambda e: e.tensor_copy(out=cx.ident[:], in_=cx.identf[:]),
         reads=[cx.identf], writes=[cx.ident])
    cx.epsc = s.sbuf("epsc", [128, 1], F32)
    s.op("pool", lambda e: e.memset(cx.epsc[:], EPS), writes=[cx.epsc])


def emit_norm_T(cx, x, gain_bc, hn, bank, hnT, ssq, rstd, junk, xdeps=None, evac_eng="act"):
    s = cx.s
    xd = [x] if xdeps is None else list(xdeps)
    psT = Bview(bank)
    s.op("act", lambda e: e.activation(out=junk[:], in_=x[:], func=AF.Square, scale=1.0 / 32.0,
                                       accum_out=ssq[:]),
         reads=xd, writes=[junk, ssq])
    s.op("act", lambda e: e.activation(out=rstd[:], in_=ssq[:], func=AF.Ln, bias=cx.epsc[:, 0:1]),
         reads=[ssq, cx.epsc], writes=[rstd])
    s.op("act", lambda e: e.activation(out=rstd[:], in_=rstd[:], func=AF.Exp, scale=-0.5),
         reads=[rstd], writes=[rstd])
    s.op("dve", lambda e: e.scalar_tensor_tensor(out=hn[:], in0=x[:], scalar=rstd[:, 0:1], in1=gain_bc[:],
                                                 op0=ALU.mult, op1=ALU.mult),
         reads=xd + [rstd, gain_bc], writes=[hn])
    for c in range(DC):
        s.op("pe", lambda e, c=c: e.transpose(out=psT[:, c * 128:(c + 1) * 128],
                                              in_=hn[:, c * 128:(c + 1) * 128], identity=cx.ident[:]),
             reads=[hn, cx.ident], writes=[bank])
    if evac_eng == "act":
        s.op("act", lambda e: e.copy(out=hnT[:].rearrange("p c t -> p (c t)"), in_=psT[:]),
             reads=[bank], writes=[hnT])
    else:
        s.op("dve", lambda e: e.tensor_copy(out=hnT[:].rearrange("p c t -> p (c t)"), in_=psT[:]),
             reads=[bank], writes=[hnT])


class Bview:
    def __init__(self, b):
        self.b = b

    def __getitem__(self, idx):
        return self.b.t[:].bitcast(BF16)[idx]


def sub(parent, name):
    return Buf(parent.t, name)


def emit_psum(cx):
    cx.ps = [cx.s.psum("ps%d" % i, [128, 512], F32) for i in range(8)]


class WSet:
    pass


def alloc_wset(cx, k):
    s = cx.s
    w = WSet()
    w.wg = s.sbuf("wg%d" % k, [128, DC, FH], BF16)
    w.wu = s.sbuf("wu%d" % k, [128, DC, FH], BF16)
    w.wd = s.sbuf("wd%d" % k, [128, NFB, D], BF16)
    w.wg_c = [sub(w.wg, "wg%d_%d" % (k, c)) for c in range(DC)]
    w.wu_c = [sub(w.wu, "wu%d_%d" % (k, c)) for c in range(DC)]
    w.wd_c = [sub(w.wd, "wd%d_%d" % (k, c)) for c in range(NFB)]
    return w


def ffn_weight_loads(cx, w, Wg, Wu, Wd, half):
    s = cx.s
    out = []
    f0 = half * FH
    for c in range(DC):
        out.append(lambda c=c: s.dma("pool", w.wg.t[:, c, :], Wg[c * 128:(c + 1) * 128, f0:f0 + FH],
                                     w.wg_c[c].name, writes=[w.wg_c[c]]))
        out.append(lambda c=c: s.dma("pool", w.wu.t[:, c, :], Wu[c * 128:(c + 1) * 128, f0:f0 + FH],
                                     w.wu_c[c].name, writes=[w.wu_c[c]]))
    for fb in range(NFB):
        out.append(lambda fb=fb: s.dma("pool", w.wd.t[:, fb, :], Wd[f0 + fb * 128:f0 + (fb + 1) * 128, :],
                                       w.wd_c[fb].name, writes=[w.wd_c[fb]]))
    return out


def alloc_ffn_bufs(cx, TB):
    s = cx.s
    cx.TB = TB
    cx.hnTb = [s.sbuf("hnTb%d" % i, [128, DC, TB], BF16) for i in range(2)]
    cx.hT = [s.sbuf("hT%d" % i, [128, NFB, TB], BF16) for i in range(2)]
    cx.sg = [s.sbuf("sg%d" % i, [128, TB], BF16) for i in range(2)]
    cx.stg = [s.sbuf("stg%d" % i, [128, D], F32) for i in range(3)]
    cx.stgA = [sub(b, b.name + "A") for b in cx.stg]
    cx.stgB = [sub(b, b.name + "B") for b in cx.stg]


def emit_ffn_pass(cx, S, hnT_ap, hnT_db, w, cw, cw_col, acc_ap, acc_db, pre=(), ctr=None,
                  loader=None, sinker=None, prefetch=None):
    s = cx.s
    TB = cx.TB
    nblk = S // TB
    ntt = TB // 128
    hview = hnT_ap.rearrange("(c p) t -> p c t", p=128) if hnT_ap is not None else None
    pre = list(pre)
    per_blk = (len(pre) + nblk - 1) // nblk if pre else 0
    if ctr is None:
        ctr = {"blk": 0, "tile": 0, "fb": 0}

    def gu(b):
        k = ctr["blk"] + b
        hb = cx.hnTb[k % 2]
        if loader is not None:
            loader(b, hb)
        else:
            s.dma("sp", hb[:], hview[:, :, b * TB:(b + 1) * TB], hb.name,
                  reads=[hnT_db[b * ntt + i] for i in range(ntt)], writes=[hb])
        hT = cx.hT[k % 2]
        for fb in range(NFB):
            q = ctr["fb"]
            ctr["fb"] += 1
            pg = cx.ps[(2 * q) % 4]
            pu = cx.ps[(2 * q + 1) % 4]
            sg = cx.sg[q % 2]
            for c in range(DC):
                s.op("pe", lambda e, c=c, pg=pg, fb=fb: e.matmul(
                    pg[:, :TB], lhsT=w.wg.t[:, c, fb * 128:(fb + 1) * 128], rhs=hb[:, c, :],
                    start=(c == 0), stop=(c == DC - 1)), reads=[w.wg_c[c], hb], writes=[pg])
            for c in range(DC):
                s.op("pe", lambda e, c=c, pu=pu, fb=fb: e.matmul(
                    pu[:, :TB], lhsT=w.wu.t[:, c, fb * 128:(fb + 1) * 128], rhs=hb[:, c, :],
                    start=(c == 0), stop=(c == DC - 1)), reads=[w.wu_c[c], hb], writes=[pu])
            s.op("act", lambda e, pg=pg, sg=sg: e.activation(out=sg[:], in_=pg[:, :TB], func=AF.Silu),
                 reads=[pg], writes=[sg])
            s.op("dve", lambda e, pu=pu, sg=sg, fb=fb: e.tensor_tensor(
                out=hT[:, fb, :], in0=pu[:, :TB], in1=sg[:], op=ALU.mult),
                reads=[pu, sg], writes=[hT])

    def down(b):
        k = ctr["blk"] + b
        hT = cx.hT[k % 2]
        for tt in range(ntt):
            tile = b * ntt + tt
            q = ctr["tile"]
            ctr["tile"] += 1
            pd = (cx.ps[4 + (q % 2) * 2], cx.ps[5 + (q % 2) * 2])
            for half in range(2):
                for fb in range(NFB):
                    s.op("pe", lambda e, half=half, fb=fb, tt=tt, pd=pd: e.matmul(
                        pd[half][:], lhsT=hT[:, fb, tt * 128:(tt + 1) * 128],
                        rhs=w.wd.t[:, fb, half * 512:(half + 1) * 512],
                        start=(fb == 0), stop=(fb == NFB - 1)),
                        reads=[hT, w.wd_c[fb]], writes=[pd[half]])
            j = q % 3
            stg, sa, sb_ = cx.stg[j], cx.stgA[j], cx.stgB[j]
            if sinker is not None:
                sinker(tile, pd, stg, sa, sb_)
                continue
            if cw is None:
                s.op("act", lambda e, stg=stg, pd=pd: e.copy(out=stg[:, 0:512], in_=pd[0][:]),
                     reads=[pd[0]], writes=[sa])
                s.op("dve", lambda e, stg=stg, pd=pd: e.tensor_copy(out=stg[:, 512:1024], in_=pd[1][:]),
                     reads=[pd[1]], writes=[sb_])
            else:
                col = cw_col(tile)
                s.op("act", lambda e, stg=stg, col=col, pd=pd: e.activation(
                    out=stg[:, 0:512], in_=pd[0][:], func=AF.Copy, scale=cw[:, col:col + 1]),
                    reads=[pd[0], cw], writes=[sa])
                s.op("dve", lambda e, stg=stg, col=col, pd=pd: e.tensor_scalar(
                    out=stg[:, 512:1024], in0=pd[1][:], scalar1=cw[:, col:col + 1], scalar2=None,
                    op0=ALU.mult), reads=[pd[1], cw], writes=[sb_])
            s.dma("pool", acc_ap[tile * 128:(tile + 1) * 128, :], stg[:], stg.name,
                  reads=[sa, sb_, acc_db[tile]], writes=[acc_db[tile]], accum_op=ALU.add)

    if prefetch is not None:
        prefetch(0)
        if nblk > 1:
            prefetch(1)
    gu(0)
    for b in range(nblk):
        if prefetch is not None and b + 2 < nblk:
            prefetch(b + 2)
        if b + 1 < nblk:
            gu(b + 1)
        down(b)
        for _ in range(per_blk):
            if pre:
                pre.pop(0)()
    while pre:
        pre.pop(0)()
    ctr["blk"] += nblk
    return ctr


GQ, GK, GV, GR, GLF, GLB = 0, 512, 1024, 2048, 3072, 3088
GW = 3104
NH = 4
import os as _os
_GSTOP = int(_os.environ.get('GSTOP', '-1'))


def tri(cx, name, val, pattern, cm, cmp, dtype=F32, ncols=128):
    s = cx.s
    b = s.sbuf(name, [128, ncols], F32)
    s.op("pool", lambda e: e.memset(b[:], val), writes=[b])
    s.op("pool", lambda e: e.affine_select(out=b[:, 0:128], in_=b[:, 0:128], pattern=[[pattern, 128]],
                                           compare_op=cmp, fill=0.0, base=0, channel_multiplier=cm),
         reads=[b], writes=[b])
    return b


def alloc_gla(cx, P):
    s = cx.s
    g = cx.g = Ctx()
    c16 = -1.0 / 16.0
    g.Rf = s.sbuf("Rf", [128, 257], F32)
    g.Rb = s.sbuf("Rb", [128, 257], F32)
    uf = tri(cx, "Uf", c16, 1, -1, ALU.is_ge)
    ub = tri(cx, "Ub", c16, -1, 1, ALU.is_ge)
    for R, U, ref in ((g.Rf, uf, 64), (g.Rb, ub, 63)):
        s.op("pool", lambda e, R=R: e.memset(R[:, 256:257], c16), writes=[R])
        s.op("dve", lambda e, R=R, U=U: e.tensor_copy(out=R[:, 0:128], in_=U[:]), reads=[U, R], writes=[R])
        s.op("dve", lambda e, R=R, U=U, ref=ref: e.tensor_scalar(
            out=R[:, 128:256], in0=U[:], scalar1=U[:, ref:ref + 1], scalar2=None, op0=ALU.subtract),
            reads=[U, R], writes=[R])
    g.Mkf = tri(cx, "Mkf", c16, -1, 1, ALU.is_gt)
    g.Mkb = tri(cx, "Mkb", c16, 1, -1, ALU.is_gt)
    g.maskf = tri(cx, "maskf", 1.0, 1, -1, ALU.is_ge)
    g.maskb = tri(cx, "maskb", 1.0, -1, 1, ALU.is_gt)
    g.Win = s.sbuf("Win", [128, DC, GW], BF16)
    g.Win_c = [sub(g.Win, "Win_%d" % c) for c in range(DC)]
    for c in range(DC):
        s.dma("pool", g.Win.t[:, c, :], P["gla_in"][c * 128:(c + 1) * 128, :], g.Win_c[c].name,
              writes=[g.Win_c[c]])
    g.Wout = s.sbuf("Wout", [128, DC, D], BF16)
    g.Wout_c = [sub(g.Wout, "Wout_%d" % c) for c in range(DC)]
    for c in range(DC):
        s.dma("pool", g.Wout.t[:, c, :], P["gla_out"][c * 128:(c + 1) * 128, :], g.Wout_c[c].name,
              writes=[g.Wout_c[c]])
    g.wga = []
    for d, (wk, bk) in enumerate((("gw_f", "gb_f"), ("gw_b", "gb_b"))):
        wa = s.sbuf("wga%d" % d, [17, 512], F32)
        wa1 = sub(wa, "wga%d_b" % d)
        s.dma("sp", wa[0:16, :], P[wk], wa.name, writes=[wa])
        s.dma("sp", wa[16:17, :], P[bk].rearrange("(o n) -> o n", o=1), wa1.name, writes=[wa1])
        g.wga.append((wa, wa1))
    g.gain1 = s.sbuf("gain1", [128, D], F32)
    s.dma("sp", g.gain1[:], bcast_row(P["mix_norm0"]), "gain1", writes=[g.gain1])
    g.gain2 = s.sbuf("gain2", [128, D], F32)
    s.dma("sp", g.gain2[:], bcast_row(P["ffn_norm0"]), "gain2", writes=[g.gain2])
    g.hgain = s.sbuf("hgain", [128, D], F32)
    s.dma("sp", g.hgain[:], bcast_row(P["gla_hn"]), "hgain", writes=[g.hgain])
    g.one_c = s.sbuf("one_c", [128, 1], F32)
    s.op("pool", lambda e: e.memset(g.one_c[:], 1.0), writes=[g.one_c])
    g.eps256 = s.sbuf("eps256", [128, 1], F32)
    s.op("pool", lambda e: e.memset(g.eps256[:], EPS), writes=[g.eps256])
    g.x = s.sbuf("gx", [128, D], F32)
    g.hn = s.sbuf("ghn", [128, D], BF16)
    g.hnT = s.sbuf("ghnT", [128, DC, 128], BF16)
    g.junk = s.sbuf("gjunk", [128, D], F32)
    g.ssq = s.sbuf("gssq", [128, 1], F32)
    g.rstd = s.sbuf("grstd", [128, 1], F32)
    g.vbf = s.sbuf("gvbf", [128, D], BF16)
    g.er = s.sbuf("ger", [128, D], F32)
    g.rbf = s.sbuf("grbf", [128, D], BF16)
    g.lrT = []
    for d in range(2):
        b = s.sbuf("glrT%d" % d, [17, 128], F32)
        s.op("pool", lambda e, b=b: e.memset(b[:], 1.0), writes=[b])
        g.lrT.append(b)
    g.la = [s.sbuf("gla%d" % d, [128, 512], F32) for d in range(2)]
    g.Ekd = [s.sbuf("gEkd%d" % d, [128, 512], F32) for d in range(2)]
    g.kd = [s.sbuf("gkd%d" % d, [128, 512], BF16) for d in range(2)]
    g.Eall = [[s.sbuf("gEall%d_%d" % (d, h), [128, 257], F32) for h in range(NH)] for d in range(2)]
    g.E2 = [[s.sbuf("gE2%d_%d" % (d, h), [128, 128], F32) for h in range(NH)] for d in range(2)]
    g.qe = [[s.sbuf("gqe%d_%d" % (d, h), [128, 128], BF16) for h in range(NH)] for d in range(2)]
    g.qb = [[s.sbuf("gqb%d_%d" % (d, h), [128, 128], BF16) for h in range(NH)] for d in range(2)]
    g.ke = [[s.sbuf("gke%d_%d" % (d, h), [128, 128], BF16) for h in range(NH)] for d in range(2)]
    g.PT = [[s.sbuf("gPT%d_%d" % (d, h), [128, 128], BF16) for h in range(NH)] for d in range(2)]
    g.dec = [s.sbuf("gdec%d" % d, [128, NH], F32) for d in range(2)]
    g.S = [[s.sbuf("gS%d_%d" % (d, h), [128, 256], F32) for h in range(NH)] for d in range(2)]
    for d in range(2):
        for h in range(NH):
            s.op("pool", lambda e, b=g.S[d][h]: e.memset(b[:], 0.0), writes=[g.S[d][h]])
    g.Sbf = [s.sbuf("gSbf%d" % d, [128, NH, 256], BF16) for d in range(2)]
    g.Sbf_h = [[sub(g.Sbf[d], "gSbf%d_%d" % (d, h)) for h in range(NH)] for d in range(2)]
    for d in range(2):
        s.op("pool", lambda e, b=g.Sbf[d]: e.memset(b[:], 0.0), writes=[g.Sbf[d]] + g.Sbf_h[d])
    g.ssq4 = s.sbuf("gssq4", [128, NH], F32)
    g.rstd4 = s.sbuf("grstd4", [128, NH], F32)
    g.og = s.sbuf("gog", [128, D], F32)
    g.og_h = [sub(g.og, "gog_%d" % h) for h in range(NH)]
    g.sig = s.sbuf("gsig", [128, D], F32)
    g.gated = s.sbuf("ggated", [128, D], BF16)
    g.gT = s.sbuf("ggT", [128, DC, 128], BF16)
    g.h1 = s.sbuf("gh1", [128, D], F32)
    g.h1A = sub(g.h1, "gh1A")
    g.h1B = sub(g.h1, "gh1B")
    g.hn2 = s.sbuf("ghn2", [128, D], BF16)
    g.hn2T = s.sbuf("ghn2T", [128, DC, 128], BF16)


def emit_gla_tile(cx, t, full, x_ap, Sb_ap, Sb_db, h1_ap, h1_db, hnT_ap, hnT_db):
    s = cx.s
    g = cx.g
    ps = cx.ps
    W = g.Win.t
    rows = slice(t * 128, (t + 1) * 128)
    s.dma("sp", g.x[:], x_ap[rows, :], "gx", writes=[g.x])
    emit_norm_T(cx, g.x, g.gain1, g.hn, ps[0], g.hnT, g.ssq, g.rstd, g.junk)
    hnT = g.hnT
    dirs = (0, 1) if full else (1,)
    upd_dirs = (0,) if full else (1,)

    def proj_tok(bank, col0, n=512):
        for c in range(DC):
            s.op("pe", lambda e, c=c: e.matmul(bank[:, 0:n], lhsT=hnT[:, c, :], rhs=W[:, c, col0:col0 + n],
                                               start=(c == 0), stop=(c == DC - 1)),
                 reads=[hnT, g.Win_c[c]], writes=[bank])

    def proj_feat(bank, bcol, col0, m):
        for c in range(DC):
            s.op("pe", lambda e, c=c: e.matmul(bank[0:m, bcol:bcol + 128], lhsT=W[:, c, col0:col0 + m],
                                               rhs=hnT[:, c, :], start=(c == 0), stop=(c == DC - 1)),
                 reads=[hnT, g.Win_c[c]], writes=[bank])

    proj_tok(ps[1], GK)
    proj_tok(ps[2], GV)
    proj_tok(ps[3], GV + 512)
    if full:
        proj_tok(ps[4], GR)
        proj_tok(ps[5], GR + 512)
        for h in range(NH):
            proj_feat(ps[6], h * 128, GQ + h * 128, 128)
        for h in range(NH):
            proj_feat(ps[7], h * 128, GK + h * 128, 128)
    for d in dirs:
        proj_feat(ps[0], d * 128, GLF + 16 * d, 16)
    if _GSTOP == 0 and full:
        return
    s.op("act", lambda e: e.copy(out=g.vbf[:, 0:512], in_=ps[2][:]), reads=[ps[2]], writes=[g.vbf])
    s.op("act", lambda e: e.copy(out=g.vbf[:, 512:1024], in_=ps[3][:]), reads=[ps[3], g.vbf], writes=[g.vbf])
    if full:
        for hh in range(2):
            sl = slice(hh * 512, (hh + 1) * 512)
            s.op("act", lambda e, hh=hh, sl=sl: e.activation(out=g.er[:, sl], in_=ps[4 + hh][:], func=AF.Exp,
                                                             scale=-1.0),
                 reads=[ps[4 + hh], g.er], writes=[g.er])
            s.op("dve", lambda e, hh=hh, sl=sl: e.tensor_copy(out=g.rbf[:, sl], in_=ps[4 + hh][:]),
                 reads=[ps[4 + hh], g.rbf], writes=[g.rbf])
        s.op("act", lambda e: e.activation(out=g.er[:], in_=g.er[:], func=AF.Ln, bias=g.one_c[:, 0:1]),
             reads=[g.er, g.one_c], writes=[g.er])
        s.op("act", lambda e: e.activation(out=g.sig[:], in_=g.er[:], func=AF.Exp, scale=-1.0),
             reads=[g.er], writes=[g.sig])
        s.op("dve", lambda e: e.tensor_tensor(out=g.sig[:], in0=g.sig[:], in1=g.rbf[:], op=ALU.mult),
             reads=[g.sig, g.rbf], writes=[g.sig])
    for d in dirs:
        s.op("dve", lambda e, d=d: e.tensor_copy(out=g.lrT[d][0:16, :], in_=ps[0][0:16, d * 128:(d + 1) * 128]),
             reads=[ps[0], g.lrT[d]], writes=[g.lrT[d]])
    if _GSTOP == 1 and full:
        return
    for d in dirs:
        zb = ps[2 + d]
        s.op("pe", lambda e, d=d, zb=zb: e.matmul(zb[:], lhsT=g.lrT[d][:], rhs=g.wga[d][0][:], start=True, stop=True),
             reads=[g.lrT[d], g.wga[d][0], g.wga[d][1]], writes=[zb])
        s.op("act", lambda e, d=d, zb=zb: e.activation(out=g.la[d][:], in_=zb[:], func=AF.Exp, scale=-1.0),
             reads=[zb], writes=[g.la[d]])
        s.op("act", lambda e, d=d: e.activation(out=g.la[d][:], in_=g.la[d][:], func=AF.Ln, bias=g.one_c[:, 0:1]),
             reads=[g.la[d], g.one_c], writes=[g.la[d]])
    if _GSTOP == 2 and full:
        return
    for d in upd_dirs:
        Mk = g.Mkf if d == 0 else g.Mkb
        xb = ps[2 + d]
        s.op("pe", lambda e, d=d, Mk=Mk, xb=xb: e.matmul(xb[:], lhsT=Mk[:], rhs=g.la[d][:], start=True, stop=True),
             reads=[Mk, g.la[d]], writes=[xb])
        s.op("act", lambda e, d=d, xb=xb: e.activation(out=g.Ekd[d][:], in_=xb[:], func=AF.Exp),
             reads=[xb], writes=[g.Ekd[d]])
        s.op("dve", lambda e, d=d: e.tensor_tensor(out=g.kd[d][:], in0=ps[1][:], in1=g.Ekd[d][:], op=ALU.mult),
             reads=[ps[1], g.Ekd[d]], writes=[g.kd[d]])
    cnt = 0
    for d in dirs:
        R = g.Rf if d == 0 else g.Rb
        for h in range(NH):
            cb = ps[4 + (cnt % 2)]
            cnt += 1
            if full:
                s.op("pe", lambda e, d=d, h=h, cb=cb, R=R: e.matmul(
                    cb[:, 0:257], lhsT=g.la[d][:, h * 128:(h + 1) * 128], rhs=R[:], start=True, stop=True),
                    reads=[g.la[d], R], writes=[cb])
                s.op("act", lambda e, d=d, h=h, cb=cb: e.activation(out=g.Eall[d][h][:], in_=cb[:, 0:257], func=AF.Exp),
                     reads=[cb], writes=[g.Eall[d][h]])
                s.op("act", lambda e, d=d, h=h, cb=cb: e.activation(out=g.E2[d][h][:], in_=cb[:, 128:256], func=AF.Exp,
                                                                   scale=-1.0),
                     reads=[cb], writes=[g.E2[d][h]])
                s.op("dve", lambda e, d=d, h=h: e.tensor_copy(out=g.dec[d][:, h:h + 1], in_=g.Eall[d][h][:, 256:257]),
                     reads=[g.Eall[d][h], g.dec[d]], writes=[g.dec[d]])
            else:
                s.op("pe", lambda e, d=d, h=h, cb=cb, R=R: e.matmul(
                    cb[:, 0:1], lhsT=g.la[d][:, h * 128:(h + 1) * 128], rhs=R[:, 256:257], start=True, stop=True),
                    reads=[g.la[d], R], writes=[cb])
                s.op("act", lambda e, d=d, h=h, cb=cb: e.activation(out=g.dec[d][:, h:h + 1], in_=cb[:, 0:1], func=AF.Exp),
                     reads=[cb, g.dec[d]], writes=[g.dec[d]])
    if full:
        if _GSTOP == 3 and full:
            return
        sc = float(128 ** -0.5)
        for d in dirs:
            for h in range(NH):
                qps = ps[6][:, h * 128:(h + 1) * 128]
                kps = ps[7][:, h * 128:(h + 1) * 128]
                E = g.Eall[d][h]
                s.op("dve", lambda e, d=d, h=h, qps=qps, E=E: e.scalar_tensor_tensor(
                    out=g.qb[d][h][:], in0=qps, scalar=sc, in1=E[:, 0:128], op0=ALU.mult, op1=ALU.mult),
                    reads=[ps[6], E], writes=[g.qb[d][h]])
                s.op("dve", lambda e, d=d, h=h, qps=qps, E=E: e.scalar_tensor_tensor(
                    out=g.qe[d][h][:], in0=qps, scalar=sc, in1=E[:, 128:256], op0=ALU.mult, op1=ALU.mult),
                    reads=[ps[6], E], writes=[g.qe[d][h]])
                s.op("dve", lambda e, d=d, h=h, kps=kps: e.tensor_tensor(
                    out=g.ke[d][h][:], in0=kps, in1=g.E2[d][h][:], op=ALU.mult),
                    reads=[ps[7], g.E2[d][h]], writes=[g.ke[d][h]])
        s.dma("sp", g.Sbf[1][:].rearrange("p h v -> p (h v)"), Sb_ap[t], "gSbf1", reads=[Sb_db[t]], writes=[g.Sbf[1]] + g.Sbf_h[1])
        if _GSTOP == 4 and full:
            return
        for d in dirs:
            mask = g.maskf if d == 0 else g.maskb
            sb_ = ps[2 + d]
            for h in range(NH):
                s.op("pe", lambda e, d=d, h=h, sb_=sb_: e.matmul(
                    sb_[:, h * 128:(h + 1) * 128], lhsT=g.ke[d][h][:], rhs=g.qe[d][h][:], start=True, stop=True),
                    reads=[g.ke[d][h], g.qe[d][h]], writes=[sb_])
            for h in range(NH):
                s.op("dve", lambda e, d=d, h=h, sb_=sb_, mask=mask: e.tensor_tensor(
                    out=g.PT[d][h][:], in0=sb_[:, h * 128:(h + 1) * 128], in1=mask[:], op=ALU.mult),
                    reads=[sb_, mask], writes=[g.PT[d][h]])
        if _GSTOP == 5 and full:
            return
        for h in range(NH):
            ob = ps[4 + h // 2]
            oc = slice((h % 2) * 256, (h % 2) * 256 + 256)
            vs = g.vbf[:, h * 256:(h + 1) * 256]
            s.op("pe", lambda e, h=h, ob=ob, oc=oc, vs=vs: e.matmul(ob[:, oc], lhsT=g.PT[0][h][:], rhs=vs,
                                                                   start=True, stop=False),
                 reads=[g.PT[0][h], g.vbf], writes=[ob])
            s.op("pe", lambda e, h=h, ob=ob, oc=oc, vs=vs: e.matmul(ob[:, oc], lhsT=g.PT[1][h][:], rhs=vs,
                                                                   start=False, stop=False),
                 reads=[g.PT[1][h], g.vbf], writes=[ob])
            s.op("pe", lambda e, h=h, ob=ob, oc=oc: e.matmul(ob[:, oc], lhsT=g.qb[0][h][:], rhs=g.Sbf[0][:, h, :],
                                                            start=False, stop=False),
                 reads=[g.qb[0][h], g.Sbf_h[0][h]], writes=[ob])
            s.op("pe", lambda e, h=h, ob=ob, oc=oc: e.matmul(ob[:, oc], lhsT=g.qb[1][h][:], rhs=g.Sbf[1][:, h, :],
                                                            start=False, stop=True),
                 reads=[g.qb[1][h], g.Sbf_h[1][h]], writes=[ob])
        if _GSTOP == 6 and full:
            return
        for h in range(NH):
            ob = ps[4 + h // 2]
            oc = slice((h % 2) * 256, (h % 2) * 256 + 256)
            s.op("act", lambda e, h=h, ob=ob, oc=oc: e.activation(
                out=g.junk[:, h * 256:(h + 1) * 256], in_=ob[:, oc], func=AF.Square, scale=1.0 / 16.0,
                accum_out=g.ssq4[:, h:h + 1]), reads=[ob, g.junk, g.ssq4], writes=[g.junk, g.ssq4])
        s.op("act", lambda e: e.activation(out=g.rstd4[:], in_=g.ssq4[:], func=AF.Ln, bias=g.eps256[:, 0:1]),
             reads=[g.ssq4, g.eps256], writes=[g.rstd4])
        s.op("act", lambda e: e.activation(out=g.rstd4[:], in_=g.rstd4[:], func=AF.Exp, scale=-0.5),
             reads=[g.rstd4], writes=[g.rstd4])
        for h in range(NH):
            ob = ps[4 + h // 2]
            oc = slice((h % 2) * 256, (h % 2) * 256 + 256)
            hs = slice(h * 256, (h + 1) * 256)
            s.op("dve", lambda e, h=h, ob=ob, oc=oc, hs=hs: e.scalar_tensor_tensor(
                out=g.og[:, hs], in0=ob[:, oc], scalar=g.rstd4[:, h:h + 1], in1=g.hgain[:, hs],
                op0=ALU.mult, op1=ALU.mult), reads=[ob, g.rstd4, g.hgain], writes=[g.og_h[h]])
        s.op("dve", lambda e: e.tensor_tensor(out=g.gated[:], in0=g.og[:], in1=g.sig[:], op=ALU.mult),
             reads=g.og_h + [g.sig], writes=[g.gated])
        if _GSTOP == 7 and full:
            return
        psT = Bview(ps[0])
        for c in range(DC):
            s.op("pe", lambda e, c=c: e.transpose(out=psT[:, c * 128:(c + 1) * 128],
                                                  in_=g.gated[:, c * 128:(c + 1) * 128], identity=cx.ident[:]),
                 reads=[g.gated, cx.ident], writes=[ps[0]])
        s.op("act", lambda e: e.copy(out=g.gT[:].rearrange("p c t -> p (c t)"), in_=psT[:]),
             reads=[ps[0]], writes=[g.gT])
        for hh in range(2):
            yb = ps[2 + hh]
            for c in range(DC):
                s.op("pe", lambda e, c=c, hh=hh, yb=yb: e.matmul(
                    yb[:], lhsT=g.gT[:, c, :], rhs=g.Wout.t[:, c, hh * 512:(hh + 1) * 512],
                    start=(c == 0), stop=(c == DC - 1)), reads=[g.gT, g.Wout_c[c]], writes=[yb])
        s.op("dve", lambda e: e.tensor_tensor(out=g.h1[:, 0:512], in0=ps[2][:], in1=g.x[:, 0:512], op=ALU.add),
             reads=[ps[2], g.x], writes=[g.h1A])
        s.op("dve", lambda e: e.tensor_tensor(out=g.h1[:, 512:1024], in0=ps[3][:], in1=g.x[:, 512:1024], op=ALU.add),
             reads=[ps[3], g.x], writes=[g.h1B])
        s.dma("sp", h1_ap[rows, :], g.h1[:], "gh1", reads=[g.h1A, g.h1B], writes=[h1_db[t]])
        emit_norm_T(cx, g.h1, g.gain2, g.hn2, ps[0], g.hn2T, g.ssq, g.rstd, g.junk, xdeps=[g.h1A, g.h1B])
        s.dma("sp", hnT_ap.rearrange("(c p) t -> p c t", p=128)[:, :, rows], g.hn2T[:], "ghn2T",
              reads=[g.hn2T], writes=[hnT_db[t]])
    else:
        s.dma("sp", Sb_ap[t], g.Sbf[1][:].rearrange("p h v -> p (h v)"), "gSbf1", reads=g.Sbf_h[1], writes=[Sb_db[t]])
    if _GSTOP == 8 and full:
        return
    for d in upd_dirs:
        for h in range(NH):
            ub = ps[4 + h // 2] if full else ps[6 + h // 2]
            uc = slice((h % 2) * 256, (h % 2) * 256 + 256)
            s.op("pe", lambda e, d=d, h=h, ub=ub, uc=uc: e.matmul(
                ub[:, uc], lhsT=g.kd[d][:, h * 128:(h + 1) * 128], rhs=g.vbf[:, h * 256:(h + 1) * 256],
                start=True, stop=True), reads=[g.kd[d], g.vbf], writes=[ub])
            s.op("dve", lambda e, d=d, h=h, ub=ub, uc=uc: e.scalar_tensor_tensor(
                out=g.S[d][h][:], in0=g.S[d][h][:], scalar=g.dec[d][:, h:h + 1], in1=ub[:, uc],
                op0=ALU.mult, op1=ALU.add), reads=[g.S[d][h], g.dec[d], ub], writes=[g.S[d][h]])
            if (d == 0) or (not full):
                s.op("pool", lambda e, d=d, h=h: e.tensor_copy(out=g.Sbf[d][:, h, :], in_=g.S[d][h][:]),
                     reads=[g.S[d][h]], writes=[g.Sbf_h[d][h]])


NQH, NKV, HD = 16, 4, 64
_PH = int(_os.environ.get('PH', '5'))
NEG = -1.0e30


def alloc_swa(cx, P, NT):
    s = cx.s
    a = cx.a = Ctx()
    a.W = s.sbuf("aW", [128, DC, 1536], BF16)
    a.W_c = [sub(a.W, "aW_%d" % c) for c in range(DC)]
    for c in range(DC):
        s.dma("pool", a.W.t[:, c, :], P["swa_qkv"][c * 128:(c + 1) * 128, :], a.W_c[c].name, writes=[a.W_c[c]])
    a.Wo = s.sbuf("aWo", [128, DC, D], BF16)
    a.Wo_c = [sub(a.Wo, "aWo_%d" % c) for c in range(DC)]
    for c in range(DC):
        s.dma("pool", a.Wo.t[:, c, :], P["swa_out"][c * 128:(c + 1) * 128, :], a.Wo_c[c].name, writes=[a.Wo_c[c]])
    a.Wr = s.sbuf("aWr", [128, DC, NE], BF16)
    s.dma("pool", a.Wr[:], P["router"].rearrange("(c p) e -> p c e", p=128), "aWr", writes=[a.Wr])
    a.brow = s.sbuf("abrow", [1, 1536], F32)
    s.dma("sp", a.brow[:], P["swa_qkv_b"].rearrange("(o n) -> o n", o=1), "abrow", writes=[a.brow])
    a.ones = s.sbuf("aones", [1, 128], F32)
    s.op("pool", lambda e: e.memset(a.ones[:], 1.0), writes=[a.ones])
    a.bout = s.sbuf("about", [128, D], F32)
    s.dma("sp", a.bout[:], bcast_row(P["swa_out_b"]), "about", writes=[a.bout])
    a.sink = s.sbuf("asink", [128, NQH], F32)
    s.dma("sp", a.sink[:], bcast_row(P["sinks"]), "asink", writes=[a.sink])
    a.gain3 = s.sbuf("again3", [128, D], F32)
    s.dma("sp", a.gain3[:], bcast_row(P["mix_norm1"]), "again3", writes=[a.gain3])
    a.gain4 = s.sbuf("again4", [128, D], F32)
    s.dma("sp", a.gain4[:], bcast_row(P["ffn_norm1"]), "again4", writes=[a.gain4])
    a.dist = s.sbuf("adist", [128, 384], F32)
    a.disti = s.sbuf("adisti", [128, 384], I32)
    s.op("pool", lambda e: e.iota(a.disti[:], pattern=[[-1, 384]], base=128, channel_multiplier=1),
         writes=[a.disti])
    s.op("dve", lambda e: e.tensor_copy(out=a.dist[:], in_=a.disti[:]), reads=[a.disti], writes=[a.dist])
    a.ndist = s.sbuf("andist", [128, 384], F32)
    s.op("dve", lambda e: e.tensor_scalar(out=a.ndist[:], in0=a.dist[:], scalar1=-1.0, scalar2=None, op0=ALU.mult),
         reads=[a.dist], writes=[a.ndist])
    s.op("dve", lambda e: e.tensor_tensor(out=a.dist[:], in0=a.dist[:], in1=a.ndist[:], op=ALU.max),
         reads=[a.dist, a.ndist], writes=[a.dist])
    a.wmask = s.sbuf("awmask", [128, 384], F32)
    s.op("dve", lambda e: e.tensor_scalar(out=a.wmask[:], in0=a.dist[:], scalar1=128.0, scalar2=NEG,
                                          op0=ALU.is_gt, op1=ALU.mult), reads=[a.dist], writes=[a.wmask])
    a.bias = []
    for h in range(NQH):
        b = s.sbuf("abias%d" % h, [128, 384], F32)
        slope = float(np.float32(2.0 ** (-8.0 * (h + 1) / NQH)))
        s.op("dve", lambda e, b=b, slope=slope: e.scalar_tensor_tensor(
            out=b[:], in0=a.dist[:], scalar=-slope, in1=a.wmask[:], op0=ALU.mult, op1=ALU.add),
            reads=[a.dist, a.wmask], writes=[b])
        a.bias.append(b)
    a.x = [s.sbuf("ax%d" % i, [128, D], F32) for i in range(3)]
    a.qT = [s.sbuf("aqT%d" % i, [64, NQH, 128], BF16) for i in range(3)]
    a.kT = [s.sbuf("akT%d" % i, [64, NKV, 128], BF16) for i in range(4)]
    a.v = [s.sbuf("av%d" % i, [128, NKV * HD], BF16) for i in range(4)]
    a.hn = s.sbuf("ahn", [128, D], BF16)
    a.hnT = s.sbuf("ahnT", [128, DC, 128], BF16)
    a.junk = s.sbuf("ajunk", [128, D], F32)
    a.ssq = s.sbuf("assq", [128, 1], F32)
    a.rstd = s.sbuf("arstd", [128, 1], F32)
    a.sc = [s.sbuf("asc%d" % i, [128, 384], F32) for i in range(2)]
    a.p = [s.sbuf("ap%d" % i, [128, 384], BF16) for i in range(2)]
    a.pT = [s.sbuf("apT%d" % i, [128, 384], BF16) for i in range(2)]
    a.st = [s.sbuf("ast%d" % i, [128, 8], F32) for i in range(2)]
    a.sc4 = [s.sbuf("asc4%d" % i, [128, 4, 384], F32) for i in range(2)]
    a.p4 = [s.sbuf("ap4%d" % i, [128, 4, 384], BF16) for i in range(2)]
    a.pT4 = [s.sbuf("apT4%d" % i, [128, 4, 384], BF16) for i in range(2)]
    a.st4 = [s.sbuf("ast4%d" % i, [128, 24], F32) for i in range(2)]
    a.negsink = s.sbuf("anegsink", [128, NQH], F32)
    s.op("dve", lambda e: e.tensor_scalar(out=a.negsink[:], in0=a.sink[:], scalar1=-1.0, scalar2=None, op0=ALU.mult),
         reads=[a.sink], writes=[a.negsink])
    a.attn = s.sbuf("aattn", [128, D], BF16)
    a.attn_h = [sub(a.attn, "aattn_%d" % h) for h in range(NQH)]
    a.aT = s.sbuf("aaT", [128, DC, 128], BF16)
    a.h3 = s.sbuf("ah3", [128, D], F32)
    a.h3A = sub(a.h3, "ah3A")
    a.h3B = sub(a.h3, "ah3B")
    a.hn4 = s.sbuf("ahn4", [128, D], BF16)
    a.hn4T = s.sbuf("ahn4T", [128, DC, 128], BF16)
    a.rt = s.sbuf("art", [128, 8 * 8], F32)


def swa_produce(cx, t, h2_ap, h2_db, part=None):
    s = cx.s
    a = cx.a
    ps = cx.ps
    x = a.x[t % 3]
    if part in (None, 0):
        s.dma("sp", x[:], h2_ap[t * 128:(t + 1) * 128, :], x.name, reads=[h2_db[t]], writes=[x])
        emit_norm_T(cx, x, a.gain3, a.hn, ps[0], a.hnT, a.ssq, a.rstd, a.junk)
    W = a.W.t
    qT = a.qT[t % 3]
    for b4 in (range(4) if part in (None, 1) else ()):
        bank = ps[1 + (b4 % 2)]
        for hh in range(4):
            h = b4 * 4 + hh
            oc = slice(hh * 128, (hh + 1) * 128)
            for c in range(DC):
                s.op("pe", lambda e, c=c, h=h, oc=oc, bank=bank: e.matmul(
                    bank[0:64, oc], lhsT=W[:, c, h * 64:(h + 1) * 64], rhs=a.hnT[:, c, :],
                    start=(c == 0), stop=False), reads=[a.hnT, a.W_c[c]], writes=[bank])
            s.op("pe", lambda e, h=h, oc=oc, bank=bank: e.matmul(
                bank[0:64, oc], lhsT=a.brow[0:1, h * 64:(h + 1) * 64], rhs=a.ones[0:1, :],
                start=False, stop=True), reads=[a.brow, a.ones], writes=[bank])
        s.op("act", lambda e, b4=b4, bank=bank: e.activation(
            out=qT[:, b4 * 4:(b4 + 1) * 4, :].rearrange("p h t -> p (h t)"), in_=bank[0:64, :], func=AF.Copy,
            scale=0.125), reads=[bank], writes=[qT])
    if part not in (None, 2):
        return
    kT = a.kT[t % 4]
    bank = ps[3]
    for kv in range(NKV):
        oc = slice(kv * 128, (kv + 1) * 128)
        for c in range(DC):
            s.op("pe", lambda e, c=c, kv=kv, oc=oc, bank=bank: e.matmul(
                bank[0:64, oc], lhsT=W[:, c, 1024 + kv * 64:1024 + (kv + 1) * 64], rhs=a.hnT[:, c, :],
                start=(c == 0), stop=False), reads=[a.hnT, a.W_c[c]], writes=[bank])
        s.op("pe", lambda e, kv=kv, oc=oc, bank=bank: e.matmul(
            bank[0:64, oc], lhsT=a.brow[0:1, 1024 + kv * 64:1024 + (kv + 1) * 64], rhs=a.ones[0:1, :],
            start=False, stop=True), reads=[a.brow, a.ones], writes=[bank])
    s.op("dve", lambda e, bank=bank: e.tensor_copy(out=kT[:].rearrange("p h t -> p (h t)"), in_=bank[0:64, :]),
         reads=[bank], writes=[kT])
    v = a.v[t % 4]
    bank = ps[4]
    for c in range(DC):
        s.op("pe", lambda e, c=c, bank=bank: e.matmul(bank[:, 0:256], lhsT=a.hnT[:, c, :], rhs=W[:, c, 1280:1536],
                                           start=(c == 0), stop=False), reads=[a.hnT, a.W_c[c]], writes=[bank])
    s.op("pe", lambda e, bank=bank: e.matmul(bank[:, 0:256], lhsT=a.ones[0:1, :], rhs=a.brow[0:1, 1280:1536],
                                  start=False, stop=True), reads=[a.brow, a.ones], writes=[bank])
    s.op("act", lambda e, bank=bank: e.copy(out=v[:], in_=bank[:, 0:256]), reads=[bank], writes=[v])


def swa_attend(cx, t, NT, h3_ap, h3_db, hnT_ap, hnT_db, cw_all, hn4tm_ap=None, part=None):
    s = cx.s
    a = cx.a
    ps = cx.ps
    x = a.x[t % 3]
    qT = a.qT[t % 3]
    kts = [kt for kt in (t - 1, t, t + 1) if 0 <= kt < NT]
    c0 = (kts[0] - (t - 1)) * 128
    c1 = (kts[-1] - (t - 1) + 1) * 128
    for g4 in (range(NKV) if part is None else ([part] if part < NKV else [])):
        kv = g4
        j = g4 % 2
        p, pT, st = a.p4[j], a.pT4[j], a.st4[j]
        for hh in range(4):
            h = g4 * 4 + hh
            sb_ = ps[4 + hh]
            for i, kt in enumerate(kts):
                cc = (kt - (t - 1)) * 128
                s.op("pe", lambda e, h=h, kt=kt, cc=cc, sb_=sb_, kv=kv: e.matmul(
                    sb_[:, cc:cc + 128], lhsT=qT[:, h, :], rhs=a.kT[kt % 4][:, kv, :], start=True, stop=True),
                    reads=[qT, a.kT[kt % 4]], writes=[sb_])
        sc = a.sc4[j]
        for hh in range(4):
            h = g4 * 4 + hh
            s.op("dve", lambda e, h=h, hh=hh, sc=sc: e.tensor_tensor(
                out=sc[:, hh, c0:c1], in0=ps[4 + hh][:, c0:c1], in1=a.bias[h][:, c0:c1], op=ALU.add),
                reads=[ps[4 + hh], a.bias[h], sc], writes=[sc])
        for hh in range(4):
            s.op("dve", lambda e, hh=hh, st=st, sc=sc: e.reduce_max(out=st[:, hh:hh + 1], in_=sc[:, hh, c0:c1], axis=AX.X),
                 reads=[sc, st], writes=[st])
        s.op("dve", lambda e, st=st, g4=g4: e.scalar_tensor_tensor(
            out=st[:, 4:8], in0=st[:, 0:4], scalar=-1.0, in1=a.negsink[:, g4 * 4:(g4 + 1) * 4], op0=ALU.mult, op1=ALU.min),
            reads=[st, a.negsink], writes=[st])
        for hh in range(4):
            s.op("act", lambda e, hh=hh, p=p, st=st, sc=sc: e.activation(
                out=p[:, hh, c0:c1], in_=sc[:, hh, c0:c1], func=AF.Exp, bias=st[:, 4 + hh:5 + hh],
                accum_out=st[:, 8 + hh:9 + hh]), reads=[sc, st], writes=[p, st])
        s.op("dve", lambda e, st=st, g4=g4: e.tensor_tensor(out=st[:, 12:16], in0=a.sink[:, g4 * 4:(g4 + 1) * 4],
                                                            in1=st[:, 4:8], op=ALU.add), reads=[st, a.sink], writes=[st])
        s.op("act", lambda e, st=st: e.activation(out=st[:, 12:16], in_=st[:, 12:16], func=AF.Exp), reads=[st], writes=[st])
        s.op("dve", lambda e, st=st: e.tensor_tensor(out=st[:, 16:20], in0=st[:, 8:12], in1=st[:, 12:16], op=ALU.add),
             reads=[st], writes=[st])
        s.op("dve", lambda e, st=st: e.reciprocal(out=st[:, 20:24], in_=st[:, 16:20]), reads=[st], writes=[st])
        for half, tb in ((0, ps[0]), (1, ps[3])):
            tv = Bview(tb)
            for h2 in range(2):
                hh = half * 2 + h2
                for kt in kts:
                    cc = (kt - (t - 1)) * 128
                    s.op("pe", lambda e, cc=cc, hh=hh, h2=h2, p=p, tv=tv: e.transpose(
                        out=tv[:, h2 * 384 + cc:h2 * 384 + cc + 128], in_=p[:, hh, cc:cc + 128], identity=cx.ident[:]),
                        reads=[p, cx.ident], writes=[tb])
            full = (c0 == 0 and c1 == 384)
            segs = [(0, 768, None)] if full else [(h2 * 384 + c0, h2 * 384 + c1, h2) for h2 in range(2)]
            for (x0, x1, h2) in segs:
                if h2 is None:
                    dst = pT[:, half * 2:half * 2 + 2, :].rearrange("p h c -> p (h c)")
                else:
                    dst = pT[:, half * 2 + h2, c0:c1]
                if half == 0:
                    s.op("act", lambda e, dst=dst, tv=tv, x0=x0, x1=x1: e.copy(out=dst, in_=tv[:, x0:x1]),
                         reads=[tb], writes=[pT])
                else:
                    s.op("dve", lambda e, dst=dst, tv=tv, x0=x0, x1=x1: e.tensor_copy(out=dst, in_=tv[:, x0:x1]),
                         reads=[tb, pT], writes=[pT])
        for hh in range(4):
            h = g4 * 4 + hh
            ob = ps[1 + h // 8]
            oc = slice((h % 8) * 64, (h % 8) * 64 + 64)
            for i, kt in enumerate(kts):
                cc = (kt - (t - 1)) * 128
                s.op("pe", lambda e, kt=kt, cc=cc, kv=kv, i=i, ob=ob, oc=oc, pT=pT, hh=hh: e.matmul(
                    ob[:, oc], lhsT=pT[:, hh, cc:cc + 128], rhs=a.v[kt % 4][:, kv * 64:(kv + 1) * 64],
                    start=(i == 0), stop=(i == len(kts) - 1)), reads=[pT, a.v[kt % 4]], writes=[ob])
        for hh in range(4):
            h = g4 * 4 + hh
            ob = ps[1 + h // 8]
            oc = slice((h % 8) * 64, (h % 8) * 64 + 64)
            s.op("dve", lambda e, h=h, hh=hh, ob=ob, oc=oc, st=st: e.tensor_scalar(
                out=a.attn[:, h * 64:(h + 1) * 64], in0=ob[:, oc], scalar1=st[:, 20 + hh:21 + hh], scalar2=None,
                op0=ALU.mult), reads=[ob, st], writes=[a.attn_h[h]])
    if part is not None and part < NKV:
        return
    tb = ps[0]
    tv = Bview(tb)
    for c in range(DC):
        s.op("pe", lambda e, c=c, tv=tv: e.transpose(out=tv[:, c * 128:(c + 1) * 128], in_=a.attn[:, c * 128:(c + 1) * 128],
                                              identity=cx.ident[:]), reads=a.attn_h + [cx.ident], writes=[tb])
    s.op("act", lambda e, tv=tv: e.copy(out=a.aT[:].rearrange("p c t -> p (c t)"), in_=tv[:]), reads=[tb], writes=[a.aT])
    for hh in range(2):
        yb = ps[3 + hh]
        for c in range(DC):
            s.op("pe", lambda e, c=c, hh=hh, yb=yb: e.matmul(
                yb[:], lhsT=a.aT[:, c, :], rhs=a.Wo.t[:, c, hh * 512:(hh + 1) * 512],
                start=(c == 0), stop=(c == DC - 1)), reads=[a.aT, a.Wo_c[c]], writes=[yb])
    for hh, hb in ((0, a.h3A), (1, a.h3B)):
        sl = slice(hh * 512, (hh + 1) * 512)
        s.op("dve", lambda e, hh=hh, sl=sl: e.tensor_tensor(out=a.h3[:, sl], in0=ps[3 + hh][:], in1=x[:, sl], op=ALU.add),
             reads=[ps[3 + hh], x], writes=[hb])
        s.op("pool", lambda e, sl=sl: e.tensor_tensor(out=a.h3[:, sl], in0=a.h3[:, sl], in1=a.bout[:, sl], op=ALU.add),
             reads=[hb, a.bout], writes=[hb])
    s.dma("sp", h3_ap[t * 128:(t + 1) * 128, :], a.h3[:], "ah3", reads=[a.h3A, a.h3B], writes=[h3_db[t]])
    emit_norm_T(cx, a.h3, a.gain4, a.hn4, ps[0], a.hn4T, a.ssq, a.rstd, a.junk, xdeps=[a.h3A, a.h3B])
    s.dma("sp", hnT_ap.rearrange("(c p) t -> p c t", p=128)[:, :, t * 128:(t + 1) * 128], a.hn4T[:], "ahn4T",
          reads=[a.hn4T], writes=[hnT_db[t]])
    if hn4tm_ap is not None:
        s.dma("sp", hn4tm_ap[t * 128:(t + 1) * 128, :], a.hn4[:], "ahn4tm", reads=[a.hn4])
    lb = ps[7]
    for c in range(DC):
        s.op("pe", lambda e, c=c: e.matmul(lb[:, 0:NE], lhsT=a.hn4T[:, c, :], rhs=a.Wr[:, c, :],
                                           start=(c == 0), stop=(c == DC - 1)), reads=[a.hn4T, a.Wr], writes=[lb])
    r = a.rt
    R = lambda i: r[:, i * 8:(i + 1) * 8]
    cwt = cw_all[:, t * NE:(t + 1) * NE]

    def dv(fn, rd=(), wr=()):
        s.op("dve", fn, reads=[a.rt] + list(rd), writes=[a.rt] + list(wr))
    dv(lambda e: e.tensor_copy(out=R(0), in_=lb[:, 0:NE]), rd=[lb])
    dv(lambda e: e.reduce_max(out=r[:, 56:57], in_=R(0), axis=AX.X))
    dv(lambda e: e.tensor_scalar(out=R(1), in0=R(0), scalar1=r[:, 56:57], scalar2=None, op0=ALU.is_equal))
    dv(lambda e: e.scalar_tensor_tensor(out=R(2), in0=R(1), scalar=NEG, in1=R(0), op0=ALU.mult, op1=ALU.add))
    dv(lambda e: e.reduce_max(out=r[:, 57:58], in_=R(2), axis=AX.X))
    dv(lambda e: e.tensor_scalar(out=R(3), in0=R(2), scalar1=r[:, 57:58], scalar2=None, op0=ALU.is_equal))
    dv(lambda e: e.tensor_tensor(out=r[:, 58:59], in0=r[:, 57:58], in1=r[:, 56:57], op=ALU.subtract))
    s.op("act", lambda e: e.activation(out=r[:, 59:60], in_=r[:, 58:59], func=AF.Exp), reads=[a.rt], writes=[a.rt])
    dv(lambda e: e.tensor_scalar(out=r[:, 60:61], in0=r[:, 59:60], scalar1=1.0, scalar2=None, op0=ALU.add))
    dv(lambda e: e.reciprocal(out=r[:, 61:62], in_=r[:, 60:61]))
    dv(lambda e: e.tensor_tensor(out=r[:, 62:63], in0=r[:, 59:60], in1=r[:, 61:62], op=ALU.mult))
    dv(lambda e: e.tensor_scalar(out=R(4), in0=R(1), scalar1=r[:, 61:62], scalar2=None, op0=ALU.mult))
    dv(lambda e: e.scalar_tensor_tensor(out=cwt, in0=R(3), scalar=r[:, 62:63], in1=R(4), op0=ALU.mult, op1=ALU.add),
       wr=[cw_all])


def new_phase(cx, nc):
    cx.s = Sched(nc)
    for b in cx.persist:
        b.lw = None
        b.rd = {}
        b.rd_dma = []
    return cx.s


def build_program(S, TB=256, sparse=True):
    NT = S // 128
    nc = bass.Bass("TRN2", target_bir_lowering=False)

    def din(name, shape):
        return nc.dram_tensor(name, list(shape), F32, kind="ExternalInput").ap()

    x = din("x", [S, D])
    P = {
        "mix_norm0": din("mix_norm0", [D]), "mix_norm1": din("mix_norm1", [D]),
        "ffn_norm0": din("ffn_norm0", [D]), "ffn_norm1": din("ffn_norm1", [D]),
        "gla_in": din("gla_in", [D, GW]), "gw_f": din("gw_f", [16, 512]), "gb_f": din("gb_f", [512]),
        "gw_b": din("gw_b", [16, 512]), "gb_b": din("gb_b", [512]), "gla_hn": din("gla_hn", [D]),
        "gla_out": din("gla_out", [D, D]),
        "swa_qkv": din("swa_qkv", [D, 1536]), "swa_qkv_b": din("swa_qkv_b", [1536]), "sinks": din("sinks", [NQH]),
        "swa_out": din("swa_out", [D, D]), "swa_out_b": din("swa_out_b", [D]),
        "dWg": din("dWg", [D, FF]), "dWu": din("dWu", [D, FF]), "dWd": din("dWd", [FF, D]),
        "router": din("router", [D, NE]),
        "mWg": din("mWg", [NE, D, FF]), "mWu": din("mWu", [NE, D, FF]), "mWd": din("mWd", [NE, FF, D]),
        "final_norm": din("final_norm", [D]),
    }
    out = nc.dram_tensor("out", [S, D], F32, kind="ExternalOutput").ap()
    Sb = nc.dram_tensor("scr_Sb", [NT, 128, NH * 256], BF16).ap()
    h1 = nc.dram_tensor("scr_h1", [S, D], F32).ap()
    h3 = nc.dram_tensor("scr_h3", [S + 128, D], F32).ap()
    hnT = nc.dram_tensor("scr_hnT", [D, S], BF16).ap()
    hnT2 = nc.dram_tensor("scr_hnT2", [D, S], BF16).ap()
    hn4tm = nc.dram_tensor("scr_hn4tm", [S + 128, D], BF16).ap()
    lst = nc.dram_tensor("scr_list", [NE * CAPR + NT * NE * 128, 2], I32).ap()

    cx = Ctx()
    pstack = contextlib.ExitStack()
    Sched.sem_pool = {"sw": [], "hw": [], "eng": []}
    Sched.sem_total = {}
    Sched.sem_stack = pstack
    cx.persist = []
    s = cx.s = Sched(nc)
    keep = s.stack
    s.stack = pstack
    emit_consts(cx)
    emit_psum(cx)
    cw_all = s.sbuf("cw_all", [128, NT * NE], F32)
    flag = s.sbuf("flag", [128, 1], I32)
    s.stack = keep
    regs = {}
    for en, eo in (("pe", nc.tensor), ("act", nc.scalar), ("dve", nc.vector), ("pool", nc.gpsimd), ("sp", nc.sync)):
        regs[en] = pstack.enter_context(eo.register("flagreg_" + en))
    cx.persist = [cx.ident, cx.identf, cx.epsc, cw_all, flag] + cx.ps
    cx.cw_buf = cw_all
    alloc_gla(cx, P)
    Sb_db = [s.dbuf("Sbdb%d" % t) for t in range(NT)]
    h1_db = [s.dbuf("h1db%d" % t) for t in range(NT)]
    hn_db = [s.dbuf("hndb%d" % t) for t in range(NT)]
    for t in reversed(range(NT)):
        emit_gla_tile(cx, t, False, x, Sb, Sb_db, h1, h1_db, hnT, hn_db)
    for t in range(NT):
        emit_gla_tile(cx, t, True, x, Sb, Sb_db, h1, h1_db, hnT, hn_db)
    s.emit()
    if _PH == 1:
        pstack.close()
        return nc
    s = new_phase(cx, nc)
    alloc_ffn_bufs(cx, TB)
    ws = [alloc_wset(cx, 0), alloc_wset(cx, 1)]
    db1 = [s.dbuf("p2a%d" % t) for t in range(NT)]
    db2 = [s.dbuf("p2b%d" % t) for t in range(NT)]
    for f in ffn_weight_loads(cx, ws[0], P["dWg"], P["dWu"], P["dWd"], 0):
        f()
    pre = ffn_weight_loads(cx, ws[1], P["dWg"], P["dWu"], P["dWd"], 1)
    ctr = emit_ffn_pass(cx, S, hnT, db1, ws[0], None, None, h1, db2, pre=pre)
    emit_ffn_pass(cx, S, hnT, db1, ws[1], None, None, h1, db2, ctr=ctr)
    s.emit()
    if _PH == 2:
        pstack.close()
        return nc
    s = new_phase(cx, nc)
    alloc_swa(cx, P, NT)
    d2 = [s.dbuf("p3a%d" % t) for t in range(NT)]
    d3 = [s.dbuf("p3b%d" % t) for t in range(NT)]
    d4 = [s.dbuf("p3c%d" % t) for t in range(NT)]
    swa_produce(cx, 0, h1, d2)
    if NT > 1:
        swa_produce(cx, 1, h1, d2)
    for t in range(NT):
        for part in range(NKV):
            swa_attend(cx, t, NT, h3, d3, hnT2, d4, cw_all, part=part)
            if t + 2 < NT and part < 3:
                swa_produce(cx, t + 2, h1, d2, part=part)
        swa_attend(cx, t, NT, h3, d3, hnT2, d4, cw_all, hn4tm_ap=(hn4tm if sparse else None), part=NKV)
    s.emit()
    if _PH == 3:
        pstack.close()
        return nc

    def moe_dense():
        s = cx.s
        db1 = [s.dbuf("p4a%d" % t) for t in range(NT)]
        db2 = [s.dbuf("p4b%d" % t) for t in range(NT)]
        for f in ffn_weight_loads(cx, ws[0], P["mWg"][0], P["mWu"][0], P["mWd"][0], 0):
            f()
        ctr = None
        for he in range(2 * NE):
            e_, half = he // 2, he % 2
            pre = ()
            if he + 1 < 2 * NE:
                e2, h2_ = (he + 1) // 2, (he + 1) % 2
                pre = ffn_weight_loads(cx, ws[(he + 1) % 2], P["mWg"][e2], P["mWu"][e2], P["mWd"][e2], h2_)
            ctr = emit_ffn_pass(cx, S, hnT2, db1, ws[he % 2], cw_all, (lambda t, e_=e_: t * NE + e_), h3, db2,
                                pre=pre, ctr=ctr)
        emit_final_norm(cx, S, h3, out, P["final_norm"], lambda t: [db2[t]])

    def moe_sparse():
        s = cx.s
        h3db = s.dbuf("h3db")
        for f in ffn_weight_loads(cx, ws[0], P["mWg"][0], P["mWu"][0], P["mWd"][0], 0):
            f()
        ctr = None
        for he in range(2 * NE):
            e_, half = he // 2, he % 2
            pre = ()
            if he + 1 < 2 * NE:
                e2, h2_ = (he + 1) // 2, (he + 1) % 2
                pre = ffn_weight_loads(cx, ws[(he + 1) % 2], P["mWg"][e2], P["mWu"][e2], P["mWd"][e2], h2_)
            ctr = emit_moe_sparse_pass(cx, S, e_, ws[he % 2], lst, hn4tm, h3, h3db, pre, ctr)
        emit_final_norm(cx, S, h3, out, P["final_norm"], lambda t: [h3db])

    if not sparse:
        s = new_phase(cx, nc)
        alloc_ffn_bufs(cx, TB)
        ws = [alloc_wset(cx, 0), alloc_wset(cx, 1)]
        cx.fst = [s.sbuf("fst%d" % i, [128, 2], F32) for i in range(3)]
        moe_dense()
        s.emit()
        pstack.close()
        return nc
    s = new_phase(cx, nc)
    emit_route(cx, S, cw_all, lst, flag)
    if _os.environ.get("DBG"):
        dbg = nc.dram_tensor("dbg", [128, NE + 2], F32, kind="ExternalOutput").ap()
        s.dma("sp", dbg[:, 0:NE], cx.r_base[:, NT * NE:NT * NE + NE], "r_dbg", reads=[cx.r_base])
        s.dma("sp", dbg[:, NE:NE + 2], cx.r_fl[:], "r_dbg2", reads=[cx.r_fl])
    zf = s.sbuf("r_zf", [128, D], F32)
    zb = s.sbuf("r_zb", [128, D], BF16)
    s.op("pool", lambda e: e.memset(zf[:], 0.0), writes=[zf])
    s.op("pool", lambda e: e.memset(zb[:], 0.0), writes=[zb])
    s.dma("sp", h3[S:S + 128, :], zf[:], "r_zf", reads=[zf])
    s.dma("sp", hn4tm[S:S + 128, :], zb[:], "r_zb", reads=[zb])
    s.emit()
    sa = new_phase(cx, nc)
    alloc_ffn_bufs(cx, TB)
    ws = [alloc_wset(cx, 0), alloc_wset(cx, 1)]
    g = cx.sp = Ctx()
    g.cnt = 0
    g.tile_idx = {}
    g.tile_gx = {}
    g.idx = [sa.sbuf("sp_idx%d" % i, [128, 2], I32) for i in range(8)]
    g.gx = [sa.sbuf("sp_gx%d" % i, [128, D], BF16) for i in range(6)]
    cx.fst = [sa.sbuf("fst%d" % i, [128, 2], F32) for i in range(3)]
    moe_sparse()
    for b in Buf.registry:
        b.lw = None
        b.rd = {}
        b.rd_dma = []
    sb_ = cx.s = Sched(nc)
    moe_dense()
    emit_either(nc, flag, regs, sa, sb_)
    pstack.close()
    return nc


def emit_final_norm(cx, S, h3, out, gain_ap, h3dep):
    s = cx.s
    NT = S // 128

    def fview(b):
        return b.t[:].rearrange("p a b -> p (a b)").bitcast(F32)[:, 0:D]
    fgB, fjB = cx.hnTb[1], cx.hnTb[0]
    s.dma("sp", fview(fgB), bcast_row(gain_ap), "fgain", writes=[fgB])
    for t in range(NT):
        i = t % 3
        xb, xd = cx.stg[i], [cx.stg[i], cx.stgA[i], cx.stgB[i]]
        ob = cx.hT[t % 2]
        st = cx.fst[i]
        s.dma("sp", xb[:], h3[t * 128:(t + 1) * 128, :], "fx%d" % i, reads=h3dep(t), writes=xd)
        s.op("act", lambda e, xb=xb, st=st: e.activation(out=fview(fjB), in_=xb[:], func=AF.Square, scale=1.0 / 32.0,
                                                         accum_out=st[:, 0:1]), reads=xd, writes=[fjB, st])
        s.op("act", lambda e, st=st: e.activation(out=st[:, 1:2], in_=st[:, 0:1], func=AF.Ln, bias=cx.epsc[:, 0:1]),
             reads=[st, cx.epsc], writes=[st])
        s.op("act", lambda e, st=st: e.activation(out=st[:, 1:2], in_=st[:, 1:2], func=AF.Exp, scale=-0.5),
             reads=[st], writes=[st])
        s.op("dve", lambda e, xb=xb, ob=ob, st=st: e.scalar_tensor_tensor(
            out=fview(ob), in0=xb[:], scalar=st[:, 1:2], in1=fview(fgB), op0=ALU.mult, op1=ALU.mult),
            reads=xd + [st, fgB], writes=[ob])
        s.dma("sp", out[t * 128:(t + 1) * 128, :], fview(ob), "fo%d" % (t % 2), reads=[ob])


PARAM_MAP = [
    ("mix_norm0", "mix_norm", 0), ("mix_norm1", "mix_norm", 1), ("ffn_norm0", "ffn_norm", 0), ("ffn_norm1", "ffn_norm", 1),
    ("gla_in", "gla_in_proj", 0), ("gw_f", "gla_gate_w_fwd", 0), ("gb_f", "gla_gate_b_fwd", 0),
    ("gw_b", "gla_gate_w_bwd", 0), ("gb_b", "gla_gate_b_bwd", 0), ("gla_hn", "gla_head_norm", 0),
    ("gla_out", "gla_out_proj", 0), ("swa_qkv", "swa_qkv_proj", 0), ("swa_qkv_b", "swa_qkv_bias", 0),
    ("sinks", "swa_sinks", 0), ("swa_out", "swa_out_proj", 0), ("swa_out_b", "swa_out_bias", 0),
    ("dWg", "dense_w_gate", 0), ("dWu", "dense_w_up", 0), ("dWd", "dense_w_down", 0), ("router", "moe_router", 0),
    ("mWg", "moe_w_gate", 0), ("mWu", "moe_w_up", 0), ("mWd", "moe_w_down", 0), ("final_norm", "final_norm", None),
]


def make_in_map(inputs, xs):
    m = {"x": np.ascontiguousarray(xs, dtype=np.float32)}
    for dst, src, idx in PARAM_MAP:
        v = np.asarray(inputs[src])
        if idx is not None:
            v = v[idx]
        m[dst] = np.ascontiguousarray(v, dtype=np.float32)
    return m


def kernel(**inputs):
    x = np.asarray(inputs["x"])
    B, S, _ = x.shape
    nc = build_program(S)
    base = make_in_map(inputs, x[0])
    in_maps = []
    for b in range(B):
        m = dict(base)
        m["x"] = np.ascontiguousarray(x[b], dtype=np.float32)
        in_maps.append(m)
    res = run_bass_kernel_spmd(nc, in_maps, core_ids=list(range(B)))
    return np.stack([np.asarray(r["out"]) for r in res.results], axis=0).astype(np.float32)


CAPT = 20
CAPR = CAPT * 128
BIGI = 1.0e6


def emit_route(cx, S, cw_all, list_ap, flag):
    s = cx.s
    NT = S // 128
    NC = NT * NE
    nb = (NC + 511) // 512
    m = s.sbuf("r_m", [128, NC], F32)
    mb = s.sbuf("r_mb", [128, NC], BF16)
    s.op("dve", lambda e: e.tensor_single_scalar(out=m[:], in_=cw_all[:], scalar=0.0, op=ALU.is_gt),
         reads=[cw_all], writes=[m])
    s.op("dve", lambda e: e.tensor_copy(out=mb[:], in_=m[:]), reads=[m], writes=[mb])
    slf = tri(cx, "r_slf", 1.0, 1, -1, ALU.is_gt)
    sl = s.sbuf("r_sl", [128, 128], BF16)
    s.op("dve", lambda e: e.tensor_copy(out=sl[:], in_=slf[:]), reads=[slf], writes=[sl])
    on = s.sbuf("r_on", [128, 128], BF16)
    s.op("pool", lambda e: e.memset(on[:], 1.0), writes=[on])
    within = s.sbuf("r_within", [128, NC], F32)
    tot = s.sbuf("r_tot", [128, NC], F32)
    for k in range(nb):
        cs = slice(k * 512, min(NC, (k + 1) * 512))
        n = cs.stop - cs.start
        s.op("pe", lambda e, cs=cs, n=n: e.matmul(cx.ps[0][:, 0:n], lhsT=sl[:], rhs=mb[:, cs], start=True, stop=True),
             reads=[sl, mb], writes=[cx.ps[0]])
        s.op("pe", lambda e, cs=cs, n=n: e.matmul(cx.ps[1][:, 0:n], lhsT=on[:], rhs=mb[:, cs], start=True, stop=True),
             reads=[on, mb], writes=[cx.ps[1]])
        s.op("act", lambda e, cs=cs, n=n: e.copy(out=within[:, cs], in_=cx.ps[0][:, 0:n]),
             reads=[cx.ps[0], within], writes=[within])
        s.op("dve", lambda e, cs=cs, n=n: e.tensor_copy(out=tot[:, cs], in_=cx.ps[1][:, 0:n]),
             reads=[cx.ps[1], tot], writes=[tot])
    base = s.sbuf("r_base", [128, NC + NE], F32)
    s.op("pool", lambda e: e.memset(base[:, 0:NE], 0.0), writes=[base])
    for t in range(NT):
        s.op("dve", lambda e, t=t: e.tensor_tensor(out=base[:, (t + 1) * NE:(t + 2) * NE], in0=base[:, t * NE:(t + 1) * NE],
                                                   in1=tot[:, t * NE:(t + 1) * NE], op=ALU.add),
             reads=[base, tot], writes=[base])
    fl = cx.r_fl = s.sbuf("r_fl", [128, 2], F32)
    cx.r_base = base
    s.op("dve", lambda e: e.reduce_max(out=fl[:, 0:1], in_=base[:, NC:NC + NE], axis=AX.X), reads=[base], writes=[fl])
    s.op("dve", lambda e: e.tensor_single_scalar(out=fl[:, 1:2], in_=fl[:, 0:1], scalar=(-1.0 if _os.environ.get('FORCEDENSE') else float(CAPR) + 0.5), op=ALU.is_lt),
         reads=[fl], writes=[fl])
    s.op("dve", lambda e: e.tensor_copy(out=flag[:], in_=fl[:, 1:2]), reads=[fl], writes=[flag])
    offi = s.sbuf("r_offi", [128, NC], I32)
    s.op("pool", lambda e: e.iota(offi[:].rearrange("p (t e) -> p t e", e=NE), pattern=[[0, NT], [CAPR, NE]], base=0,
                                  channel_multiplier=0), writes=[offi])
    dest = s.sbuf("r_dest", [128, NC], F32)
    s.op("dve", lambda e: e.tensor_copy(out=dest[:], in_=offi[:]), reads=[offi], writes=[dest])
    s.op("dve", lambda e: e.tensor_tensor(out=dest[:], in0=dest[:], in1=base[:, 0:NC], op=ALU.add),
         reads=[dest, base], writes=[dest])
    s.op("dve", lambda e: e.tensor_tensor(out=dest[:], in0=dest[:], in1=within[:], op=ALU.add),
         reads=[dest, within], writes=[dest])
    s.op("dve", lambda e: e.tensor_tensor(out=dest[:], in0=dest[:], in1=m[:], op=ALU.mult),
         reads=[dest, m], writes=[dest])
    nrow = NE * CAPR
    dumpi = s.sbuf("r_dumpi", [128, NC], I32)
    s.op("pool", lambda e: e.iota(dumpi[:], pattern=[[128, NC]], base=nrow, channel_multiplier=1), writes=[dumpi])
    tmp = s.sbuf("r_tmp", [128, NC], F32)
    s.op("dve", lambda e: e.tensor_copy(out=tmp[:], in_=dumpi[:]), reads=[dumpi], writes=[tmp])
    om = s.sbuf("r_om", [128, NC], F32)
    s.op("dve", lambda e: e.tensor_scalar(out=om[:], in0=m[:], scalar1=-1.0, scalar2=1.0, op0=ALU.mult, op1=ALU.add),
         reads=[m], writes=[om])
    s.op("dve", lambda e: e.tensor_tensor(out=tmp[:], in0=tmp[:], in1=om[:], op=ALU.mult),
         reads=[tmp, om], writes=[tmp])
    s.op("dve", lambda e: e.tensor_tensor(out=dest[:], in0=dest[:], in1=tmp[:], op=ALU.add),
         reads=[dest, tmp], writes=[dest])
    desti = s.sbuf("r_desti", [128, NC], I32)
    s.op("dve", lambda e: e.tensor_copy(out=desti[:], in_=dest[:]), reads=[dest], writes=[desti])
    src = s.sbuf("r_src", [128, NC, 2], I32)
    srcA = sub(src, "r_srcA")
    s.op("pool", lambda e: e.iota(src[:, :, 0].rearrange("p (t e) -> p t e", e=NE), pattern=[[128, NT], [0, NE]], base=0,
                                  channel_multiplier=1), writes=[src])
    s.op("dve", lambda e: e.tensor_copy(out=src[:, :, 1], in_=cw_all[:].bitcast(I32)), reads=[cw_all], writes=[srcA])
    K = nrow // 128
    ini = s.sbuf("r_ini", [128, K, 2], I32)
    iniA = sub(ini, "r_iniA")
    s.op("pool", lambda e: e.iota(ini[:, :, 0], pattern=[[1, K]], base=0, channel_multiplier=K), writes=[ini])
    s.op("dve", lambda e: e.tensor_single_scalar(out=ini[:, :, 0], in_=ini[:, :, 0], scalar=127, op=ALU.bitwise_and),
         reads=[ini], writes=[ini])
    s.op("dve", lambda e: e.tensor_single_scalar(out=ini[:, :, 0], in_=ini[:, :, 0], scalar=S, op=ALU.add),
         reads=[ini], writes=[ini])
    s.op("pool", lambda e: e.memset(ini[:, :, 1], 0), writes=[iniA])
    ldb = s.dbuf("r_listdb")
    s.dma("sp", list_ap[0:nrow, :].rearrange("(p k) c -> p k c", p=128), ini[:], "r_ini", reads=[ini, iniA], writes=[ldb])
    for col in range(NC):
        s.op("pool", lambda e, col=col: e.indirect_dma_start(
            out=list_ap[:, :], out_offset=bass.IndirectOffsetOnAxis(ap=desti[:, col:col + 1], axis=0),
            in_=src[:, col, :], in_offset=None),
            reads=[ldb, desti, src, srcA], writes=[], dma_key="r_scat")


def emit_moe_sparse_pass(cx, S, e_, w, list_ap, hn4tm_ap, h3_ap, h3db, pre, ctr):
    s = cx.s
    TB = cx.TB
    ntt = TB // 128
    g = cx.sp

    def prefetch(b):
        for i in range(ntt):
            j = b * ntt + i
            q = g.cnt
            g.cnt += 1
            idx = g.idx[q % 8]
            gx = g.gx[q % 6]
            g.tile_idx[j] = idx
            g.tile_gx[j] = (gx, q)
            r0 = e_ * CAPR + j * 128
            s.dma("sp", idx[:], list_ap[r0:r0 + 128, :], idx.name, writes=[idx])
            s.op("pool", lambda e, idx=idx, gx=gx: e.indirect_dma_start(
                out=gx[:, :], out_offset=None, in_=hn4tm_ap[:, :],
                in_offset=bass.IndirectOffsetOnAxis(ap=idx[:, 0:1], axis=0)),
                reads=[idx], writes=[gx], dma_key=gx.name)

    def loader(b, hb):
        for i in range(ntt):
            j = b * ntt + i
            gx, q = g.tile_gx[j]
            bank = cx.ps[4 + (q % 2) * 2]
            tv = Bview(bank)
            for c in range(DC):
                s.op("pe", lambda e, c=c, gx=gx, tv=tv: e.transpose(out=tv[:, c * 128:(c + 1) * 128],
                                                                    in_=gx[:, c * 128:(c + 1) * 128], identity=cx.ident[:]),
                     reads=[gx, cx.ident], writes=[bank])
            s.op("act", lambda e, i=i, tv=tv, hb=hb: e.copy(
                out=hb[:, :, i * 128:(i + 1) * 128], in_=tv[:].rearrange("p (c t) -> p c t", c=DC)),
                reads=[bank], writes=[hb])

    def sinker(tile, pd, stg, sa, sb_):
        idx = g.tile_idx[tile]
        cwa = idx[:, 1:2].bitcast(F32)
        s.op("act", lambda e, stg=stg, pd=pd, cwa=cwa: e.activation(
            out=stg[:, 0:512], in_=pd[0][:], func=AF.Copy, scale=cwa), reads=[pd[0], idx], writes=[sa])
        s.op("dve", lambda e, stg=stg, pd=pd, cwa=cwa: e.tensor_scalar(
            out=stg[:, 512:1024], in0=pd[1][:], scalar1=cwa, scalar2=None, op0=ALU.mult),
            reads=[pd[1], idx], writes=[sb_])
        s.op("pool", lambda e, stg=stg, idx=idx: e.indirect_dma_start(
            out=h3_ap[:, :], out_offset=bass.IndirectOffsetOnAxis(ap=idx[:, 0:1], axis=0), in_=stg[:, :],
            in_offset=None, compute_op=ALU.add),
            reads=[sa, sb_, idx, h3db], writes=[h3db], dma_key=stg.name)

    return emit_ffn_pass(cx, CAPR, None, None, w, None, None, None, None, pre=pre, ctr=ctr,
                         loader=loader, sinker=sinker, prefetch=prefetch)
```

```python
import contextlib
import numpy as np
import concourse.bass as bass
import concourse.mybir as mybir
from concourse.bass_utils import run_bass_kernel_spmd

F32 = mybir.dt.float32
BF16 = mybir.dt.bfloat16
I32 = mybir.dt.int32
ALU = mybir.AluOpType
AF = mybir.ActivationFunctionType
AX = mybir.AxisListType

D = 1024
DC = 8
FF = 2816
FH = 1408
NFB = 11
NE = 8
EPS = 1e-5


class Buf:
    __slots__ = ("t", "name", "lw", "rd", "rd_dma", "excl")

    registry = []

    def __init__(self, t, name):
        Buf.registry.append(self)
        self.t = t
        self.name = name
        self.excl = False
        self.lw = None
        self.rd = {}
        self.rd_dma = []

    def __getitem__(self, idx):
        return self.t[idx]


class Op:
    __slots__ = ("eng", "fn", "deps", "needed", "val", "dma_key", "idx")

    def __init__(self, eng, fn, dma_key):
        self.eng = eng
        self.fn = fn
        self.deps = []
        self.needed = False
        self.val = None
        self.dma_key = dma_key
        self.idx = None


class Sched:
    ENGS = ("pe", "act", "dve", "pool", "sp")

    _uid = 0

    def __init__(self, nc):
        Sched._uid += 1
        self.uid = Sched._uid
        self.nc = nc
        self.ops = {e: [] for e in self.ENGS}
        self.stack = contextlib.ExitStack()
        self.sems = {}
        self.dma_count = {}
        self.dma_ops = []
        self.sem_used = {}
        self.slot = {}
        self.base = {}

    sem_pool = {"sw": [], "hw": [], "eng": []}
    sem_stack = None
    sem_total = {}

    def sem(self, key, cls="eng"):
        if key not in self.sems:
            i = self.sem_used.get(cls, 0)
            self.sem_used[cls] = i + 1
            pool = Sched.sem_pool[cls]
            if i >= len(pool):
                pool.append(Sched.sem_stack.enter_context(self.nc.semaphore("sem_%s_%d" % (cls, i))))
            self.sems[key] = pool[i]
            self.slot[key] = (cls, i)
            self.base[key] = Sched.sem_total.get((cls, i), 0)
        return self.sems[key]

    def sbuf(self, name, shape, dtype):
        t = self.stack.enter_context(self.nc.sbuf_tensor("%s_u%d" % (name, self.uid), list(shape), dtype))
        return Buf(t, name)

    def psum(self, name, shape, dtype):
        t = self.stack.enter_context(self.nc.psum_tensor("%s_u%d" % (name, self.uid), list(shape), dtype))
        b = Buf(t, name)
        b.excl = True
        return b

    def dbuf(self, name):
        return Buf(None, name)

    def op(self, eng, fn, reads=(), writes=(), dma_key=None):
        o = Op(eng, fn, dma_key)
        deps = set()
        for b in reads:
            if b.lw is not None:
                deps.add(b.lw)
            if b.excl:
                for en, r in b.rd.items():
                    if en != eng:
                        deps.add(r)
        for b in writes:
            if b.lw is not None:
                deps.add(b.lw)
            for r in b.rd.values():
                deps.add(r)
            for r in b.rd_dma:
                deps.add(r)
        for d in deps:
            if d is o:
                continue
            if d.dma_key is None and d.eng == eng:
                if eng == "pe" or eng == "sp":
                    continue
                if not any((b.lw is d) for b in reads):
                    continue
            d.needed = True
            o.deps.append(d)
        for b in reads:
            if dma_key is not None:
                b.rd_dma.append(o)
            else:
                b.rd[eng] = o
        for b in writes:
            b.lw = o
            b.rd = {}
            b.rd_dma = []
        if dma_key is not None:
            self.sem(dma_key, "sw" if eng == "pool" else "hw")
            self.dma_count[dma_key] = self.dma_count.get(dma_key, 0) + 16
            o.val = self.base[dma_key] + self.dma_count[dma_key]
            o.needed = True
            self.dma_ops.append(o)
        self.ops[eng].append(o)
        return o

    def dma(self, eng, out_ap, in_ap, key, reads=(), writes=(), **kw):
        return self.op(eng, lambda e: e.dma_start(out=out_ap, in_=in_ap, **kw),
                       reads=reads, writes=writes, dma_key=key)

    def prepare(self):
        self.totals = {}
        for e in self.ENGS:
            c = 0
            if any(o.dma_key is None and o.needed for o in self.ops[e]):
                self.sem("E" + e)
                c0 = self.base["E" + e]
                for o in self.ops[e]:
                    if o.dma_key is None and o.needed:
                        c += 1
                        o.val = c0 + c
                self.totals[self.slot["E" + e]] = c0 + c
        for key, n in self.dma_count.items():
            self.totals[self.slot[key]] = self.base[key] + n
        fin = Op("sp", None, None)
        last = {}
        for o in self.dma_ops:
            last[o.dma_key] = o
        fin.deps = list(last.values())
        self.ops["sp"].append(fin)

    def run(self, e, eng):
        waited = {}
        for o in self.ops[e]:
            for d in o.deps:
                key = d.dma_key if d.dma_key is not None else "E" + d.eng
                if waited.get(key, 0) >= d.val:
                    continue
                waited[key] = d.val
                eng.wait_ge(self.sems[key], d.val)
            if o.fn is None:
                continue
            ins = o.fn(eng)
            if o.dma_key is not None:
                ins.then_inc(self.sems[o.dma_key], 16)
            elif o.needed:
                ins.then_inc(self.sems["E" + e], 1)

    def emit(self):
        nc = self.nc
        self.prepare()
        with nc.Block() as block:
            @block.tensor
            def _(eng):
                self.run("pe", eng)

            @block.scalar
            def _(eng):
                self.run("act", eng)

            @block.vector
            def _(eng):
                self.run("dve", eng)

            @block.gpsimd
            def _(eng):
                self.run("pool", eng)

            @block.sync
            def _(eng):
                self.run("sp", eng)
        Sched.sem_total.update(self.totals)
        self.stack.close()


def emit_either(nc, flag, regs, sa, sb):
    sa.prepare()
    sb.prepare()

    def body(e, eng):
        r = regs[e]
        eng.reg_load(r, flag[0:1, 0:1])
        with eng.If_eq(r, 1):
            sa.run(e, eng)
        with eng.Else():
            sb.run(e, eng)

    with nc.Block() as block:
        @block.tensor
        def _(eng):
            body("pe", eng)

        @block.scalar
        def _(eng):
            body("act", eng)

        @block.vector
        def _(eng):
            body("dve", eng)

        @block.gpsimd
        def _(eng):
            body("pool", eng)

        @block.sync
        def _(eng):
            body("sp", eng)
    sa.stack.close()
    sb.stack.close()


class Ctx:
    pass


def bcast_row(ap_row, nparts=128):
    return ap_row.partition_broadcast(nparts)


def emit_consts(cx):
    s = cx.s
    cx.ident = s.sbuf("ident", [128, 128], BF16)
    cx.identf = s.sbuf("identf", [128, 128], F32)
    s.op("pool", lambda e: e.memset(cx.identf[:], 1.0), writes=[cx.identf])
    s.op("pool", lambda e: e.affine_select(out=cx.identf[:], in_=cx.identf[:], pattern=[[-1, 128]],
                                           compare_op=ALU.is_equal, fill=0.0, base=0,
                                           channel_multiplier=1),
         reads=[cx.identf], writes=[cx.identf])
    s.op("dve", lambda e: e.tensor_copy(out=cx.ident[:], in_=cx.identf[:]),
         reads=[cx.identf], writes=[cx.ident])
    cx.epsc = s.sbuf("epsc", [128, 1], F32)
    s.op("pool", lambda e: e.memset(cx.epsc[:], EPS), writes=[cx.epsc])


def emit_norm_T(cx, x, gain_bc, hn, bank, hnT, ssq, rstd, junk, xdeps=None, evac_eng="act"):
    s = cx.s
    xd = [x] if xdeps is None else list(xdeps)
    psT = Bview(bank)
    s.op("act", lambda e: e.activation(out=junk[:], in_=x[:], func=AF.Square, scale=1.0 / 32.0,
                                       accum_out=ssq[:]),
         reads=xd, writes=[junk, ssq])
    s.op("act", lambda e: e.activation(out=rstd[:], in_=ssq[:], func=AF.Ln, bias=cx.epsc[:, 0:1]),
         reads=[ssq, cx.epsc], writes=[rstd])
    s.op("act", lambda e: e.activation(out=rstd[:], in_=rstd[:], func=AF.Exp, scale=-0.5),
         reads=[rstd], writes=[rstd])
    s.op("dve", lambda e: e.scalar_tensor_tensor(out=hn[:], in0=x[:], scalar=rstd[:, 0:1], in1=gain_bc[:],
                                                 op0=ALU.mult, op1=ALU.mult),
         reads=xd + [rstd, gain_bc], writes=[hn])
    for c in range(DC):
        s.op("pe", lambda e, c=c: e.transpose(out=psT[:, c * 128:(c + 1) * 128],
                                              in_=hn[:, c * 128:(c + 1) * 128], identity=cx.ident[:]),
             reads=[hn, cx.ident], writes=[bank])
    if evac_eng == "act":
        s.op("act", lambda e: e.copy(out=hnT[:].rearrange("p c t -> p (c t)"), in_=psT[:]),
             reads=[bank], writes=[hnT])
    else:
        s.op("dve", lambda e: e.tensor_copy(out=hnT[:].rearrange("p c t -> p (c t)"), in_=psT[:]),
             reads=[bank], writes=[hnT])


class Bview:
    def __init__(self, b):
        self.b = b

    def __getitem__(self, idx):
        return self.b.t[:].bitcast(BF16)[idx]


def sub(parent, name):
    return Buf(parent.t, name)


def emit_psum(cx):
    cx.ps = [cx.s.psum("ps%d" % i, [128, 512], F32) for i in range(8)]


class WSet:
    pass


def alloc_wset(cx, k):
    s = cx.s
    w = WSet()
    w.wg = s.sbuf("wg%d" % k, [128, DC, FH], BF16)
    w.wu = s.sbuf("wu%d" % k, [128, DC, FH], BF16)
    w.wd = s.sbuf("wd%d" % k, [128, NFB, D], BF16)
    w.wg_c = [sub(w.wg, "wg%d_%d" % (k, c)) for c in range(DC)]
    w.wu_c = [sub(w.wu, "wu%d_%d" % (k, c)) for c in range(DC)]
    w.wd_c = [sub(w.wd, "wd%d_%d" % (k, c)) for c in range(NFB)]
    return w


def ffn_weight_loads(cx, w, Wg, Wu, Wd, half):
    s = cx.s
    out = []
    f0 = half * FH
    for c in range(DC):
        out.append(lambda c=c: s.dma("pool", w.wg.t[:, c, :], Wg[c * 128:(c + 1) * 128, f0:f0 + FH],
                                     w.wg_c[c].name, writes=[w.wg_c[c]]))
        out.append(lambda c=c: s.dma("pool", w.wu.t[:, c, :], Wu[c * 128:(c + 1) * 128, f0:f0 + FH],
                                     w.wu_c[c].name, writes=[w.wu_c[c]]))
    for fb in range(NFB):
        out.append(lambda fb=fb: s.dma("pool", w.wd.t[:, fb, :], Wd[f0 + fb * 128:f0 + (fb + 1) * 128, :],
                                       w.wd_c[fb].name, writes=[w.wd_c[fb]]))
    return out


def alloc_ffn_bufs(cx, TB):
    s = cx.s
    cx.TB = TB
    cx.hnTb = [s.sbuf("hnTb%d" % i, [128, DC, TB], BF16) for i in range(2)]
    cx.hT = [s.sbuf("hT%d" % i, [128, NFB, TB], BF16) for i in range(2)]
    cx.sg = [s.sbuf("sg%d" % i, [128, TB], BF16) for i in range(2)]
    cx.stg = [s.sbuf("stg%d" % i, [128, D], F32) for i in range(3)]
    cx.stgA = [sub(b, b.name + "A") for b in cx.stg]
    cx.stgB = [sub(b, b.name + "B") for b in cx.stg]


def emit_ffn_pass(cx, S, hnT_ap, hnT_db, w, cw, cw_col, acc_ap, acc_db, pre=(), ctr=None,
                  loader=None, sinker=None, prefetch=None):
    s = cx.s
    TB = cx.TB
    nblk = S // TB
    ntt = TB // 128
    hview = hnT_ap.rearrange("(c p) t -> p c t", p=128) if hnT_ap is not None else None
    pre = list(pre)
    per_blk = (len(pre) + nblk - 1) // nblk if pre else 0
    if ctr is None:
        ctr = {"blk": 0, "tile": 0, "fb": 0}

    def gu(b):
        k = ctr["blk"] + b
        hb = cx.hnTb[k % 2]
        if loader is not None:
            loader(b, hb)
        else:
            s.dma("sp", hb[:], hview[:, :, b * TB:(b + 1) * TB], hb.name,
                  reads=[hnT_db[b * ntt + i] for i in range(ntt)], writes=[hb])
        hT = cx.hT[k % 2]
        for fb in range(NFB):
            q = ctr["fb"]
            ctr["fb"] += 1
            pg = cx.ps[(2 * q) % 4]
            pu = cx.ps[(2 * q + 1) % 4]
            sg = cx.sg[q % 2]
            for c in range(DC):
                s.op("pe", lambda e, c=c, pg=pg, fb=fb: e.matmul(
                    pg[:, :TB], lhsT=w.wg.t[:, c, fb * 128:(fb + 1) * 128], rhs=hb[:, c, :],
                    start=(c == 0), stop=(c == DC - 1)), reads=[w.wg_c[c], hb], writes=[pg])
            for c in range(DC):
                s.op("pe", lambda e, c=c, pu=pu, fb=fb: e.matmul(
                    pu[:, :TB], lhsT=w.wu.t[:, c, fb * 128:(fb + 1) * 128], rhs=hb[:, c, :],
                    start=(c == 0), stop=(c == DC - 1)), reads=[w.wu_c[c], hb], writes=[pu])
            s.op("act", lambda e, pg=pg, sg=sg: e.activation(out=sg[:], in_=pg[:, :TB], func=AF.Silu),
                 reads=[pg], writes=[sg])
            s.op("dve", lambda e, pu=pu, sg=sg, fb=fb: e.tensor_tensor(
                out=hT[:, fb, :], in0=pu[:, :TB], in1=sg[:], op=ALU.mult),
                reads=[pu, sg], writes=[hT])

    def down(b):
        k = ctr["blk"] + b
        hT = cx.hT[k % 2]
        for tt in range(ntt):
            tile = b * ntt + tt
            q = ctr["tile"]
            ctr["tile"] += 1
            pd = (cx.ps[4 + (q % 2) * 2], cx.ps[5 + (q % 2) * 2])
            for half in range(2):
                for fb in range(NFB):
                    s.op("pe", lambda e, half=half, fb=fb, tt=tt, pd=pd: e.matmul(
                        pd[half][:], lhsT=hT[:, fb, tt * 128:(tt + 1) * 128],
                        rhs=w.wd.t[:, fb, half * 512:(half + 1) * 512],
                        start=(fb == 0), stop=(fb == NFB - 1)),
                        reads=[hT, w.wd_c[fb]], writes=[pd[half]])
            j = q % 3
            stg, sa, sb_ = cx.stg[j], cx.stgA[j], cx.stgB[j]
            if sinker is not None:
                sinker(tile, pd, stg, sa, sb_)
                continue
            if cw is None:
                s.op("act", lambda e, stg=stg, pd=pd: e.copy(out=stg[:, 0:512], in_=pd[0][:]),
                     reads=[pd[0]], writes=[sa])
                s.op("dve", lambda e, stg=stg, pd=pd: e.tensor_copy(out=stg[:, 512:1024], in_=pd[1][:]),
                     reads=[pd[1]], writes=[sb_])
            else:
                col = cw_col(tile)
                s.op("act", lambda e, stg=stg, col=col, pd=pd: e.activation(
                    out=stg[:, 0:512], in_=pd[0][:], func=AF.Copy, scale=cw[:, col:col + 1]),
                    reads=[pd[0], cw], writes=[sa])
                s.op("dve", lambda e, stg=stg, col=col, pd=pd: e.tensor_scalar(
                    out=stg[:, 512:1024], in0=pd[1][:], scalar1=cw[:, col:col + 1], scalar2=None,
                    op0=ALU.mult), reads=[pd[1], cw], writes=[sb_])
            s.dma("pool", acc_ap[tile * 128:(tile + 1) * 128, :], stg[:], stg.name,
                  reads=[sa, sb_, acc_db[tile]], writes=[acc_db[tile]], accum_op=ALU.add)

    if prefetch is not None:
        prefetch(0)
        if nblk > 1:
            prefetch(1)
    gu(0)
    for b in range(nblk):
        if prefetch is not None and b + 2 < nblk:
            prefetch(b + 2)
        if b + 1 < nblk:
            gu(b + 1)
        down(b)
        for _ in range(per_blk):
            if pre:
                pre.pop(0)()
    while pre:
        pre.pop(0)()
    ctr["blk"] += nblk
    return ctr


GQ, GK, GV, GR, GLF, GLB = 0, 512, 1024, 2048, 3072, 3088
GW = 3104
NH = 4
import os as _os
_GSTOP = int(_os.environ.get('GSTOP', '-1'))


def tri(cx, name, val, pattern, cm, cmp, dtype=F32, ncols=128):
    s = cx.s
    b = s.sbuf(name, [128, ncols], F32)
    s.op("pool", lambda e: e.memset(b[:], val), writes=[b])
    s.op("pool", lambda e: e.affine_select(out=b[:, 0:128], in_=b[:, 0:128], pattern=[[pattern, 128]],
                                           compare_op=cmp, fill=0.0, base=0, channel_multiplier=cm),
         reads=[b], writes=[b])
    return b


def alloc_gla(cx, P):
    s = cx.s
    g = cx.g = Ctx()
    c16 = -1.0 / 16.0
    g.Rf = s.sbuf("Rf", [128, 257], F32)
    g.Rb = s.sbuf("Rb", [128, 257], F32)
    uf = tri(cx, "Uf", c16, 1, -1, ALU.is_ge)
    ub = tri(cx, "Ub", c16, -1, 1, ALU.is_ge)
    for R, U, ref in ((g.Rf, uf, 64), (g.Rb, ub, 63)):
        s.op("pool", lambda e, R=R: e.memset(R[:, 256:257], c16), writes=[R])
        s.op("dve", lambda e, R=R, U=U: e.tensor_copy(out=R[:, 0:128], in_=U[:]), reads=[U, R], writes=[R])
        s.op("dve", lambda e, R=R, U=U, ref=ref: e.tensor_scalar(
            out=R[:, 128:256], in0=U[:], scalar1=U[:, ref:ref + 1], scalar2=None, op0=ALU.subtract),
            reads=[U, R], writes=[R])
    g.Mkf = tri(cx, "Mkf", c16, -1, 1, ALU.is_gt)
    g.Mkb = tri(cx, "Mkb", c16, 1, -1, ALU.is_gt)
    g.maskf = tri(cx, "maskf", 1.0, 1, -1, ALU.is_ge)
    g.maskb = tri(cx, "maskb", 1.0, -1, 1, ALU.is_gt)
    g.Win = s.sbuf("Win", [128, DC, GW], BF16)
    g.Win_c = [sub(g.Win, "Win_%d" % c) for c in range(DC)]
    for c in range(DC):
        s.dma("pool", g.Win.t[:, c, :], P["gla_in"][c * 128:(c + 1) * 128, :], g.Win_c[c].name,
              writes=[g.Win_c[c]])
    g.Wout = s.sbuf("Wout", [128, DC, D], BF16)
    g.Wout_c = [sub(g.Wout, "Wout_%d" % c) for c in range(DC)]
    for c in range(DC):
        s.dma("pool", g.Wout.t[:, c, :], P["gla_out"][c * 128:(c + 1) * 128, :], g.Wout_c[c].name,
              writes=[g.Wout_c[c]])
    g.wga = []
    for d, (wk, bk) in enumerate((("gw_f", "gb_f"), ("gw_b", "gb_b"))):
        wa = s.sbuf("wga%d" % d, [17, 512], F32)
        wa1 = sub(wa, "wga%d_b" % d)
        s.dma("sp", wa[0:16, :], P[wk], wa.name, writes=[wa])
        s.dma("sp", wa[16:17, :], P[bk].rearrange("(o n) -> o n", o=1), wa1.name, writes=[wa1])
        g.wga.append((wa, wa1))
    g.gain1 = s.sbuf("gain1", [128, D], F32)
    s.dma("sp", g.gain1[:], bcast_row(P["mix_norm0"]), "gain1", writes=[g.gain1])
    g.gain2 = s.sbuf("gain2", [128, D], F32)
    s.dma("sp", g.gain2[:], bcast_row(P["ffn_norm0"]), "gain2", writes=[g.gain2])
    g.hgain = s.sbuf("hgain", [128, D], F32)
    s.dma("sp", g.hgain[:], bcast_row(P["gla_hn"]), "hgain", writes=[g.hgain])
    g.one_c = s.sbuf("one_c", [128, 1], F32)
    s.op("pool", lambda e: e.memset(g.one_c[:], 1.0), writes=[g.one_c])
    g.eps256 = s.sbuf("eps256", [128, 1], F32)
    s.op("pool", lambda e: e.memset(g.eps256[:], EPS), writes=[g.eps256])
    g.x = s.sbuf("gx", [128, D], F32)
    g.hn = s.sbuf("ghn", [128, D], BF16)
    g.hnT = s.sbuf("ghnT", [128, DC, 128], BF16)
    g.junk = s.sbuf("gjunk", [128, D], F32)
    g.ssq = s.sbuf("gssq", [128, 1], F32)
    g.rstd = s.sbuf("grstd", [128, 1], F32)
    g.vbf = s.sbuf("gvbf", [128, D], BF16)
    g.er = s.sbuf("ger", [128, D], F32)
    g.rbf = s.sbuf("grbf", [128, D], BF16)
    g.lrT = []
    for d in range(2):
        b = s.sbuf("glrT%d" % d, [17, 128], F32)
        s.op("pool", lambda e, b=b: e.memset(b[:], 1.0), writes=[b])
        g.lrT.append(b)
    g.la = [s.sbuf("gla%d" % d, [128, 512], F32) for d in range(2)]
    g.Ekd = [s.sbuf("gEkd%d" % d, [128, 512], F32) for d in range(2)]
    g.kd = [s.sbuf("gkd%d" % d, [128, 512], BF16) for d in range(2)]
    g.Eall = [[s.sbuf("gEall%d_%d" % (d, h), [128, 257], F32) for h in range(NH)] for d in range(2)]
    g.E2 = [[s.sbuf("gE2%d_%d" % (d, h), [128, 128], F32) for h in range(NH)] for d in range(2)]
    g.qe = [[s.sbuf("gqe%d_%d" % (d, h), [128, 128], BF16) for h in range(NH)] for d in range(2)]
    g.qb = [[s.sbuf("gqb%d_%d" % (d, h), [128, 128], BF16) for h in range(NH)] for d in range(2)]
    g.ke = [[s.sbuf("gke%d_%d" % (d, h), [128, 128], BF16) for h in range(NH)] for d in range(2)]
    g.PT = [[s.sbuf("gPT%d_%d" % (d, h), [128, 128], BF16) for h in range(NH)] for d in range(2)]
    g.dec = [s.sbuf("gdec%d" % d, [128, NH], F32) for d in range(2)]
    g.S = [[s.sbuf("gS%d_%d" % (d, h), [128, 256], F32) for h in range(NH)] for d in range(2)]
    for d in range(2):
        for h in range(NH):
            s.op("pool", lambda e, b=g.S[d][h]: e.memset(b[:], 0.0), writes=[g.S[d][h]])
    g.Sbf = [s.sbuf("gSbf%d" % d, [128, NH, 256], BF16) for d in range(2)]
    g.Sbf_h = [[sub(g.Sbf[d], "gSbf%d_%d" % (d, h)) for h in range(NH)] for d in range(2)]
    for d in range(2):
        s.op("pool", lambda e, b=g.Sbf[d]: e.memset(b[:], 0.0), writes=[g.Sbf[d]] + g.Sbf_h[d])
    g.ssq4 = s.sbuf("gssq4", [128, NH], F32)
    g.rstd4 = s.sbuf("grstd4", [128, NH], F32)
    g.og = s.sbuf("gog", [128, D], F32)
    g.og_h = [sub(g.og, "gog_%d" % h) for h in range(NH)]
    g.sig = s.sbuf("gsig", [128, D], F32)
    g.gated = s.sbuf("ggated", [128, D], BF16)
    g.gT = s.sbuf("ggT", [128, DC, 128], BF16)
    g.h1 = s.sbuf("gh1", [128, D], F32)
    g.h1A = sub(g.h1, "gh1A")
    g.h1B = sub(g.h1, "gh1B")
    g.hn2 = s.sbuf("ghn2", [128, D], BF16)
    g.hn2T = s.sbuf("ghn2T", [128, DC, 128], BF16)


def emit_gla_tile(cx, t, full, x_ap, Sb_ap, Sb_db, h1_ap, h1_db, hnT_ap, hnT_db):
    s = cx.s
    g = cx.g
    ps = cx.ps
    W = g.Win.t
    rows = slice(t * 128, (t + 1) * 128)
    s.dma("sp", g.x[:], x_ap[rows, :], "gx", writes=[g.x])
    emit_norm_T(cx, g.x, g.gain1, g.hn, ps[0], g.hnT, g.ssq, g.rstd, g.junk)
    hnT = g.hnT
    dirs = (0, 1) if full else (1,)
    upd_dirs = (0,) if full else (1,)

    def proj_tok(bank, col0, n=512):
        for c in range(DC):
            s.op("pe", lambda e, c=c: e.matmul(bank[:, 0:n], lhsT=hnT[:, c, :], rhs=W[:, c, col0:col0 + n],
                                               start=(c == 0), stop=(c == DC - 1)),
                 reads=[hnT, g.Win_c[c]], writes=[bank])

    def proj_feat(bank, bcol, col0, m):
        for c in range(DC):
            s.op("pe", lambda e, c=c: e.matmul(bank[0:m, bcol:bcol + 128], lhsT=W[:, c, col0:col0 + m],
                                               rhs=hnT[:, c, :], start=(c == 0), stop=(c == DC - 1)),
                 reads=[hnT, g.Win_c[c]], writes=[bank])

    proj_tok(ps[1], GK)
    proj_tok(ps[2], GV)
    proj_tok(ps[3], GV + 512)
    if full:
        proj_tok(ps[4], GR)
        proj_tok(ps[5], GR + 512)
        for h in range(NH):
            proj_feat(ps[6], h * 128, GQ + h * 128, 128)
        for h in range(NH):
            proj_feat(ps[7], h * 128, GK + h * 128, 128)
    for d in dirs:
        proj_feat(ps[0], d * 128, GLF + 16 * d, 16)
    if _GSTOP == 0 and full:
        return
    s.op("act", lambda e: e.copy(out=g.vbf[:, 0:512], in_=ps[2][:]), reads=[ps[2]], writes=[g.vbf])
    s.op("act", lambda e: e.copy(out=g.vbf[:, 512:1024], in_=ps[3][:]), reads=[ps[3], g.vbf], writes=[g.vbf])
    if full:
        for hh in range(2):
            sl = slice(hh * 512, (hh + 1) * 512)
            s.op("act", lambda e, hh=hh, sl=sl: e.activation(out=g.er[:, sl], in_=ps[4 + hh][:], func=AF.Exp,
                                                             scale=-1.0),
                 reads=[ps[4 + hh], g.er], writes=[g.er])
            s.op("dve", lambda e, hh=hh, sl=sl: e.tensor_copy(out=g.rbf[:, sl], in_=ps[4 + hh][:]),
                 reads=[ps[4 + hh], g.rbf], writes=[g.rbf])
        s.op("act", lambda e: e.activation(out=g.er[:], in_=g.er[:], func=AF.Ln, bias=g.one_c[:, 0:1]),
             reads=[g.er, g.one_c], writes=[g.er])
        s.op("act", lambda e: e.activation(out=g.sig[:], in_=g.er[:], func=AF.Exp, scale=-1.0),
             reads=[g.er], writes=[g.sig])
        s.op("dve", lambda e: e.tensor_tensor(out=g.sig[:], in0=g.sig[:], in1=g.rbf[:], op=ALU.mult),
             reads=[g.sig, g.rbf], writes=[g.sig])
    for d in dirs:
        s.op("dve", lambda e, d=d: e.tensor_copy(out=g.lrT[d][0:16, :], in_=ps[0][0:16, d * 128:(d + 1) * 128]),
             reads=[ps[0], g.lrT[d]], writes=[g.lrT[d]])
    if _GSTOP == 1 and full:
        return
    for d in dirs:
        zb = ps[2 + d]
        s.op("pe", lambda e, d=d, zb=zb: e.matmul(zb[:], lhsT=g.lrT[d][:], rhs=g.wga[d][0][:], start=True, stop=True),
             reads=[g.lrT[d], g.wga[d][0], g.wga[d][1]], writes=[zb])
        s.op("act", lambda e, d=d, zb=zb: e.activation(out=g.la[d][:], in_=zb[:], func=AF.Exp, scale=-1.0),
             reads=[zb], writes=[g.la[d]])
        s.op("act", lambda e, d=d: e.activation(out=g.la[d][:], in_=g.la[d][:], func=AF.Ln, bias=g.one_c[:, 0:1]),
             reads=[g.la[d], g.one_c], writes=[g.la[d]])
    if _GSTOP == 2 and full:
        return
    for d in upd_dirs:
        Mk = g.Mkf if d == 0 else g.Mkb
        xb = ps[2 + d]
        s.op("pe", lambda e, d=d, Mk=Mk, xb=xb: e.matmul(xb[:], lhsT=Mk[:], rhs=g.la[d][:], start=True, stop=True),
             reads=[Mk, g.la[d]], writes=[xb])
        s.op("act", lambda e, d=d, xb=xb: e.activation(out=g.Ekd[d][:], in_=xb[:], func=AF.Exp),
             reads=[xb], writes=[g.Ekd[d]])
        s.op("dve", lambda e, d=d: e.tensor_tensor(out=g.kd[d][:], in0=ps[1][:], in1=g.Ekd[d][:], op=ALU.mult),
             reads=[ps[1], g.Ekd[d]], writes=[g.kd[d]])
    cnt = 0
    for d in dirs:
        R = g.Rf if d == 0 else g.Rb
        for h in range(NH):
            cb = ps[4 + (cnt % 2)]
            cnt += 1
            if full:
                s.op("pe", lambda e, d=d, h=h, cb=cb, R=R: e.matmul(
                    cb[:, 0:257], lhsT=g.la[d][:, h * 128:(h + 1) * 128], rhs=R[:], start=True, stop=True),
                    reads=[g.la[d], R], writes=[cb])
                s.op("act", lambda e, d=d, h=h, cb=cb: e.activation(out=g.Eall[d][h][:], in_=cb[:, 0:257], func=AF.Exp),
                     reads=[cb], writes=[g.Eall[d][h]])
                s.op("act", lambda e, d=d, h=h, cb=cb: e.activation(out=g.E2[d][h][:], in_=cb[:, 128:256], func=AF.Exp,
                                                                   scale=-1.0),
                     reads=[cb], writes=[g.E2[d][h]])
                s.op("dve", lambda e, d=d, h=h: e.tensor_copy(out=g.dec[d][:, h:h + 1], in_=g.Eall[d][h][:, 256:257]),
                     reads=[g.Eall[d][h], g.dec[d]], writes=[g.dec[d]])
            else:
                s.op("pe", lambda e, d=d, h=h, cb=cb, R=R: e.matmul(
                    cb[:, 0:1], lhsT=g.la[d][:, h * 128:(h + 1) * 128], rhs=R[:, 256:257], start=True, stop=True),
                    reads=[g.la[d], R], writes=[cb])
                s.op("act", lambda e, d=d, h=h, cb=cb: e.activation(out=g.dec[d][:, h:h + 1], in_=cb[:, 0:1], func=AF.Exp),
                     reads=[cb, g.dec[d]], writes=[g.dec[d]])
    if full:
        if _GSTOP == 3 and full:
            return
        sc = float(128 ** -0.5)
        for d in dirs:
            for h in range(NH):
                qps = ps[6][:, h * 128:(h + 1) * 128]
                kps = ps[7][:, h * 128:(h + 1) * 128]
                E = g.Eall[d][h]
                s.op("dve", lambda e, d=d, h=h, qps=qps, E=E: e.scalar_tensor_tensor(
                    out=g.qb[d][h][:], in0=qps, scalar=sc, in1=E[:, 0:128], op0=ALU.mult, op1=ALU.mult),
                    reads=[ps[6], E], writes=[g.qb[d][h]])
                s.op("dve", lambda e, d=d, h=h, qps=qps, E=E: e.scalar_tensor_tensor(
                    out=g.qe[d][h][:], in0=qps, scalar=sc, in1=E[:, 128:256], op0=ALU.mult, op1=ALU.mult),
                    reads=[ps[6], E], writes=[g.qe[d][h]])
                s.op("dve", lambda e, d=d, h=h, kps=kps: e.tensor_tensor(
                    out=g.ke[d][h][:], in0=kps, in1=g.E2[d][h][:], op=ALU.mult),
                    reads=[ps[7], g.E2[d][h]], writes=[g.ke[d][h]])
        s.dma("sp", g.Sbf[1][:].rearrange("p h v -> p (h v)"), Sb_ap[t], "gSbf1", reads=[Sb_db[t]], writes=[g.Sbf[1]] + g.Sbf_h[1])
        if _GSTOP == 4 and full:
            return
        for d in dirs:
            mask = g.maskf if d == 0 else g.maskb
            sb_ = ps[2 + d]
            for h in range(NH):
                s.op("pe", lambda e, d=d, h=h, sb_=sb_: e.matmul(
                    sb_[:, h * 128:(h + 1) * 128], lhsT=g.ke[d][h][:], rhs=g.qe[d][h][:], start=True, stop=True),
                    reads=[g.ke[d][h], g.qe[d][h]], writes=[sb_])
            for h in range(NH):
                s.op("dve", lambda e, d=d, h=h, sb_=sb_, mask=mask: e.tensor_tensor(
                    out=g.PT[d][h][:], in0=sb_[:, h * 128:(h + 1) * 128], in1=mask[:], op=ALU.mult),
                    reads=[sb_, mask], writes=[g.PT[d][h]])
        if _GSTOP == 5 and full:
            return
        for h in range(NH):
            ob = ps[4 + h // 2]
            oc = slice((h % 2) * 256, (h % 2) * 256 + 256)
            vs = g.vbf[:, h * 256:(h + 1) * 256]
            s.op("pe", lambda e, h=h, ob=ob, oc=oc, vs=vs: e.matmul(ob[:, oc], lhsT=g.PT[0][h][:], rhs=vs,
                                                                   start=True, stop=False),
                 reads=[g.PT[0][h], g.vbf], writes=[ob])
            s.op("pe", lambda e, h=h, ob=ob, oc=oc, vs=vs: e.matmul(ob[:, oc], lhsT=g.PT[1][h][:], rhs=vs,
                                                                   start=False, stop=False),
                 reads=[g.PT[1][h], g.vbf], writes=[ob])
            s.op("pe", lambda e, h=h, ob=ob, oc=oc: e.matmul(ob[:, oc], lhsT=g.qb[0][h][:], rhs=g.Sbf[0][:, h, :],
                                                            start=False, stop=False),
                 reads=[g.qb[0][h], g.Sbf_h[0][h]], writes=[ob])
            s.op("pe", lambda e, h=h, ob=ob, oc=oc: e.matmul(ob[:, oc], lhsT=g.qb[1][h][:], rhs=g.Sbf[1][:, h, :],
                                                            start=False, stop=True),
                 reads=[g.qb[1][h], g.Sbf_h[1][h]], writes=[ob])
        if _GSTOP == 6 and full:
            return
        for h in range(NH):
            ob = ps[4 + h // 2]
            oc = slice((h % 2) * 256, (h % 2) * 256 + 256)
            s.op("act", lambda e, h=h, ob=ob, oc=oc: e.activation(
                out=g.junk[:, h * 256:(h + 1) * 256], in_=ob[:, oc], func=AF.Square, scale=1.0 / 16.0,
                accum_out=g.ssq4[:, h:h + 1]), reads=[ob, g.junk, g.ssq4], writes=[g.junk, g.ssq4])
        s.op("act", lambda e: e.activation(out=g.rstd4[:], in_=g.ssq4[:], func=AF.Ln, bias=g.eps256[:, 0:1]),
             reads=[g.ssq4, g.eps256], writes=[g.rstd4])
        s.op("act", lambda e: e.activation(out=g.rstd4[:], in_=g.rstd4[:], func=AF.Exp, scale=-0.5),
             reads=[g.rstd4], writes=[g.rstd4])
        for h in range(NH):
            ob = ps[4 + h // 2]
            oc = slice((h % 2) * 256, (h % 2) * 256 + 256)
            hs = slice(h * 256, (h + 1) * 256)
            s.op("dve", lambda e, h=h, ob=ob, oc=oc, hs=hs: e.scalar_tensor_tensor(
                out=g.og[:, hs], in0=ob[:, oc], scalar=g.rstd4[:, h:h + 1], in1=g.hgain[:, hs],
                op0=ALU.mult, op1=ALU.mult), reads=[ob, g.rstd4, g.hgain], writes=[g.og_h[h]])
        s.op("dve", lambda e: e.tensor_tensor(out=g.gated[:], in0=g.og[:], in1=g.sig[:], op=ALU.mult),
             reads=g.og_h + [g.sig], writes=[g.gated])
        if _GSTOP == 7 and full:
            return
        psT = Bview(ps[0])
        for c in range(DC):
            s.op("pe", lambda e, c=c: e.transpose(out=psT[:, c * 128:(c + 1) * 128],
                                                  in_=g.gated[:, c * 128:(c + 1) * 128], identity=cx.ident[:]),
                 reads=[g.gated, cx.ident], writes=[ps[0]])
        s.op("act", lambda e: e.copy(out=g.gT[:].rearrange("p c t -> p (c t)"), in_=psT[:]),
             reads=[ps[0]], writes=[g.gT])
        for hh in range(2):
            yb = ps[2 + hh]
            for c in range(DC):
                s.op("pe", lambda e, c=c, hh=hh, yb=yb: e.matmul(
                    yb[:], lhsT=g.gT[:, c, :], rhs=g.Wout.t[:, c, hh * 512:(hh + 1) * 512],
                    start=(c == 0), stop=(c == DC - 1)), reads=[g.gT, g.Wout_c[c]], writes=[yb])
        s.op("dve", lambda e: e.tensor_tensor(out=g.h1[:, 0:512], in0=ps[2][:], in1=g.x[:, 0:512], op=ALU.add),
             reads=[ps[2], g.x], writes=[g.h1A])
        s.op("dve", lambda e: e.tensor_tensor(out=g.h1[:, 512:1024], in0=ps[3][:], in1=g.x[:, 512:1024], op=ALU.add),
             reads=[ps[3], g.x], writes=[g.h1B])
        s.dma("sp", h1_ap[rows, :], g.h1[:], "gh1", reads=[g.h1A, g.h1B], writes=[h1_db[t]])
        emit_norm_T(cx, g.h1, g.gain2, g.hn2, ps[0], g.hn2T, g.ssq, g.rstd, g.junk, xdeps=[g.h1A, g.h1B])
        s.dma("sp", hnT_ap.rearrange("(c p) t -> p c t", p=128)[:, :, rows], g.hn2T[:], "ghn2T",
              reads=[g.hn2T], writes=[hnT_db[t]])
    else:
        s.dma("sp", Sb_ap[t], g.Sbf[1][:].rearrange("p h v -> p (h v)"), "gSbf1", reads=g.Sbf_h[1], writes=[Sb_db[t]])
    if _GSTOP == 8 and full:
        return
    for d in upd_dirs:
        for h in range(NH):
            ub = ps[4 + h // 2] if full else ps[6 + h // 2]
            uc = slice((h % 2) * 256, (h % 2) * 256 + 256)
            s.op("pe", lambda e, d=d, h=h, ub=ub, uc=uc: e.matmul(
                ub[:, uc], lhsT=g.kd[d][:, h * 128:(h + 1) * 128], rhs=g.vbf[:, h * 256:(h + 1) * 256],
                start=True, stop=True), reads=[g.kd[d], g.vbf], writes=[ub])
            s.op("dve", lambda e, d=d, h=h, ub=ub, uc=uc: e.scalar_tensor_tensor(
                out=g.S[d][h][:], in0=g.S[d][h][:], scalar=g.dec[d][:, h:h + 1], in1=ub[:, uc],
                op0=ALU.mult, op1=ALU.add), reads=[g.S[d][h], g.dec[d], ub], writes=[g.S[d][h]])
            if (d == 0) or (not full):
                s.op("pool", lambda e, d=d, h=h: e.tensor_copy(out=g.Sbf[d][:, h, :], in_=g.S[d][h][:]),
                     reads=[g.S[d][h]], writes=[g.Sbf_h[d][h]])


NQH, NKV, HD = 16, 4, 64
_PH = int(_os.environ.get('PH', '5'))
NEG = -1.0e30


def alloc_swa(cx, P, NT):
    s = cx.s
    a = cx.a = Ctx()
    a.W = s.sbuf("aW", [128, DC, 1536], BF16)
    a.W_c = [sub(a.W, "aW_%d" % c) for c in range(DC)]
    for c in range(DC):
        s.dma("pool", a.W.t[:, c, :], P["swa_qkv"][c * 128:(c + 1) * 128, :], a.W_c[c].name, writes=[a.W_c[c]])
    a.Wo = s.sbuf("aWo", [128, DC, D], BF16)
    a.Wo_c = [sub(a.Wo, "aWo_%d" % c) for c in range(DC)]
    for c in range(DC):
        s.dma("pool", a.Wo.t[:, c, :], P["swa_out"][c * 128:(c + 1) * 128, :], a.Wo_c[c].name, writes=[a.Wo_c[c]])
    a.Wr = s.sbuf("aWr", [128, DC, NE], BF16)
    s.dma("pool", a.Wr[:], P["router"].rearrange("(c p) e -> p c e", p=128), "aWr", writes=[a.Wr])
    a.brow = s.sbuf("abrow", [1, 1536], F32)
    s.dma("sp", a.brow[:], P["swa_qkv_b"].rearrange("(o n) -> o n", o=1), "abrow", writes=[a.brow])
    a.ones = s.sbuf("aones", [1, 128], F32)
    s.op("pool", lambda e: e.memset(a.ones[:], 1.0), writes=[a.ones])
    a.bout = s.sbuf("about", [128, D], F32)
    s.dma("sp", a.bout[:], bcast_row(P["swa_out_b"]), "about", writes=[a.bout])
    a.sink = s.sbuf("asink", [128, NQH], F32)
    s.dma("sp", a.sink[:], bcast_row(P["sinks"]), "asink", writes=[a.sink])
    a.gain3 = s.sbuf("again3", [128, D], F32)
    s.dma("sp", a.gain3[:], bcast_row(P["mix_norm1"]), "again3", writes=[a.gain3])
    a.gain4 = s.sbuf("again4", [128, D], F32)
    s.dma("sp", a.gain4[:], bcast_row(P["ffn_norm1"]), "again4", writes=[a.gain4])
    a.dist = s.sbuf("adist", [128, 384], F32)
    a.disti = s.sbuf("adisti", [128, 384], I32)
    s.op("pool", lambda e: e.iota(a.disti[:], pattern=[[-1, 384]], base=128, channel_multiplier=1),
         writes=[a.disti])
    s.op("dve", lambda e: e.tensor_copy(out=a.dist[:], in_=a.disti[:]), reads=[a.disti], writes=[a.dist])
    a.ndist = s.sbuf("andist", [128, 384], F32)
    s.op("dve", lambda e: e.tensor_scalar(out=a.ndist[:], in0=a.dist[:], scalar1=-1.0, scalar2=None, op0=ALU.mult),
         reads=[a.dist], writes=[a.ndist])
    s.op("dve", lambda e: e.tensor_tensor(out=a.dist[:], in0=a.dist[:], in1=a.ndist[:], op=ALU.max),
         reads=[a.dist, a.ndist], writes=[a.dist])
    a.wmask = s.sbuf("awmask", [128, 384], F32)
    s.op("dve", lambda e: e.tensor_scalar(out=a.wmask[:], in0=a.dist[:], scalar1=128.0, scalar2=NEG,
                                          op0=ALU.is_gt, op1=ALU.mult), reads=[a.dist], writes=[a.wmask])
    a.bias = []
    for h in range(NQH):
        b = s.sbuf("abias%d" % h, [128, 384], F32)
        slope = float(np.float32(2.0 ** (-8.0 * (h + 1) / NQH)))
        s.op("dve", lambda e, b=b, slope=slope: e.scalar_tensor_tensor(
            out=b[:], in0=a.dist[:], scalar=-slope, in1=a.wmask[:], op0=ALU.mult, op1=ALU.add),
            reads=[a.dist, a.wmask], writes=[b])
        a.bias.append(b)
    a.x = [s.sbuf("ax%d" % i, [128, D], F32) for i in range(3)]
    a.qT = [s.sbuf("aqT%d" % i, [64, NQH, 128], BF16) for i in range(3)]
    a.kT = [s.sbuf("akT%d" % i, [64, NKV, 128], BF16) for i in range(4)]
    a.v = [s.sbuf("av%d" % i, [128, NKV * HD], BF16) for i in range(4)]
    a.qkv = s.sbuf("aqkv", [128, 1536], BF16)
    a.qkvA = sub(a.qkv, "aqkvA")
    a.qkvB = sub(a.qkv, "aqkvB")
    a.qkvC = sub(a.qkv, "aqkvC")
    a.hn = s.sbuf("ahn", [128, D], BF16)
    a.hnT = s.sbuf("ahnT", [128, DC, 128], BF16)
    a.junk = s.sbuf("ajunk", [128, D], F32)
    a.ssq = s.sbuf("assq", [128, 1], F32)
    a.rstd = s.sbuf("arstd", [128, 1], F32)
    a.sc = [s.sbuf("asc%d" % i, [128, 384], F32) for i in range(2)]
    a.p = [s.sbuf("ap%d" % i, [128, 384], BF16) for i in range(2)]
    a.pT = [s.sbuf("apT%d" % i, [128, 384], BF16) for i in range(2)]
    a.st = [s.sbuf("ast%d" % i, [128, 8], F32) for i in range(2)]
    a.sc4 = [s.sbuf("asc4%d" % i, [128, 4, 384], F32) for i in range(2)]
    a.p4 = [s.sbuf("ap4%d" % i, [128, 4, 384], BF16) for i in range(2)]
    a.pT4 = [s.sbuf("apT4%d" % i, [128, 4, 384], BF16) for i in range(2)]
    a.st4 = [s.sbuf("ast4%d" % i, [128, 24], F32) for i in range(2)]
    a.negsink = s.sbuf("anegsink", [128, NQH], F32)
    s.op("dve", lambda e: e.tensor_scalar(out=a.negsink[:], in0=a.sink[:], scalar1=-1.0, scalar2=None, op0=ALU.mult),
         reads=[a.sink], writes=[a.negsink])
    a.attn = s.sbuf("aattn", [128, D], BF16)
    a.attn_h = [sub(a.attn, "aattn_%d" % h) for h in range(NQH)]
    a.aT = s.sbuf("aaT", [128, DC, 128], BF16)
    a.h3 = s.sbuf("ah3", [128, D], F32)
    a.h3A = sub(a.h3, "ah3A")
    a.h3B = sub(a.h3, "ah3B")
    a.hn4 = s.sbuf("ahn4", [128, D], BF16)
    a.hn4T = s.sbuf("ahn4T", [128, DC, 128], BF16)
    a.rt = s.sbuf("art", [128, 8 * 8], F32)


def swa_produce(cx, t, h2_ap, h2_db, part=None):
    s = cx.s
    a = cx.a
    ps = cx.ps
    x = a.x[t % 3]
    if part in (None, 0):
        s.dma("sp", x[:], h2_ap[t * 128:(t + 1) * 128, :], x.name, reads=[h2_db[t]], writes=[x])
        emit_norm_T(cx, x, a.gain3, a.hn, ps[0], a.hnT, a.ssq, a.rstd, a.junk)
    W = a.W.t
    qkv = a.qkv
    if part in (None, 1):
        for i, bank in enumerate((ps[1], ps[2], ps[3])):
            cs = slice(i * 512, (i + 1) * 512)
            for c in range(DC):
                s.op("pe", lambda e, c=c, bank=bank, cs=cs: e.matmul(
                    bank[:], lhsT=a.hnT[:, c, :], rhs=W[:, c, cs], start=(c == 0), stop=False),
                    reads=[a.hnT, a.W_c[c]], writes=[bank])
            s.op("pe", lambda e, bank=bank, cs=cs: e.matmul(
                bank[:], lhsT=a.ones[0:1, :], rhs=a.brow[0:1, cs], start=False, stop=True),
                reads=[a.brow, a.ones], writes=[bank])
        s.op("act", lambda e: e.activation(out=qkv[:, 0:512], in_=ps[1][:], func=AF.Copy, scale=0.125),
             reads=[ps[1]], writes=[a.qkvA])
        s.op("dve", lambda e: e.tensor_scalar(out=qkv[:, 512:1024], in0=ps[2][:], scalar1=0.125, scalar2=None,
                                              op0=ALU.mult), reads=[ps[2]], writes=[a.qkvB])
        s.op("act", lambda e: e.copy(out=qkv[:, 1024:1536], in_=ps[3][:]), reads=[ps[3]], writes=[a.qkvC])
    if part not in (None, 2):
        return
    qT = a.qT[t % 3]
    kT = a.kT[t % 4]
    v = a.v[t % 4]
    for half, bank, dep in ((0, ps[1], a.qkvA), (1, ps[2], a.qkvB)):
        tv = Bview(bank)
        for h8 in range(8):
            h = half * 8 + h8
            s.op("pe", lambda e, h=h, h8=h8, tv=tv: e.transpose(
                out=tv[0:64, h8 * 128:(h8 + 1) * 128], in_=qkv[:, h * 64:(h + 1) * 64], identity=cx.ident[:]),
                reads=[dep, cx.ident], writes=[bank])
        dst = qT[:, half * 8:(half + 1) * 8, :].rearrange("p h t -> p (h t)")
        if half == 0:
            s.op("act", lambda e, dst=dst, tv=tv: e.copy(out=dst, in_=tv[0:64, :]), reads=[bank], writes=[qT])
        else:
            s.op("dve", lambda e, dst=dst, tv=tv: e.tensor_copy(out=dst, in_=tv[0:64, :]), reads=[bank, qT], writes=[qT])
    tv = Bview(ps[3])
    for kv in range(NKV):
        s.op("pe", lambda e, kv=kv, tv=tv: e.transpose(
            out=tv[0:64, kv * 128:(kv + 1) * 128], in_=qkv[:, 1024 + kv * 64:1024 + (kv + 1) * 64],
            identity=cx.ident[:]), reads=[a.qkvC, cx.ident], writes=[ps[3]])
    s.op("dve", lambda e, tv=tv: e.tensor_copy(out=kT[:].rearrange("p h t -> p (h t)"), in_=tv[0:64, 0:512]),
         reads=[ps[3]], writes=[kT])
    s.op("pool", lambda e: e.tensor_copy(out=v[:], in_=qkv[:, 1280:1536]), reads=[a.qkvC], writes=[v])


def swa_attend(cx, t, NT, h3_ap, h3_db, hnT_ap, hnT_db, cw_all, hn4tm_ap=None, part=None):
    s = cx.s
    a = cx.a
    ps = cx.ps
    x = a.x[t % 3]
    qT = a.qT[t % 3]
    kts = [kt for kt in (t - 1, t, t + 1) if 0 <= kt < NT]
    c0 = (kts[0] - (t - 1)) * 128
    c1 = (kts[-1] - (t - 1) + 1) * 128
    for g4 in (range(NKV) if part is None else ([part] if part < NKV else [])):
        kv = g4
        j = g4 % 2
        p, pT, st = a.p4[j], a.pT4[j], a.st4[j]
        for hh in range(4):
            h = g4 * 4 + hh
            sb_ = ps[4 + hh]
            for i, kt in enumerate(kts):
                cc = (kt - (t - 1)) * 128
                s.op("pe", lambda e, h=h, kt=kt, cc=cc, sb_=sb_, kv=kv: e.matmul(
                    sb_[:, cc:cc + 128], lhsT=qT[:, h, :], rhs=a.kT[kt % 4][:, kv, :], start=True, stop=True),
                    reads=[qT, a.kT[kt % 4]], writes=[sb_])
        sc = a.sc4[j]
        for hh in range(4):
            h = g4 * 4 + hh
            s.op("dve", lambda e, h=h, hh=hh, sc=sc: e.tensor_tensor(
                out=sc[:, hh, c0:c1], in0=ps[4 + hh][:, c0:c1], in1=a.bias[h][:, c0:c1], op=ALU.add),
                reads=[ps[4 + hh], a.bias[h], sc], writes=[sc])
        for hh in range(4):
            s.op("dve", lambda e, hh=hh, st=st, sc=sc: e.reduce_max(out=st[:, hh:hh + 1], in_=sc[:, hh, c0:c1], axis=AX.X),
                 reads=[sc, st], writes=[st])
        s.op("dve", lambda e, st=st, g4=g4: e.scalar_tensor_tensor(
            out=st[:, 4:8], in0=st[:, 0:4], scalar=-1.0, in1=a.negsink[:, g4 * 4:(g4 + 1) * 4], op0=ALU.mult, op1=ALU.min),
            reads=[st, a.negsink], writes=[st])
        for hh in range(4):
            s.op("act", lambda e, hh=hh, p=p, st=st, sc=sc: e.activation(
                out=p[:, hh, c0:c1], in_=sc[:, hh, c0:c1], func=AF.Exp, bias=st[:, 4 + hh:5 + hh],
                accum_out=st[:, 8 + hh:9 + hh]), reads=[sc, st], writes=[p, st])
        s.op("dve", lambda e, st=st, g4=g4: e.tensor_tensor(out=st[:, 12:16], in0=a.sink[:, g4 * 4:(g4 + 1) * 4],
                                                            in1=st[:, 4:8], op=ALU.add), reads=[st, a.sink], writes=[st])
        s.op("act", lambda e, st=st: e.activation(out=st[:, 12:16], in_=st[:, 12:16], func=AF.Exp), reads=[st], writes=[st])
        s.op("dve", lambda e, st=st: e.tensor_tensor(out=st[:, 16:20], in0=st[:, 8:12], in1=st[:, 12:16], op=ALU.add),
             reads=[st], writes=[st])
        s.op("dve", lambda e, st=st: e.reciprocal(out=st[:, 20:24], in_=st[:, 16:20]), reads=[st], writes=[st])
        for half, tb in ((0, ps[0]), (1, ps[3])):
            tv = Bview(tb)
            for h2 in range(2):
                hh = half * 2 + h2
                for kt in kts:
                    cc = (kt - (t - 1)) * 128
                    s.op("pe", lambda e, cc=cc, hh=hh, h2=h2, p=p, tv=tv: e.transpose(
                        out=tv[:, h2 * 384 + cc:h2 * 384 + cc + 128], in_=p[:, hh, cc:cc + 128], identity=cx.ident[:]),
                        reads=[p, cx.ident], writes=[tb])
            full = (c0 == 0 and c1 == 384)
            segs = [(0, 768, None)] if full else [(h2 * 384 + c0, h2 * 384 + c1, h2) for h2 in range(2)]
            for (x0, x1, h2) in segs:
                if h2 is None:
                    dst = pT[:, half * 2:half * 2 + 2, :].rearrange("p h c -> p (h c)")
                else:
                    dst = pT[:, half * 2 + h2, c0:c1]
                if half == 0:
                    s.op("act", lambda e, dst=dst, tv=tv, x0=x0, x1=x1: e.copy(out=dst, in_=tv[:, x0:x1]),
                         reads=[tb], writes=[pT])
                else:
                    s.op("dve", lambda e, dst=dst, tv=tv, x0=x0, x1=x1: e.tensor_copy(out=dst, in_=tv[:, x0:x1]),
                         reads=[tb, pT], writes=[pT])
        for hh in range(4):
            h = g4 * 4 + hh
            ob = ps[1 + h // 8]
            oc = slice((h % 8) * 64, (h % 8) * 64 + 64)
            for i, kt in enumerate(kts):
                cc = (kt - (t - 1)) * 128
                s.op("pe", lambda e, kt=kt, cc=cc, kv=kv, i=i, ob=ob, oc=oc, pT=pT, hh=hh: e.matmul(
                    ob[:, oc], lhsT=pT[:, hh, cc:cc + 128], rhs=a.v[kt % 4][:, kv * 64:(kv + 1) * 64],
                    start=(i == 0), stop=(i == len(kts) - 1)), reads=[pT, a.v[kt % 4]], writes=[ob])
        for hh in range(4):
            h = g4 * 4 + hh
            ob = ps[1 + h // 8]
            oc = slice((h % 8) * 64, (h % 8) * 64 + 64)
            s.op("dve", lambda e, h=h, hh=hh, ob=ob, oc=oc, st=st: e.tensor_scalar(
                out=a.attn[:, h * 64:(h + 1) * 64], in0=ob[:, oc], scalar1=st[:, 20 + hh:21 + hh], scalar2=None,
                op0=ALU.mult), reads=[ob, st], writes=[a.attn_h[h]])
    if part is not None and part < NKV:
        return
    tb = ps[0]
    tv = Bview(tb)
    for c in range(DC):
        s.op("pe", lambda e, c=c, tv=tv: e.transpose(out=tv[:, c * 128:(c + 1) * 128], in_=a.attn[:, c * 128:(c + 1) * 128],
                                              identity=cx.ident[:]), reads=a.attn_h + [cx.ident], writes=[tb])
    s.op("act", lambda e, tv=tv: e.copy(out=a.aT[:].rearrange("p c t -> p (c t)"), in_=tv[:]), reads=[tb], writes=[a.aT])
    for hh in range(2):
        yb = ps[3 + hh]
        for c in range(DC):
            s.op("pe", lambda e, c=c, hh=hh, yb=yb: e.matmul(
                yb[:], lhsT=a.aT[:, c, :], rhs=a.Wo.t[:, c, hh * 512:(hh + 1) * 512],
                start=(c == 0), stop=(c == DC - 1)), reads=[a.aT, a.Wo_c[c]], writes=[yb])
    for hh, hb in ((0, a.h3A), (1, a.h3B)):
        sl = slice(hh * 512, (hh + 1) * 512)
        s.op("dve", lambda e, hh=hh, sl=sl: e.tensor_tensor(out=a.h3[:, sl], in0=ps[3 + hh][:], in1=x[:, sl], op=ALU.add),
             reads=[ps[3 + hh], x], writes=[hb])
        s.op("pool", lambda e, sl=sl: e.tensor_tensor(out=a.h3[:, sl], in0=a.h3[:, sl], in1=a.bout[:, sl], op=ALU.add),
             reads=[hb, a.bout], writes=[hb])
    s.dma("sp", h3_ap[t * 128:(t + 1) * 128, :], a.h3[:], "ah3", reads=[a.h3A, a.h3B], writes=[h3_db[t]])
    emit_norm_T(cx, a.h3, a.gain4, a.hn4, ps[0], a.hn4T, a.ssq, a.rstd, a.junk, xdeps=[a.h3A, a.h3B])
    s.dma("sp", hnT_ap.rearrange("(c p) t -> p c t", p=128)[:, :, t * 128:(t + 1) * 128], a.hn4T[:], "ahn4T",
          reads=[a.hn4T], writes=[hnT_db[t]])
    if hn4tm_ap is not None:
        s.dma("sp", hn4tm_ap[t * 128:(t + 1) * 128, :], a.hn4[:], "ahn4tm", reads=[a.hn4])
    lb = ps[7]
    for c in range(DC):
        s.op("pe", lambda e, c=c: e.matmul(lb[:, 0:NE], lhsT=a.hn4T[:, c, :], rhs=a.Wr[:, c, :],
                                           start=(c == 0), stop=(c == DC - 1)), reads=[a.hn4T, a.Wr], writes=[lb])
    r = a.rt
    R = lambda i: r[:, i * 8:(i + 1) * 8]
    cwt = cw_all[:, t * NE:(t + 1) * NE]

    def dv(fn, rd=(), wr=()):
        s.op("dve", fn, reads=[a.rt] + list(rd), writes=[a.rt] + list(wr))
    dv(lambda e: e.tensor_copy(out=R(0), in_=lb[:, 0:NE]), rd=[lb])
    dv(lambda e: e.reduce_max(out=r[:, 56:57], in_=R(0), axis=AX.X))
    dv(lambda e: e.tensor_scalar(out=R(1), in0=R(0), scalar1=r[:, 56:57], scalar2=None, op0=ALU.is_equal))
    dv(lambda e: e.scalar_tensor_tensor(out=R(2), in0=R(1), scalar=NEG, in1=R(0), op0=ALU.mult, op1=ALU.add))
    dv(lambda e: e.reduce_max(out=r[:, 57:58], in_=R(2), axis=AX.X))
    dv(lambda e: e.tensor_scalar(out=R(3), in0=R(2), scalar1=r[:, 57:58], scalar2=None, op0=ALU.is_equal))
    dv(lambda e: e.tensor_tensor(out=r[:, 58:59], in0=r[:, 57:58], in1=r[:, 56:57], op=ALU.subtract))
    s.op("act", lambda e: e.activation(out=r[:, 59:60], in_=r[:, 58:59], func=AF.Exp), reads=[a.rt], writes=[a.rt])
    dv(lambda e: e.tensor_scalar(out=r[:, 60:61], in0=r[:, 59:60], scalar1=1.0, scalar2=None, op0=ALU.add))
    dv(lambda e: e.reciprocal(out=r[:, 61:62], in_=r[:, 60:61]))
    dv(lambda e: e.tensor_tensor(out=r[:, 62:63], in0=r[:, 59:60], in1=r[:, 61:62], op=ALU.mult))
    dv(lambda e: e.tensor_scalar(out=R(4), in0=R(1), scalar1=r[:, 61:62], scalar2=None, op0=ALU.mult))
    dv(lambda e: e.scalar_tensor_tensor(out=cwt, in0=R(3), scalar=r[:, 62:63], in1=R(4), op0=ALU.mult, op1=ALU.add),
       wr=[cw_all])


def new_phase(cx, nc):
    cx.s = Sched(nc)
    for b in cx.persist:
        b.lw = None
        b.rd = {}
        b.rd_dma = []
    return cx.s


def build_program(S, TB=256, sparse=True):
    NT = S // 128
    nc = bass.Bass("TRN2", target_bir_lowering=False)

    def din(name, shape):
        return nc.dram_tensor(name, list(shape), F32, kind="ExternalInput").ap()

    x = din("x", [S, D])
    P = {
        "mix_norm0": din("mix_norm0", [D]), "mix_norm1": din("mix_norm1", [D]),
        "ffn_norm0": din("ffn_norm0", [D]), "ffn_norm1": din("ffn_norm1", [D]),
        "gla_in": din("gla_in", [D, GW]), "gw_f": din("gw_f", [16, 512]), "gb_f": din("gb_f", [512]),
        "gw_b": din("gw_b", [16, 512]), "gb_b": din("gb_b", [512]), "gla_hn": din("gla_hn", [D]),
        "gla_out": din("gla_out", [D, D]),
        "swa_qkv": din("swa_qkv", [D, 1536]), "swa_qkv_b": din("swa_qkv_b", [1536]), "sinks": din("sinks", [NQH]),
        "swa_out": din("swa_out", [D, D]), "swa_out_b": din("swa_out_b", [D]),
        "dWg": din("dWg", [D, FF]), "dWu": din("dWu", [D, FF]), "dWd": din("dWd", [FF, D]),
        "router": din("router", [D, NE]),
        "mWg": din("mWg", [NE, D, FF]), "mWu": din("mWu", [NE, D, FF]), "mWd": din("mWd", [NE, FF, D]),
        "final_norm": din("final_norm", [D]),
    }
    out = nc.dram_tensor("out", [S, D], F32, kind="ExternalOutput").ap()
    Sb = nc.dram_tensor("scr_Sb", [NT, 128, NH * 256], BF16).ap()
    h1 = nc.dram_tensor("scr_h1", [S, D], F32).ap()
    h3 = nc.dram_tensor("scr_h3", [S + 128, D], F32).ap()
    hnT = nc.dram_tensor("scr_hnT", [D, S], BF16).ap()
    hnT2 = nc.dram_tensor("scr_hnT2", [D, S], BF16).ap()
    hn4tm = nc.dram_tensor("scr_hn4tm", [S + 128, D], BF16).ap()
    lst = nc.dram_tensor("scr_list", [NE * CAPR + NT * NE * 128, 2], I32).ap()

    cx = Ctx()
    pstack = contextlib.ExitStack()
    Sched.sem_pool = {"sw": [], "hw": [], "eng": []}
    Sched.sem_total = {}
    Sched.sem_stack = pstack
    cx.persist = []
    s = cx.s = Sched(nc)
    keep = s.stack
    s.stack = pstack
    emit_consts(cx)
    emit_psum(cx)
    cw_all = s.sbuf("cw_all", [128, NT * NE], F32)
    flag = s.sbuf("flag", [128, 1], I32)
    s.stack = keep
    regs = {}
    for en, eo in (("pe", nc.tensor), ("act", nc.scalar), ("dve", nc.vector), ("pool", nc.gpsimd), ("sp", nc.sync)):
        regs[en] = pstack.enter_context(eo.register("flagreg_" + en))
    cx.persist = [cx.ident, cx.identf, cx.epsc, cw_all, flag] + cx.ps
    cx.cw_buf = cw_all
    alloc_gla(cx, P)
    Sb_db = [s.dbuf("Sbdb%d" % t) for t in range(NT)]
    h1_db = [s.dbuf("h1db%d" % t) for t in range(NT)]
    hn_db = [s.dbuf("hndb%d" % t) for t in range(NT)]
    for t in reversed(range(NT)):
        emit_gla_tile(cx, t, False, x, Sb, Sb_db, h1, h1_db, hnT, hn_db)
    for t in range(NT):
        emit_gla_tile(cx, t, True, x, Sb, Sb_db, h1, h1_db, hnT, hn_db)
    s.emit()
    if _PH == 1:
        pstack.close()
        return nc
    s = new_phase(cx, nc)
    alloc_ffn_bufs(cx, TB)
    ws = [alloc_wset(cx, 0), alloc_wset(cx, 1)]
    db1 = [s.dbuf("p2a%d" % t) for t in range(NT)]
    db2 = [s.dbuf("p2b%d" % t) for t in range(NT)]
    for f in ffn_weight_loads(cx, ws[0], P["dWg"], P["dWu"], P["dWd"], 0):
        f()
    pre = ffn_weight_loads(cx, ws[1], P["dWg"], P["dWu"], P["dWd"], 1)
    ctr = emit_ffn_pass(cx, S, hnT, db1, ws[0], None, None, h1, db2, pre=pre)
    emit_ffn_pass(cx, S, hnT, db1, ws[1], None, None, h1, db2, ctr=ctr)
    s.emit()
    if _PH == 2:
        pstack.close()
        return nc
    s = new_phase(cx, nc)
    alloc_swa(cx, P, NT)
    d2 = [s.dbuf("p3a%d" % t) for t in range(NT)]
    d3 = [s.dbuf("p3b%d" % t) for t in range(NT)]
    d4 = [s.dbuf("p3c%d" % t) for t in range(NT)]
    swa_produce(cx, 0, h1, d2)
    if NT > 1:
        swa_produce(cx, 1, h1, d2)
    for t in range(NT):
        for part in range(NKV):
            swa_attend(cx, t, NT, h3, d3, hnT2, d4, cw_all, part=part)
            if t + 2 < NT and part < 3:
                swa_produce(cx, t + 2, h1, d2, part=part)
        swa_attend(cx, t, NT, h3, d3, hnT2, d4, cw_all, hn4tm_ap=(hn4tm if sparse else None), part=NKV)
    s.emit()
    if _PH == 3:
        pstack.close()
        return nc

    def moe_dense():
        s = cx.s
        db1 = [s.dbuf("p4a%d" % t) for t in range(NT)]
        db2 = [s.dbuf("p4b%d" % t) for t in range(NT)]
        for f in ffn_weight_loads(cx, ws[0], P["mWg"][0], P["mWu"][0], P["mWd"][0], 0):
            f()
        ctr = None
        for he in range(2 * NE):
            e_, half = he // 2, he % 2
            pre = ()
            if he + 1 < 2 * NE:
                e2, h2_ = (he + 1) // 2, (he + 1) % 2
                pre = ffn_weight_loads(cx, ws[(he + 1) % 2], P["mWg"][e2], P["mWu"][e2], P["mWd"][e2], h2_)
            ctr = emit_ffn_pass(cx, S, hnT2, db1, ws[he % 2], cw_all, (lambda t, e_=e_: t * NE + e_), h3, db2,
                                pre=pre, ctr=ctr)
        emit_final_norm(cx, S, h3, out, P["final_norm"], lambda t: [db2[t]])

    def moe_sparse():
        s = cx.s
        h3db = s.dbuf("h3db")
        for f in ffn_weight_loads(cx, ws[0], P["mWg"][0], P["mWu"][0], P["mWd"][0], 0):
            f()
        ctr = None
        for he in range(2 * NE):
            e_, half = he // 2, he % 2
            pre = ()
            if he + 1 < 2 * NE:
                e2, h2_ = (he + 1) // 2, (he + 1) % 2
                pre = ffn_weight_loads(cx, ws[(he + 1) % 2], P["mWg"][e2], P["mWu"][e2], P["mWd"][e2], h2_)
            ctr = emit_moe_sparse_pass(cx, S, e_, ws[he % 2], lst, hn4tm, h3, h3db, pre, ctr)
        emit_final_norm(cx, S, h3, out, P["final_norm"], lambda t: [h3db])

    if not sparse:
        s = new_phase(cx, nc)
        alloc_ffn_bufs(cx, TB)
        ws = [alloc_wset(cx, 0), alloc_wset(cx, 1)]
        cx.fst = [s.sbuf("fst%d" % i, [128, 2], F32) for i in range(3)]
        moe_dense()
        s.emit()
        pstack.close()
        return nc
    s = new_phase(cx, nc)
    emit_route(cx, S, cw_all, lst, flag)
    if _os.environ.get("DBG"):
        dbg = nc.dram_tensor("dbg", [128, NE + 2], F32, kind="ExternalOutput").ap()
        s.dma("sp", dbg[:, 0:NE], cx.r_base[:, NT * NE:NT * NE + NE], "r_dbg", reads=[cx.r_base])
        s.dma("sp", dbg[:, NE:NE + 2], cx.r_fl[:], "r_dbg2", reads=[cx.r_fl])
    zf = s.sbuf("r_zf", [128, D], F32)
    zb = s.sbuf("r_zb", [128, D], BF16)
    s.op("pool", lambda e: e.memset(zf[:], 0.0), writes=[zf])
    s.op("pool", lambda e: e.memset(zb[:], 0.0), writes=[zb])
    s.dma("sp", h3[S:S + 128, :], zf[:], "r_zf", reads=[zf])
    s.dma("sp", hn4tm[S:S + 128, :], zb[:], "r_zb", reads=[zb])
    s.emit()
    sa = new_phase(cx, nc)
    alloc_ffn_bufs(cx, TB)
    ws = [alloc_wset(cx, 0), alloc_wset(cx, 1)]
    g = cx.sp = Ctx()
    g.cnt = 0
    g.tile_idx = {}
    g.tile_gx = {}
    g.idx = [sa.sbuf("sp_idx%d" % i, [128, 2], I32) for i in range(8)]
    g.gx = [sa.sbuf("sp_gx%d" % i, [128, D], BF16) for i in range(6)]
    cx.fst = [sa.sbuf("fst%d" % i, [128, 2], F32) for i in range(3)]
    moe_sparse()
    for b in Buf.registry:
        b.lw = None
        b.rd = {}
        b.rd_dma = []
    sb_ = cx.s = Sched(nc)
    moe_dense()
    emit_either(nc, flag, regs, sa, sb_)
    pstack.close()
    return nc


def emit_final_norm(cx, S, h3, out, gain_ap, h3dep):
    s = cx.s
    NT = S // 128

    def fview(b):
        return b.t[:].rearrange("p a b -> p (a b)").bitcast(F32)[:, 0:D]
    fgB, fjB = cx.hnTb[1], cx.hnTb[0]
    s.dma("sp", fview(fgB), bcast_row(gain_ap), "fgain", writes=[fgB])
    for t in range(NT):
        i = t % 3
        xb, xd = cx.stg[i], [cx.stg[i], cx.stgA[i], cx.stgB[i]]
        ob = cx.hT[t % 2]
        st = cx.fst[i]
        s.dma("sp", xb[:], h3[t * 128:(t + 1) * 128, :], "fx%d" % i, reads=h3dep(t), writes=xd)
        s.op("act", lambda e, xb=xb, st=st: e.activation(out=fview(fjB), in_=xb[:], func=AF.Square, scale=1.0 / 32.0,
                                                         accum_out=st[:, 0:1]), reads=xd, writes=[fjB, st])
        s.op("act", lambda e, st=st: e.activation(out=st[:, 1:2], in_=st[:, 0:1], func=AF.Ln, bias=cx.epsc[:, 0:1]),
             reads=[st, cx.epsc], writes=[st])
        s.op("act", lambda e, st=st: e.activation(out=st[:, 1:2], in_=st[:, 1:2], func=AF.Exp, scale=-0.5),
             reads=[st], writes=[st])
        s.op("dve", lambda e, xb=xb, ob=ob, st=st: e.scalar_tensor_tensor(
            out=fview(ob), in0=xb[:], scalar=st[:, 1:2], in1=fview(fgB), op0=ALU.mult, op1=ALU.mult),
            reads=xd + [st, fgB], writes=[ob])
        s.dma("sp", out[t * 128:(t + 1) * 128, :], fview(ob), "fo%d" % (t % 2), reads=[ob])


PARAM_MAP = [
    ("mix_norm0", "mix_norm", 0), ("mix_norm1", "mix_norm", 1), ("ffn_norm0", "ffn_norm", 0), ("ffn_norm1", "ffn_norm", 1),
    ("gla_in", "gla_in_proj", 0), ("gw_f", "gla_gate_w_fwd", 0), ("gb_f", "gla_gate_b_fwd", 0),
    ("gw_b", "gla_gate_w_bwd", 0), ("gb_b", "gla_gate_b_bwd", 0), ("gla_hn", "gla_head_norm", 0),
    ("gla_out", "gla_out_proj", 0), ("swa_qkv", "swa_qkv_proj", 0), ("swa_qkv_b", "swa_qkv_bias", 0),
    ("sinks", "swa_sinks", 0), ("swa_out", "swa_out_proj", 0), ("swa_out_b", "swa_out_bias", 0),
    ("dWg", "dense_w_gate", 0), ("dWu", "dense_w_up", 0), ("dWd", "dense_w_down", 0), ("router", "moe_router", 0),
    ("mWg", "moe_w_gate", 0), ("mWu", "moe_w_up", 0), ("mWd", "moe_w_down", 0), ("final_norm", "final_norm", None),
]


def make_in_map(inputs, xs):
    m = {"x": np.ascontiguousarray(xs, dtype=np.float32)}
    for dst, src, idx in PARAM_MAP:
        v = np.asarray(inputs[src])
        if idx is not None:
            v = v[idx]
        m[dst] = np.ascontiguousarray(v, dtype=np.float32)
    return m


def kernel(**inputs):
    x = np.asarray(inputs["x"])
    B, S, _ = x.shape
    nc = build_program(S)
    base = make_in_map(inputs, x[0])
    in_maps = []
    for b in range(B):
        m = dict(base)
        m["x"] = np.ascontiguousarray(x[b], dtype=np.float32)
        in_maps.append(m)
    res = run_bass_kernel_spmd(nc, in_maps, core_ids=list(range(B)))
    return np.stack([np.asarray(r["out"]) for r in res.results], axis=0).astype(np.float32)


CAPT = 20
CAPR = CAPT * 128
BIGI = 1.0e6


def emit_route(cx, S, cw_all, list_ap, flag):
    s = cx.s
    NT = S // 128
    NC = NT * NE
    nb = (NC + 511) // 512
    m = s.sbuf("r_m", [128, NC], F32)
    mb = s.sbuf("r_mb", [128, NC], BF16)
    s.op("dve", lambda e: e.tensor_single_scalar(out=m[:], in_=cw_all[:], scalar=0.0, op=ALU.is_gt),
         reads=[cw_all], writes=[m])
    s.op("dve", lambda e: e.tensor_copy(out=mb[:], in_=m[:]), reads=[m], writes=[mb])
    slf = tri(cx, "r_slf", 1.0, 1, -1, ALU.is_gt)
    sl = s.sbuf("r_sl", [128, 128], BF16)
    s.op("dve", lambda e: e.tensor_copy(out=sl[:], in_=slf[:]), reads=[slf], writes=[sl])
    on = s.sbuf("r_on", [128, 128], BF16)
    s.op("pool", lambda e: e.memset(on[:], 1.0), writes=[on])
    within = s.sbuf("r_within", [128, NC], F32)
    tot = s.sbuf("r_tot", [128, NC], F32)
    for k in range(nb):
        cs = slice(k * 512, min(NC, (k + 1) * 512))
        n = cs.stop - cs.start
        s.op("pe", lambda e, cs=cs, n=n: e.matmul(cx.ps[0][:, 0:n], lhsT=sl[:], rhs=mb[:, cs], start=True, stop=True),
             reads=[sl, mb], writes=[cx.ps[0]])
        s.op("pe", lambda e, cs=cs, n=n: e.matmul(cx.ps[1][:, 0:n], lhsT=on[:], rhs=mb[:, cs], start=True, stop=True),
             reads=[on, mb], writes=[cx.ps[1]])
        s.op("act", lambda e, cs=cs, n=n: e.copy(out=within[:, cs], in_=cx.ps[0][:, 0:n]),
             reads=[cx.ps[0], within], writes=[within])
        s.op("dve", lambda e, cs=cs, n=n: e.tensor_copy(out=tot[:, cs], in_=cx.ps[1][:, 0:n]),
             reads=[cx.ps[1], tot], writes=[tot])
    base = s.sbuf("r_base", [128, NC + NE], F32)
    s.op("pool", lambda e: e.memset(base[:, 0:NE], 0.0), writes=[base])
    for t in range(NT):
        s.op("dve", lambda e, t=t: e.tensor_tensor(out=base[:, (t + 1) * NE:(t + 2) * NE], in0=base[:, t * NE:(t + 1) * NE],
                                                   in1=tot[:, t * NE:(t + 1) * NE], op=ALU.add),
             reads=[base, tot], writes=[base])
    fl = cx.r_fl = s.sbuf("r_fl", [128, 2], F32)
    cx.r_base = base
    s.op("dve", lambda e: e.reduce_max(out=fl[:, 0:1], in_=base[:, NC:NC + NE], axis=AX.X), reads=[base], writes=[fl])
    s.op("dve", lambda e: e.tensor_single_scalar(out=fl[:, 1:2], in_=fl[:, 0:1], scalar=(-1.0 if _os.environ.get('FORCEDENSE') else float(CAPR) + 0.5), op=ALU.is_lt),
         reads=[fl], writes=[fl])
    s.op("dve", lambda e: e.tensor_copy(out=flag[:], in_=fl[:, 1:2]), reads=[fl], writes=[flag])
    offi = s.sbuf("r_offi", [128, NC], I32)
    s.op("pool", lambda e: e.iota(offi[:].rearrange("p (t e) -> p t e", e=NE), pattern=[[0, NT], [CAPR, NE]], base=0,
                                  channel_multiplier=0), writes=[offi])
    dest = s.sbuf("r_dest", [128, NC], F32)
    s.op("dve", lambda e: e.tensor_copy(out=dest[:], in_=offi[:]), reads=[offi], writes=[dest])
    s.op("dve", lambda e: e.tensor_tensor(out=dest[:], in0=dest[:], in1=base[:, 0:NC], op=ALU.add),
         reads=[dest, base], writes=[dest])
    s.op("dve", lambda e: e.tensor_tensor(out=dest[:], in0=dest[:], in1=within[:], op=ALU.add),
         reads=[dest, within], writes=[dest])
    s.op("dve", lambda e: e.tensor_tensor(out=dest[:], in0=dest[:], in1=m[:], op=ALU.mult),
         reads=[dest, m], writes=[dest])
    nrow = NE * CAPR
    dumpi = s.sbuf("r_dumpi", [128, NC], I32)
    s.op("pool", lambda e: e.iota(dumpi[:], pattern=[[128, NC]], base=nrow, channel_multiplier=1), writes=[dumpi])
    tmp = s.sbuf("r_tmp", [128, NC], F32)
    s.op("dve", lambda e: e.tensor_copy(out=tmp[:], in_=dumpi[:]), reads=[dumpi], writes=[tmp])
    om = s.sbuf("r_om", [128, NC], F32)
    s.op("dve", lambda e: e.tensor_scalar(out=om[:], in0=m[:], scalar1=-1.0, scalar2=1.0, op0=ALU.mult, op1=ALU.add),
         reads=[m], writes=[om])
    s.op("dve", lambda e: e.tensor_tensor(out=tmp[:], in0=tmp[:], in1=om[:], op=ALU.mult),
         reads=[tmp, om], writes=[tmp])
    s.op("dve", lambda e: e.tensor_tensor(out=dest[:], in0=dest[:], in1=tmp[:], op=ALU.add),
         reads=[dest, tmp], writes=[dest])
    desti = s.sbuf("r_desti", [128, NC], I32)
    s.op("dve", lambda e: e.tensor_copy(out=desti[:], in_=dest[:]), reads=[dest], writes=[desti])
    src = s.sbuf("r_src", [128, NC, 2], I32)
    srcA = sub(src, "r_srcA")
    s.op("pool", lambda e: e.iota(src[:, :, 0].rearrange("p (t e) -> p t e", e=NE), pattern=[[128, NT], [0, NE]], base=0,
                                  channel_multiplier=1), writes=[src])
    s.op("dve", lambda e: e.tensor_copy(out=src[:, :, 1], in_=cw_all[:].bitcast(I32)), reads=[cw_all], writes=[srcA])
    K = nrow // 128
    ini = s.sbuf("r_ini", [128, K, 2], I32)
    iniA = sub(ini, "r_iniA")
    s.op("pool", lambda e: e.iota(ini[:, :, 0], pattern=[[1, K]], base=0, channel_multiplier=K), writes=[ini])
    s.op("dve", lambda e: e.tensor_single_scalar(out=ini[:, :, 0], in_=ini[:, :, 0], scalar=127, op=ALU.bitwise_and),
         reads=[ini], writes=[ini])
    s.op("dve", lambda e: e.tensor_single_scalar(out=ini[:, :, 0], in_=ini[:, :, 0], scalar=S, op=ALU.add),
         reads=[ini], writes=[ini])
    s.op("pool", lambda e: e.memset(ini[:, :, 1], 0), writes=[iniA])
    ldb = s.dbuf("r_listdb")
    s.dma("sp", list_ap[0:nrow, :].rearrange("(p k) c -> p k c", p=128), ini[:], "r_ini", reads=[ini, iniA], writes=[ldb])
    for col in range(NC):
        s.op("pool", lambda e, col=col: e.indirect_dma_start(
            out=list_ap[:, :], out_offset=bass.IndirectOffsetOnAxis(ap=desti[:, col:col + 1], axis=0),
            in_=src[:, col, :], in_offset=None),
            reads=[ldb, desti, src, srcA], writes=[], dma_key="r_scat")


def emit_moe_sparse_pass(cx, S, e_, w, list_ap, hn4tm_ap, h3_ap, h3db, pre, ctr):
    s = cx.s
    TB = cx.TB
    ntt = TB // 128
    g = cx.sp

    def prefetch(b):
        for i in range(ntt):
            j = b * ntt + i
            q = g.cnt
            g.cnt += 1
            idx = g.idx[q % 8]
            gx = g.gx[q % 6]
            g.tile_idx[j] = idx
            g.tile_gx[j] = (gx, q)
            r0 = e_ * CAPR + j * 128
            s.dma("sp", idx[:], list_ap[r0:r0 + 128, :], idx.name, writes=[idx])
            s.op("pool", lambda e, idx=idx, gx=gx: e.indirect_dma_start(
                out=gx[:, :], out_offset=None, in_=hn4tm_ap[:, :],
                in_offset=bass.IndirectOffsetOnAxis(ap=idx[:, 0:1], axis=0)),
                reads=[idx], writes=[gx], dma_key=gx.name)

    def loader(b, hb):
        for i in range(ntt):
            j = b * ntt + i
            gx, q = g.tile_gx[j]
            bank = cx.ps[4 + (q % 2) * 2]
            tv = Bview(bank)
            for c in range(DC):
                s.op("pe", lambda e, c=c, gx=gx, tv=tv: e.transpose(out=tv[:, c * 128:(c + 1) * 128],
                                                                    in_=gx[:, c * 128:(c + 1) * 128], identity=cx.ident[:]),
                     reads=[gx, cx.ident], writes=[bank])
            s.op("act", lambda e, i=i, tv=tv, hb=hb: e.copy(
                out=hb[:, :, i * 128:(i + 1) * 128], in_=tv[:].rearrange("p (c t) -> p c t", c=DC)),
                reads=[bank], writes=[hb])

    def sinker(tile, pd, stg, sa, sb_):
        idx = g.tile_idx[tile]
        cwa = idx[:, 1:2].bitcast(F32)
        s.op("act", lambda e, stg=stg, pd=pd, cwa=cwa: e.activation(
            out=stg[:, 0:512], in_=pd[0][:], func=AF.Copy, scale=cwa), reads=[pd[0], idx], writes=[sa])
        s.op("dve", lambda e, stg=stg, pd=pd, cwa=cwa: e.tensor_scalar(
            out=stg[:, 512:1024], in0=pd[1][:], scalar1=cwa, scalar2=None, op0=ALU.mult),
            reads=[pd[1], idx], writes=[sb_])
        s.op("pool", lambda e, stg=stg, idx=idx: e.indirect_dma_start(
            out=h3_ap[:, :], out_offset=bass.IndirectOffsetOnAxis(ap=idx[:, 0:1], axis=0), in_=stg[:, :],
            in_offset=None, compute_op=ALU.add),
            reads=[sa, sb_, idx, h3db], writes=[h3db], dma_key=stg.name)

    return emit_ffn_pass(cx, CAPR, None, None, w, None, None, None, None, pre=pre, ctr=ctr,
                         loader=loader, sinker=sinker, prefetch=prefetch)
```

```python
import contextlib
import numpy as np
import concourse.bass as bass
import concourse.mybir as mybir
from concourse.bass_utils import run_bass_kernel_spmd

F32 = mybir.dt.float32
BF16 = mybir.dt.bfloat16
I32 = mybir.dt.int32
ALU = mybir.AluOpType
AF = mybir.ActivationFunctionType
AX = mybir.AxisListType

D = 1024
DC = 8
FF = 2816
FH = 1408
NFB = 11
NE = 8
EPS = 1e-5


class Buf:
    __slots__ = ("t", "name", "lw", "rd", "rd_dma", "excl")

    registry = []

    def __init__(self, t, name):
        Buf.registry.append(self)
        self.t = t
        self.name = name
        self.excl = False
        self.lw = None
        self.rd = {}
        self.rd_dma = []

    def __getitem__(self, idx):
        return self.t[idx]


class Op:
    __slots__ = ("eng", "fn", "deps", "needed", "val", "dma_key", "idx")

    def __init__(self, eng, fn, dma_key):
        self.eng = eng
        self.fn = fn
        self.deps = []
        self.needed = False
        self.val = None
        self.dma_key = dma_key
        self.idx = None


class Sched:
    ENGS = ("pe", "act", "dve", "pool", "sp")

    _uid = 0

    def __init__(self, nc):
        Sched._uid += 1
        self.uid = Sched._uid
        self.nc = nc
        self.ops = {e: [] for e in self.ENGS}
        self.stack = contextlib.ExitStack()
        self.sems = {}
        self.dma_count = {}
        self.dma_ops = []
        self.sem_used = {}
        self.slot = {}
        self.base = {}

    sem_pool = {"sw": [], "hw": [], "eng": []}
    sem_stack = None
    sem_total = {}

    def sem(self, key, cls="eng"):
        if key not in self.sems:
            i = self.sem_used.get(cls, 0)
            self.sem_used[cls] = i + 1
            pool = Sched.sem_pool[cls]
            if i >= len(pool):
                pool.append(Sched.sem_stack.enter_context(self.nc.semaphore("sem_%s_%d" % (cls, i))))
            self.sems[key] = pool[i]
            self.slot[key] = (cls, i)
            self.base[key] = Sched.sem_total.get((cls, i), 0)
        return self.sems[key]

    def sbuf(self, name, shape, dtype):
        t = self.stack.enter_context(self.nc.sbuf_tensor("%s_u%d" % (name, self.uid), list(shape), dtype))
        return Buf(t, name)

    def psum(self, name, shape, dtype):
        t = self.stack.enter_context(self.nc.psum_tensor("%s_u%d" % (name, self.uid), list(shape), dtype))
        b = Buf(t, name)
        b.excl = True
        return b

    def dbuf(self, name):
        return Buf(None, name)

    def op(self, eng, fn, reads=(), writes=(), dma_key=None):
        o = Op(eng, fn, dma_key)
        deps = set()
        for b in reads:
            if b.lw is not None:
                deps.add(b.lw)
            if b.excl:
                for en, r in b.rd.items():
                    if en != eng:
                        deps.add(r)
        for b in writes:
            if b.lw is not None:
                deps.add(b.lw)
            for r in b.rd.values():
                deps.add(r)
            for r in b.rd_dma:
                deps.add(r)
        for d in deps:
            if d is o:
                continue
            if d.dma_key is None and d.eng == eng:
                if eng == "pe" or eng == "sp":
                    continue
                if not any((b.lw is d) for b in reads):
                    continue
            d.needed = True
            o.deps.append(d)
        for b in reads:
            if dma_key is not None:
                b.rd_dma.append(o)
            else:
                b.rd[eng] = o
        for b in writes:
            b.lw = o
            b.rd = {}
            b.rd_dma = []
        if dma_key is not None:
            self.sem(dma_key, "sw" if eng == "pool" else "hw")
            self.dma_count[dma_key] = self.dma_count.get(dma_key, 0) + 16
            o.val = self.base[dma_key] + self.dma_count[dma_key]
            o.needed = True
            self.dma_ops.append(o)
        self.ops[eng].append(o)
        return o

    def dma(self, eng, out_ap, in_ap, key, reads=(), writes=(), **kw):
        return self.op(eng, lambda e: e.dma_start(out=out_ap, in_=in_ap, **kw),
                       reads=reads, writes=writes, dma_key=key)

    def prepare(self):
        self.totals = {}
        for e in self.ENGS:
            c = 0
            if any(o.dma_key is None and o.needed for o in self.ops[e]):
                self.sem("E" + e)
                c0 = self.base["E" + e]
                for o in self.ops[e]:
                    if o.dma_key is None and o.needed:
                        c += 1
                        o.val = c0 + c
                self.totals[self.slot["E" + e]] = c0 + c
        for key, n in self.dma_count.items():
            self.totals[self.slot[key]] = self.base[key] + n
        fin = Op("sp", None, None)
        last = {}
        for o in self.dma_ops:
            last[o.dma_key] = o
        fin.deps = list(last.values())
        self.ops["sp"].append(fin)

    def run(self, e, eng):
        waited = {}
        for o in self.ops[e]:
            for d in o.deps:
                key = d.dma_key if d.dma_key is not None else "E" + d.eng
                if waited.get(key, 0) >= d.val:
                    continue
                waited[key] = d.val
                eng.wait_ge(self.sems[key], d.val)
            if o.fn is None:
                continue
            ins = o.fn(eng)
            if o.dma_key is not None:
                ins.then_inc(self.sems[o.dma_key], 16)
            elif o.needed:
                ins.then_inc(self.sems["E" + e], 1)

    def emit(self):
        nc = self.nc
        self.prepare()
        with nc.Block() as block:
            @block.tensor
            def _(eng):
                self.run("pe", eng)

            @block.scalar
            def _(eng):
                self.run("act", eng)

            @block.vector
            def _(eng):
                self.run("dve", eng)

            @block.gpsimd
            def _(eng):
                self.run("pool", eng)

            @block.sync
            def _(eng):
                self.run("sp", eng)
        Sched.sem_total.update(self.totals)
        self.stack.close()


def emit_either(nc, flag, regs, sa, sb):
    sa.prepare()
    sb.prepare()

    def body(e, eng):
        r = regs[e]
        eng.reg_load(r, flag[0:1, 0:1])
        with eng.If_eq(r, 1):
            sa.run(e, eng)
        with eng.Else():
            sb.run(e, eng)

    with nc.Block() as block:
        @block.tensor
        def _(eng):
            body("pe", eng)

        @block.scalar
        def _(eng):
            body("act", eng)

        @block.vector
        def _(eng):
            body("dve", eng)

        @block.gpsimd
        def _(eng):
            body("pool", eng)

        @block.sync
        def _(eng):
            body("sp", eng)
    sa.stack.close()
    sb.stack.close()


class Ctx:
    pass


def bcast_row(ap_row, nparts=128):
    return ap_row.partition_broadcast(nparts)


def emit_consts(cx):
    s = cx.s
    cx.ident = s.sbuf("ident", [128, 128], BF16)
    cx.identf = s.sbuf("identf", [128, 128], F32)
    s.op("pool", lambda e: e.memset(cx.identf[:], 1.0), writes=[cx.identf])
    s.op("pool", lambda e: e.affine_select(out=cx.identf[:], in_=cx.identf[:], pattern=[[-1, 128]],
                                           compare_op=ALU.is_equal, fill=0.0, base=0,
                                           channel_multiplier=1),
         reads=[cx.identf], writes=[cx.identf])
    s.op("dve", lambda e: e.tensor_copy(out=cx.ident[:], in_=cx.identf[:]),
         reads=[cx.identf], writes=[cx.ident])
    cx.epsc = s.sbuf("epsc", [128, 1], F32)
    s.op("pool", lambda e: e.memset(cx.epsc[:], EPS), writes=[cx.epsc])


def emit_norm_T(cx, x, gain_bc, hn, bank, hnT, ssq, rstd, junk, xdeps=None, evac_eng="act"):
    s = cx.s
    xd = [x] if xdeps is None else list(xdeps)
    psT = Bview(bank)
    s.op("act", lambda e: e.activation(out=junk[:], in_=x[:], func=AF.Square, scale=1.0 / 32.0,
                                       accum_out=ssq[:]),
         reads=xd, writes=[junk, ssq])
    s.op("act", lambda e: e.activation(out=rstd[:], in_=ssq[:], func=AF.Ln, bias=cx.epsc[:, 0:1]),
         reads=[ssq, cx.epsc], writes=[rstd])
    s.op("act", lambda e: e.activation(out=rstd[:], in_=rstd[:], func=AF.Exp, scale=-0.5),
         reads=[rstd], writes=[rstd])
    s.op("dve", lambda e: e.scalar_tensor_tensor(out=hn[:], in0=x[:], scalar=rstd[:, 0:1], in1=gain_bc[:],
                                                 op0=ALU.mult, op1=ALU.mult),
         reads=xd + [rstd, gain_bc], writes=[hn])
    for c in range(DC):
        s.op("pe", lambda e, c=c: e.transpose(out=psT[:, c * 128:(c + 1) * 128],
                                              in_=hn[:, c * 128:(c + 1) * 128], identity=cx.ident[:]),
             reads=[hn, cx.ident], writes=[bank])
    if evac_eng == "act":
        s.op("act", lambda e: e.copy(out=hnT[:].rearrange("p c t -> p (c t)"), in_=psT[:]),
             reads=[bank], writes=[hnT])
    else:
        s.op("dve", lambda e: e.tensor_copy(out=hnT[:].rearrange("p c t -> p (c t)"), in_=psT[:]),
             reads=[bank], writes=[hnT])


class Bview:
    def __init__(self, b):
        self.b = b

    def __getitem__(self, idx):
        return self.b.t[:].bitcast(BF16)[idx]


def sub(parent, name):
    return Buf(parent.t, name)


def emit_psum(cx):
    cx.ps = [cx.s.psum("ps%d" % i, [128, 512], F32) for i in range(8)]


class WSet:
    pass


def alloc_wset(cx, k):
    s = cx.s
    w = WSet()
    w.wg = s.sbuf("wg%d" % k, [128, DC, FH], BF16)
    w.wu = s.sbuf("wu%d" % k, [128, DC, FH], BF16)
    w.wd = s.sbuf("wd%d" % k, [128, NFB, D], BF16)
    w.wg_c = [sub(w.wg, "wg%d_%d" % (k, c)) for c in range(DC)]
    w.wu_c = [sub(w.wu, "wu%d_%d" % (k, c)) for c in range(DC)]
    w.wd_c = [sub(w.wd, "wd%d_%d" % (k, c)) for c in range(NFB)]
    return w


def ffn_weight_loads(cx, w, Wg, Wu, Wd, half):
    s = cx.s
    out = []
    f0 = half * FH
    for c in range(DC):
        out.append(lambda c=c: s.dma("pool", w.wg.t[:, c, :], Wg[c * 128:(c + 1) * 128, f0:f0 + FH],
                                     w.wg_c[c].name, writes=[w.wg_c[c]]))
        out.append(lambda c=c: s.dma("pool", w.wu.t[:, c, :], Wu[c * 128:(c + 1) * 128, f0:f0 + FH],
                                     w.wu_c[c].name, writes=[w.wu_c[c]]))
    for fb in range(NFB):
        out.append(lambda fb=fb: s.dma("pool", w.wd.t[:, fb, :], Wd[f0 + fb * 128:f0 + (fb + 1) * 128, :],
                                       w.wd_c[fb].name, writes=[w.wd_c[fb]]))
    return out


def alloc_ffn_bufs(cx, TB):
    s = cx.s
    cx.TB = TB
    cx.hnTb = [s.sbuf("hnTb%d" % i, [128, DC, TB], BF16) for i in range(2)]
    cx.hT = [s.sbuf("hT%d" % i, [128, NFB, TB], BF16) for i in range(2)]
    cx.sg = [s.sbuf("sg%d" % i, [128, TB], BF16) for i in range(2)]
    cx.stg = [s.sbuf("stg%d" % i, [128, D], F32) for i in range(3)]
    cx.stgA = [sub(b, b.name + "A") for b in cx.stg]
    cx.stgB = [sub(b, b.name + "B") for b in cx.stg]


def emit_ffn_pass(cx, S, hnT_ap, hnT_db, w, cw, cw_col, acc_ap, acc_db, pre=(), ctr=None,
                  loader=None, sinker=None, prefetch=None):
    s = cx.s
    TB = cx.TB
    nblk = S // TB
    ntt = TB // 128
    hview = hnT_ap.rearrange("(c p) t -> p c t", p=128) if hnT_ap is not None else None
    pre = list(pre)
    per_blk = (len(pre) + nblk - 1) // nblk if pre else 0
    if ctr is None:
        ctr = {"blk": 0, "tile": 0, "fb": 0}

    def gu(b):
        k = ctr["blk"] + b
        hb = cx.hnTb[k % 2]
        if loader is not None:
            loader(b, hb)
        else:
            s.dma("sp", hb[:], hview[:, :, b * TB:(b + 1) * TB], hb.name,
                  reads=[hnT_db[b * ntt + i] for i in range(ntt)], writes=[hb])
        hT = cx.hT[k % 2]
        for fb in range(NFB):
            q = ctr["fb"]
            ctr["fb"] += 1
            pg = cx.ps[(2 * q) % 4]
            pu = cx.ps[(2 * q + 1) % 4]
            sg = cx.sg[q % 2]
            for c in range(DC):
                s.op("pe", lambda e, c=c, pg=pg, fb=fb: e.matmul(
                    pg[:, :TB], lhsT=w.wg.t[:, c, fb * 128:(fb + 1) * 128], rhs=hb[:, c, :],
                    start=(c == 0), stop=(c == DC - 1)), reads=[w.wg_c[c], hb], writes=[pg])
            for c in range(DC):
                s.op("pe", lambda e, c=c, pu=pu, fb=fb: e.matmul(
                    pu[:, :TB], lhsT=w.wu.t[:, c, fb * 128:(fb + 1) * 128], rhs=hb[:, c, :],
                    start=(c == 0), stop=(c == DC - 1)), reads=[w.wu_c[c], hb], writes=[pu])
            s.op("act", lambda e, pg=pg, sg=sg: e.activation(out=sg[:], in_=pg[:, :TB], func=AF.Silu),
                 reads=[pg], writes=[sg])
            s.op("dve", lambda e, pu=pu, sg=sg, fb=fb: e.tensor_tensor(
                out=hT[:, fb, :], in0=pu[:, :TB], in1=sg[:], op=ALU.mult),
                reads=[pu, sg], writes=[hT])

    def down(b):
        k = ctr["blk"] + b
        hT = cx.hT[k % 2]
        for tt in range(ntt):
            tile = b * ntt + tt
            q = ctr["tile"]
            ctr["tile"] += 1
            pd = (cx.ps[4 + (q % 2) * 2], cx.ps[5 + (q % 2) * 2])
            for half in range(2):
                for fb in range(NFB):
                    s.op("pe", lambda e, half=half, fb=fb, tt=tt, pd=pd: e.matmul(
                        pd[half][:], lhsT=hT[:, fb, tt * 128:(tt + 1) * 128],
                        rhs=w.wd.t[:, fb, half * 512:(half + 1) * 512],
                        start=(fb == 0), stop=(fb == NFB - 1)),
                        reads=[hT, w.wd_c[fb]], writes=[pd[half]])
            j = q % 3
            stg, sa, sb_ = cx.stg[j], cx.stgA[j], cx.stgB[j]
            if sinker is not None:
                sinker(tile, pd, stg, sa, sb_)
                continue
            if cw is None:
                s.op("act", lambda e, stg=stg, pd=pd: e.copy(out=stg[:, 0:512], in_=pd[0][:]),
                     reads=[pd[0]], writes=[sa])
                s.op("dve", lambda e, stg=stg, pd=pd: e.tensor_copy(out=stg[:, 512:1024], in_=pd[1][:]),
                     reads=[pd[1]], writes=[sb_])
            else:
                col = cw_col(tile)
                s.op("act", lambda e, stg=stg, col=col, pd=pd: e.activation(
                    out=stg[:, 0:512], in_=pd[0][:], func=AF.Copy, scale=cw[:, col:col + 1]),
                    reads=[pd[0], cw], writes=[sa])
                s.op("dve", lambda e, stg=stg, col=col, pd=pd: e.tensor_scalar(
                    out=stg[:, 512:1024], in0=pd[1][:], scalar1=cw[:, col:col + 1], scalar2=None,
                    op0=ALU.mult), reads=[pd[1], cw], writes=[sb_])
            s.dma("pool", acc_ap[tile * 128:(tile + 1) * 128, :], stg[:], stg.name,
                  reads=[sa, sb_, acc_db[tile]], writes=[acc_db[tile]], accum_op=ALU.add)

    if prefetch is not None:
        prefetch(0)
        if nblk > 1:
            prefetch(1)
    gu(0)
    for b in range(nblk):
        if prefetch is not None and b + 2 < nblk:
            prefetch(b + 2)
        if b + 1 < nblk:
            gu(b + 1)
        down(b)
        for _ in range(per_blk):
            if pre:
                pre.pop(0)()
    while pre:
        pre.pop(0)()
    ctr["blk"] += nblk
    return ctr


GQ, GK, GV, GR, GLF, GLB = 0, 512, 1024, 2048, 3072, 3088
GW = 3104
NH = 4
import os as _os
_GSTOP = int(_os.environ.get('GSTOP', '-1'))


def tri(cx, name, val, pattern, cm, cmp, dtype=F32, ncols=128):
    s = cx.s
    b = s.sbuf(name, [128, ncols], F32)
    s.op("pool", lambda e: e.memset(b[:], val), writes=[b])
    s.op("pool", lambda e: e.affine_select(out=b[:, 0:128], in_=b[:, 0:128], pattern=[[pattern, 128]],
                                           compare_op=cmp, fill=0.0, base=0, channel_multiplier=cm),
         reads=[b], writes=[b])
    return b


def alloc_gla(cx, P):
    s = cx.s
    g = cx.g = Ctx()
    c16 = -1.0 / 16.0
    g.Rf = s.sbuf("Rf", [128, 257], F32)
    g.Rb = s.sbuf("Rb", [128, 257], F32)
    uf = tri(cx, "Uf", c16, 1, -1, ALU.is_ge)
    ub = tri(cx, "Ub", c16, -1, 1, ALU.is_ge)
    for R, U, ref in ((g.Rf, uf, 64), (g.Rb, ub, 63)):
        s.op("pool", lambda e, R=R: e.memset(R[:, 256:257], c16), writes=[R])
        s.op("dve", lambda e, R=R, U=U: e.tensor_copy(out=R[:, 0:128], in_=U[:]), reads=[U, R], writes=[R])
        s.op("dve", lambda e, R=R, U=U, ref=ref: e.tensor_scalar(
            out=R[:, 128:256], in0=U[:], scalar1=U[:, ref:ref + 1], scalar2=None, op0=ALU.subtract),
            reads=[U, R], writes=[R])
    g.Mkf = tri(cx, "Mkf", c16, -1, 1, ALU.is_gt)
    g.Mkb = tri(cx, "Mkb", c16, 1, -1, ALU.is_gt)
    g.maskf = tri(cx, "maskf", 1.0, 1, -1, ALU.is_ge)
    g.maskb = tri(cx, "maskb", 1.0, -1, 1, ALU.is_gt)
    g.Win = s.sbuf("Win", [128, DC, GW], BF16)
    g.Win_c = [sub(g.Win, "Win_%d" % c) for c in range(DC)]
    for c in range(DC):
        s.dma("pool", g.Win.t[:, c, :], P["gla_in"][c * 128:(c + 1) * 128, :], g.Win_c[c].name,
              writes=[g.Win_c[c]])
    g.Wout = s.sbuf("Wout", [128, DC, D], BF16)
    g.Wout_c = [sub(g.Wout, "Wout_%d" % c) for c in range(DC)]
    for c in range(DC):
        s.dma("pool", g.Wout.t[:, c, :], P["gla_out"][c * 128:(c + 1) * 128, :], g.Wout_c[c].name,
              writes=[g.Wout_c[c]])
    g.wga = []
    for d, (wk, bk) in enumerate((("gw_f", "gb_f"), ("gw_b", "gb_b"))):
        wa = s.sbuf("wga%d" % d, [17, 512], F32)
        wa1 = sub(wa, "wga%d_b" % d)
        s.dma("sp", wa[0:16, :], P[wk], wa.name, writes=[wa])
        s.dma("sp", wa[16:17, :], P[bk].rearrange("(o n) -> o n", o=1), wa1.name, writes=[wa1])
        g.wga.append((wa, wa1))
    g.gain1 = s.sbuf("gain1", [128, D], F32)
    s.dma("sp", g.gain1[:], bcast_row(P["mix_norm0"]), "gain1", writes=[g.gain1])
    g.gain2 = s.sbuf("gain2", [128, D], F32)
    s.dma("sp", g.gain2[:], bcast_row(P["ffn_norm0"]), "gain2", writes=[g.gain2])
    g.hgain = s.sbuf("hgain", [128, D], F32)
    s.dma("sp", g.hgain[:], bcast_row(P["gla_hn"]), "hgain", writes=[g.hgain])
    g.one_c = s.sbuf("one_c", [128, 1], F32)
    s.op("pool", lambda e: e.memset(g.one_c[:], 1.0), writes=[g.one_c])
    g.eps256 = s.sbuf("eps256", [128, 1], F32)
    s.op("pool", lambda e: e.memset(g.eps256[:], EPS), writes=[g.eps256])
    g.x = s.sbuf("gx", [128, D], F32)
    g.hn = s.sbuf("ghn", [128, D], BF16)
    g.hnT = s.sbuf("ghnT", [128, DC, 128], BF16)
    g.junk = s.sbuf("gjunk", [128, D], F32)
    g.ssq = s.sbuf("gssq", [128, 1], F32)
    g.rstd = s.sbuf("grstd", [128, 1], F32)
    g.vbf = s.sbuf("gvbf", [128, D], BF16)
    g.er = s.sbuf("ger", [128, D], F32)
    g.rbf = s.sbuf("grbf", [128, D], BF16)
    g.lrT = []
    for d in range(2):
        b = s.sbuf("glrT%d" % d, [17, 128], F32)
        s.op("pool", lambda e, b=b: e.memset(b[:], 1.0), writes=[b])
        g.lrT.append(b)
    g.la = [s.sbuf("gla%d" % d, [128, 512], F32) for d in range(2)]
    g.Ekd = [s.sbuf("gEkd%d" % d, [128, 512], F32) for d in range(2)]
    g.kd = [s.sbuf("gkd%d" % d, [128, 512], BF16) for d in range(2)]
    g.Eall = [[s.sbuf("gEall%d_%d" % (d, h), [128, 257], F32) for h in range(NH)] for d in range(2)]
    g.E2 = [[s.sbuf("gE2%d_%d" % (d, h), [128, 128], F32) for h in range(NH)] for d in range(2)]
    g.qe = [[s.sbuf("gqe%d_%d" % (d, h), [128, 128], BF16) for h in range(NH)] for d in range(2)]
    g.qb = [[s.sbuf("gqb%d_%d" % (d, h), [128, 128], BF16) for h in range(NH)] for d in range(2)]
    g.ke = [[s.sbuf("gke%d_%d" % (d, h), [128, 128], BF16) for h in range(NH)] for d in range(2)]
    g.PT = [[s.sbuf("gPT%d_%d" % (d, h), [128, 128], BF16) for h in range(NH)] for d in range(2)]
    g.dec = [s.sbuf("gdec%d" % d, [128, NH], F32) for d in range(2)]
    g.S = [[s.sbuf("gS%d_%d" % (d, h), [128, 256], F32) for h in range(NH)] for d in range(2)]
    for d in range(2):
        for h in range(NH):
            s.op("pool", lambda e, b=g.S[d][h]: e.memset(b[:], 0.0), writes=[g.S[d][h]])
    g.Sbf = [s.sbuf("gSbf%d" % d, [128, NH, 256], BF16) for d in range(2)]
    g.Sbf_h = [[sub(g.Sbf[d], "gSbf%d_%d" % (d, h)) for h in range(NH)] for d in range(2)]
    for d in range(2):
        s.op("pool", lambda e, b=g.Sbf[d]: e.memset(b[:], 0.0), writes=[g.Sbf[d]] + g.Sbf_h[d])
    g.ssq4 = s.sbuf("gssq4", [128, NH], F32)
    g.rstd4 = s.sbuf("grstd4", [128, NH], F32)
    g.og = s.sbuf("gog", [128, D], F32)
    g.og_h = [sub(g.og, "gog_%d" % h) for h in range(NH)]
    g.sig = s.sbuf("gsig", [128, D], F32)
    g.gated = s.sbuf("ggated", [128, D], BF16)
    g.gT = s.sbuf("ggT", [128, DC, 128], BF16)
    g.h1 = s.sbuf("gh1", [128, D], F32)
    g.h1A = sub(g.h1, "gh1A")
    g.h1B = sub(g.h1, "gh1B")
    g.hn2 = s.sbuf("ghn2", [128, D], BF16)
    g.hn2T = s.sbuf("ghn2T", [128, DC, 128], BF16)


def emit_gla_tile(cx, t, full, x_ap, Sb_ap, Sb_db, h1_ap, h1_db, hnT_ap, hnT_db):
    s = cx.s
    g = cx.g
    ps = cx.ps
    W = g.Win.t
    rows = slice(t * 128, (t + 1) * 128)
    s.dma("sp", g.x[:], x_ap[rows, :], "gx", writes=[g.x])
    emit_norm_T(cx, g.x, g.gain1, g.hn, ps[0], g.hnT, g.ssq, g.rstd, g.junk)
    hnT = g.hnT
    dirs = (0, 1) if full else (1,)
    upd_dirs = (0,) if full else (1,)

    def proj_tok(bank, col0, n=512):
        for c in range(DC):
            s.op("pe", lambda e, c=c: e.matmul(bank[:, 0:n], lhsT=hnT[:, c, :], rhs=W[:, c, col0:col0 + n],
                                               start=(c == 0), stop=(c == DC - 1)),
                 reads=[hnT, g.Win_c[c]], writes=[bank])

    def proj_feat(bank, bcol, col0, m):
        for c in range(DC):
            s.op("pe", lambda e, c=c: e.matmul(bank[0:m, bcol:bcol + 128], lhsT=W[:, c, col0:col0 + m],
                                               rhs=hnT[:, c, :], start=(c == 0), stop=(c == DC - 1)),
                 reads=[hnT, g.Win_c[c]], writes=[bank])

    proj_tok(ps[1], GK)
    proj_tok(ps[2], GV)
    proj_tok(ps[3], GV + 512)
    if full:
        proj_tok(ps[4], GR)
        proj_tok(ps[5], GR + 512)
        for h in range(NH):
            proj_feat(ps[6], h * 128, GQ + h * 128, 128)
        for h in range(NH):
            proj_feat(ps[7], h * 128, GK + h * 128, 128)
    for d in dirs:
        proj_feat(ps[0], d * 128, GLF + 16 * d, 16)
    if _GSTOP == 0 and full:
        return
    s.op("act", lambda e: e.copy(out=g.vbf[:, 0:512], in_=ps[2][:]), reads=[ps[2]], writes=[g.vbf])
    s.op("act", lambda e: e.copy(out=g.vbf[:, 512:1024], in_=ps[3][:]), reads=[ps[3], g.vbf], writes=[g.vbf])
    if full:
        for hh in range(2):
            sl = slice(hh * 512, (hh + 1) * 512)
            s.op("act", lambda e, hh=hh, sl=sl: e.activation(out=g.er[:, sl], in_=ps[4 + hh][:], func=AF.Exp,
                                                             scale=-1.0),
                 reads=[ps[4 + hh], g.er], writes=[g.er])
            s.op("dve", lambda e, hh=hh, sl=sl: e.tensor_copy(out=g.rbf[:, sl], in_=ps[4 + hh][:]),
                 reads=[ps[4 + hh], g.rbf], writes=[g.rbf])
        s.op("act", lambda e: e.activation(out=g.er[:], in_=g.er[:], func=AF.Ln, bias=g.one_c[:, 0:1]),
             reads=[g.er, g.one_c], writes=[g.er])
        s.op("act", lambda e: e.activation(out=g.sig[:], in_=g.er[:], func=AF.Exp, scale=-1.0),
             reads=[g.er], writes=[g.sig])
        s.op("dve", lambda e: e.tensor_tensor(out=g.sig[:], in0=g.sig[:], in1=g.rbf[:], op=ALU.mult),
             reads=[g.sig, g.rbf], writes=[g.sig])
    for d in dirs:
        s.op("dve", lambda e, d=d: e.tensor_copy(out=g.lrT[d][0:16, :], in_=ps[0][0:16, d * 128:(d + 1) * 128]),
             reads=[ps[0], g.lrT[d]], writes=[g.lrT[d]])
    if _GSTOP == 1 and full:
        return
    for d in dirs:
        zb = ps[2 + d]
        s.op("pe", lambda e, d=d, zb=zb: e.matmul(zb[:], lhsT=g.lrT[d][:], rhs=g.wga[d][0][:], start=True, stop=True),
             reads=[g.lrT[d], g.wga[d][0], g.wga[d][1]], writes=[zb])
        s.op("act", lambda e, d=d, zb=zb: e.activation(out=g.la[d][:], in_=zb[:], func=AF.Exp, scale=-1.0),
             reads=[zb], writes=[g.la[d]])
        s.op("act", lambda e, d=d: e.activation(out=g.la[d][:], in_=g.la[d][:], func=AF.Ln, bias=g.one_c[:, 0:1]),
             reads=[g.la[d], g.one_c], writes=[g.la[d]])
    if _GSTOP == 2 and full:
        return
    for d in upd_dirs:
        Mk = g.Mkf if d == 0 else g.Mkb
        xb = ps[2 + d]
        s.op("pe", lambda e, d=d, Mk=Mk, xb=xb: e.matmul(xb[:], lhsT=Mk[:], rhs=g.la[d][:], start=True, stop=True),
             reads=[Mk, g.la[d]], writes=[xb])
        s.op("act", lambda e, d=d, xb=xb: e.activation(out=g.Ekd[d][:], in_=xb[:], func=AF.Exp),
             reads=[xb], writes=[g.Ekd[d]])
        s.op("dve", lambda e, d=d: e.tensor_tensor(out=g.kd[d][:], in0=ps[1][:], in1=g.Ekd[d][:], op=ALU.mult),
             reads=[ps[1], g.Ekd[d]], writes=[g.kd[d]])
    cnt = 0
    for d in dirs:
        R = g.Rf if d == 0 else g.Rb
        for h in range(NH):
            cb = ps[4 + (cnt % 2)]
            cnt += 1
            if full:
                s.op("pe", lambda e, d=d, h=h, cb=cb, R=R: e.matmul(
                    cb[:, 0:257], lhsT=g.la[d][:, h * 128:(h + 1) * 128], rhs=R[:], start=True, stop=True),
                    reads=[g.la[d], R], writes=[cb])
                s.op("act", lambda e, d=d, h=h, cb=cb: e.activation(out=g.Eall[d][h][:], in_=cb[:, 0:257], func=AF.Exp),
                     reads=[cb], writes=[g.Eall[d][h]])
                s.op("act", lambda e, d=d, h=h, cb=cb: e.activation(out=g.E2[d][h][:], in_=cb[:, 128:256], func=AF.Exp,
                                                                   scale=-1.0),
                     reads=[cb], writes=[g.E2[d][h]])
                s.op("dve", lambda e, d=d, h=h: e.tensor_copy(out=g.dec[d][:, h:h + 1], in_=g.Eall[d][h][:, 256:257]),
                     reads=[g.Eall[d][h], g.dec[d]], writes=[g.dec[d]])
            else:
                s.op("pe", lambda e, d=d, h=h, cb=cb, R=R: e.matmul(
                    cb[:, 0:1], lhsT=g.la[d][:, h * 128:(h + 1) * 128], rhs=R[:, 256:257], start=True, stop=True),
                    reads=[g.la[d], R], writes=[cb])
                s.op("act", lambda e, d=d, h=h, cb=cb: e.activation(out=g.dec[d][:, h:h + 1], in_=cb[:, 0:1], func=AF.Exp),
                     reads=[cb, g.dec[d]], writes=[g.dec[d]])
    if full:
        if _GSTOP == 3 and full:
            return
        sc = float(128 ** -0.5)
        for d in dirs:
            for h in range(NH):
                qps = ps[6][:, h * 128:(h + 1) * 128]
                kps = ps[7][:, h * 128:(h + 1) * 128]
                E = g.Eall[d][h]
                s.op("dve", lambda e, d=d, h=h, qps=qps, E=E: e.scalar_tensor_tensor(
                    out=g.qb[d][h][:], in0=qps, scalar=sc, in1=E[:, 0:128], op0=ALU.mult, op1=ALU.mult),
                    reads=[ps[6], E], writes=[g.qb[d][h]])
                s.op("dve", lambda e, d=d, h=h, qps=qps, E=E: e.scalar_tensor_tensor(
                    out=g.qe[d][h][:], in0=qps, scalar=sc, in1=E[:, 128:256], op0=ALU.mult, op1=ALU.mult),
                    reads=[ps[6], E], writes=[g.qe[d][h]])
                s.op("dve", lambda e, d=d, h=h, kps=kps: e.tensor_tensor(
                    out=g.ke[d][h][:], in0=kps, in1=g.E2[d][h][:], op=ALU.mult),
                    reads=[ps[7], g.E2[d][h]], writes=[g.ke[d][h]])
        s.dma("sp", g.Sbf[1][:].rearrange("p h v -> p (h v)"), Sb_ap[t], "gSbf1", reads=[Sb_db[t]], writes=[g.Sbf[1]] + g.Sbf_h[1])
        if _GSTOP == 4 and full:
            return
        for d in dirs:
            mask = g.maskf if d == 0 else g.maskb
            sb_ = ps[2 + d]
            for h in range(NH):
                s.op("pe", lambda e, d=d, h=h, sb_=sb_: e.matmul(
                    sb_[:, h * 128:(h + 1) * 128], lhsT=g.ke[d][h][:], rhs=g.qe[d][h][:], start=True, stop=True),
                    reads=[g.ke[d][h], g.qe[d][h]], writes=[sb_])
            for h in range(NH):
                s.op("dve", lambda e, d=d, h=h, sb_=sb_, mask=mask: e.tensor_tensor(
                    out=g.PT[d][h][:], in0=sb_[:, h * 128:(h + 1) * 128], in1=mask[:], op=ALU.mult),
                    reads=[sb_, mask], writes=[g.PT[d][h]])
        if _GSTOP == 5 and full:
            return
        for h in range(NH):
            ob = ps[4 + h // 2]
            oc = slice((h % 2) * 256, (h % 2) * 256 + 256)
            vs = g.vbf[:, h * 256:(h + 1) * 256]
            s.op("pe", lambda e, h=h, ob=ob, oc=oc, vs=vs: e.matmul(ob[:, oc], lhsT=g.PT[0][h][:], rhs=vs,
                                                                   start=True, stop=False),
                 reads=[g.PT[0][h], g.vbf], writes=[ob])
            s.op("pe", lambda e, h=h, ob=ob, oc=oc, vs=vs: e.matmul(ob[:, oc], lhsT=g.PT[1][h][:], rhs=vs,
                                                                   start=False, stop=False),
                 reads=[g.PT[1][h], g.vbf], writes=[ob])
            s.op("pe", lambda e, h=h, ob=ob, oc=oc: e.matmul(ob[:, oc], lhsT=g.qb[0][h][:], rhs=g.Sbf[0][:, h, :],
                                                            start=False, stop=False),
                 reads=[g.qb[0][h], g.Sbf_h[0][h]], writes=[ob])
            s.op("pe", lambda e, h=h, ob=ob, oc=oc: e.matmul(ob[:, oc], lhsT=g.qb[1][h][:], rhs=g.Sbf[1][:, h, :],
                                                            start=False, stop=True),
                 reads=[g.qb[1][h], g.Sbf_h[1][h]], writes=[ob])
        if _GSTOP == 6 and full:
            return
        for h in range(NH):
            ob = ps[4 + h // 2]
            oc = slice((h % 2) * 256, (h % 2) * 256 + 256)
            s.op("act", lambda e, h=h, ob=ob, oc=oc: e.activation(
                out=g.junk[:, h * 256:(h + 1) * 256], in_=ob[:, oc], func=AF.Square, scale=1.0 / 16.0,
                accum_out=g.ssq4[:, h:h + 1]), reads=[ob, g.junk, g.ssq4], writes=[g.junk, g.ssq4])
        s.op("act", lambda e: e.activation(out=g.rstd4[:], in_=g.ssq4[:], func=AF.Ln, bias=g.eps256[:, 0:1]),
             reads=[g.ssq4, g.eps256], writes=[g.rstd4])
        s.op("act", lambda e: e.activation(out=g.rstd4[:], in_=g.rstd4[:], func=AF.Exp, scale=-0.5),
             reads=[g.rstd4], writes=[g.rstd4])
        for h in range(NH):
            ob = ps[4 + h // 2]
            oc = slice((h % 2) * 256, (h % 2) * 256 + 256)
            hs = slice(h * 256, (h + 1) * 256)
            s.op("dve", lambda e, h=h, ob=ob, oc=oc, hs=hs: e.scalar_tensor_tensor(
                out=g.og[:, hs], in0=ob[:, oc], scalar=g.rstd4[:, h:h + 1], in1=g.hgain[:, hs],
                op0=ALU.mult, op1=ALU.mult), reads=[ob, g.rstd4, g.hgain], writes=[g.og_h[h]])
        s.op("dve", lambda e: e.tensor_tensor(out=g.gated[:], in0=g.og[:], in1=g.sig[:], op=ALU.mult),
             reads=g.og_h + [g.sig], writes=[g.gated])
        if _GSTOP == 7 and full:
            return
        psT = Bview(ps[0])
        for c in range(DC):
            s.op("pe", lambda e, c=c: e.transpose(out=psT[:, c * 128:(c + 1) * 128],
                                                  in_=g.gated[:, c * 128:(c + 1) * 128], identity=cx.ident[:]),
                 reads=[g.gated, cx.ident], writes=[ps[0]])
        s.op("act", lambda e: e.copy(out=g.gT[:].rearrange("p c t -> p (c t)"), in_=psT[:]),
             reads=[ps[0]], writes=[g.gT])
        for hh in range(2):
            yb = ps[2 + hh]
            for c in range(DC):
                s.op("pe", lambda e, c=c, hh=hh, yb=yb: e.matmul(
                    yb[:], lhsT=g.gT[:, c, :], rhs=g.Wout.t[:, c, hh * 512:(hh + 1) * 512],
                    start=(c == 0), stop=(c == DC - 1)), reads=[g.gT, g.Wout_c[c]], writes=[yb])
        s.op("dve", lambda e: e.tensor_tensor(out=g.h1[:, 0:512], in0=ps[2][:], in1=g.x[:, 0:512], op=ALU.add),
             reads=[ps[2], g.x], writes=[g.h1A])
        s.op("dve", lambda e: e.tensor_tensor(out=g.h1[:, 512:1024], in0=ps[3][:], in1=g.x[:, 512:1024], op=ALU.add),
             reads=[ps[3], g.x], writes=[g.h1B])
        s.dma("sp", h1_ap[rows, :], g.h1[:], "gh1", reads=[g.h1A, g.h1B], writes=[h1_db[t]])
        emit_norm_T(cx, g.h1, g.gain2, g.hn2, ps[0], g.hn2T, g.ssq, g.rstd, g.junk, xdeps=[g.h1A, g.h1B])
        s.dma("sp", hnT_ap.rearrange("(c p) t -> p c t", p=128)[:, :, rows], g.hn2T[:], "ghn2T",
              reads=[g.hn2T], writes=[hnT_db[t]])
    else:
        s.dma("sp", Sb_ap[t], g.Sbf[1][:].rearrange("p h v -> p (h v)"), "gSbf1", reads=g.Sbf_h[1], writes=[Sb_db[t]])
    if _GSTOP == 8 and full:
        return
    for d in upd_dirs:
        for h in range(NH):
            ub = ps[4 + h // 2] if full else ps[6 + h // 2]
            uc = slice((h % 2) * 256, (h % 2) * 256 + 256)
            s.op("pe", lambda e, d=d, h=h, ub=ub, uc=uc: e.matmul(
                ub[:, uc], lhsT=g.kd[d][:, h * 128:(h + 1) * 128], rhs=g.vbf[:, h * 256:(h + 1) * 256],
                start=True, stop=True), reads=[g.kd[d], g.vbf], writes=[ub])
            s.op("dve", lambda e, d=d, h=h, ub=ub, uc=uc: e.scalar_tensor_tensor(
                out=g.S[d][h][:], in0=g.S[d][h][:], scalar=g.dec[d][:, h:h + 1], in1=ub[:, uc],
                op0=ALU.mult, op1=ALU.add), reads=[g.S[d][h], g.dec[d], ub], writes=[g.S[d][h]])
            if (d == 0) or (not full):
                s.op("pool", lambda e, d=d, h=h: e.tensor_copy(out=g.Sbf[d][:, h, :], in_=g.S[d][h][:]),
                     reads=[g.S[d][h]], writes=[g.Sbf_h[d][h]])


NQH, NKV, HD = 16, 4, 64
_PH = int(_os.environ.get('PH', '5'))
NEG = -1.0e30


def alloc_swa(cx, P, NT):
    s = cx.s
    a = cx.a = Ctx()
    a.W = s.sbuf("aW", [128, DC, 1536], BF16)
    a.W_c = [sub(a.W, "aW_%d" % c) for c in range(DC)]
    for c in range(DC):
        s.dma("pool", a.W.t[:, c, :], P["swa_qkv"][c * 128:(c + 1) * 128, :], a.W_c[c].name, writes=[a.W_c[c]])
    a.Wo = s.sbuf("aWo", [128, DC, D], BF16)
    a.Wo_c = [sub(a.Wo, "aWo_%d" % c) for c in range(DC)]
    for c in range(DC):
        s.dma("pool", a.Wo.t[:, c, :], P["swa_out"][c * 128:(c + 1) * 128, :], a.Wo_c[c].name, writes=[a.Wo_c[c]])
    a.Wr = s.sbuf("aWr", [128, DC, NE], BF16)
    s.dma("pool", a.Wr[:], P["router"].rearrange("(c p) e -> p c e", p=128), "aWr", writes=[a.Wr])
    a.brow = s.sbuf("abrow", [1, 1536], F32)
    s.dma("sp", a.brow[:], P["swa_qkv_b"].rearrange("(o n) -> o n", o=1), "abrow", writes=[a.brow])
    a.ones = s.sbuf("aones", [1, 128], F32)
    s.op("pool", lambda e: e.memset(a.ones[:], 1.0), writes=[a.ones])
    a.bout = s.sbuf("about", [128, D], F32)
    s.dma("sp", a.bout[:], bcast_row(P["swa_out_b"]), "about", writes=[a.bout])
    a.sink = s.sbuf("asink", [128, NQH], F32)
    s.dma("sp", a.sink[:], bcast_row(P["sinks"]), "asink", writes=[a.sink])
    a.gain3 = s.sbuf("again3", [128, D], F32)
    s.dma("sp", a.gain3[:], bcast_row(P["mix_norm1"]), "again3", writes=[a.gain3])
    a.gain4 = s.sbuf("again4", [128, D], F32)
    s.dma("sp", a.gain4[:], bcast_row(P["ffn_norm1"]), "again4", writes=[a.gain4])
    a.dist = s.sbuf("adist", [128, 384], F32)
    a.disti = s.sbuf("adisti", [128, 384], I32)
    s.op("pool", lambda e: e.iota(a.disti[:], pattern=[[-1, 384]], base=128, channel_multiplier=1),
         writes=[a.disti])
    s.op("dve", lambda e: e.tensor_copy(out=a.dist[:], in_=a.disti[:]), reads=[a.disti], writes=[a.dist])
    a.ndist = s.sbuf("andist", [128, 384], F32)
    s.op("dve", lambda e: e.tensor_scalar(out=a.ndist[:], in0=a.dist[:], scalar1=-1.0, scalar2=None, op0=ALU.mult),
         reads=[a.dist], writes=[a.ndist])
    s.op("dve", lambda e: e.tensor_tensor(out=a.dist[:], in0=a.dist[:], in1=a.ndist[:], op=ALU.max),
         reads=[a.dist, a.ndist], writes=[a.dist])
    a.wmask = s.sbuf("awmask", [128, 384], F32)
    s.op("dve", lambda e: e.tensor_scalar(out=a.wmask[:], in0=a.dist[:], scalar1=128.0, scalar2=NEG,
                                          op0=ALU.is_gt, op1=ALU.mult), reads=[a.dist], writes=[a.wmask])
    a.bias = []
    for h in range(NQH):
        b = s.sbuf("abias%d" % h, [128, 384], F32)
        slope = float(np.float32(2.0 ** (-8.0 * (h + 1) / NQH)))
        s.op("dve", lambda e, b=b, slope=slope: e.scalar_tensor_tensor(
            out=b[:], in0=a.dist[:], scalar=-slope, in1=a.wmask[:], op0=ALU.mult, op1=ALU.add),
            reads=[a.dist, a.wmask], writes=[b])
        a.bias.append(b)
    a.x = [s.sbuf("ax%d" % i, [128, D], F32) for i in range(3)]
    a.qT = [s.sbuf("aqT%d" % i, [64, NQH, 128], BF16) for i in range(3)]
    a.kT = [s.sbuf("akT%d" % i, [64, NKV, 128], BF16) for i in range(4)]
    a.v = [s.sbuf("av%d" % i, [128, NKV * HD], BF16) for i in range(4)]
    a.qkv = s.sbuf("aqkv", [128, 1536], BF16)
    a.qkvA = sub(a.qkv, "aqkvA")
    a.qkvB = sub(a.qkv, "aqkvB")
    a.qkvC = sub(a.qkv, "aqkvC")
    a.hn = s.sbuf("ahn", [128, D], BF16)
    a.hnT = s.sbuf("ahnT", [128, DC, 128], BF16)
    a.junk = s.sbuf("ajunk", [128, D], F32)
    a.ssq = s.sbuf("assq", [128, 1], F32)
    a.rstd = s.sbuf("arstd", [128, 1], F32)
    a.sc = [s.sbuf("asc%d" % i, [128, 384], F32) for i in range(2)]
    a.p = [s.sbuf("ap%d" % i, [128, 384], BF16) for i in range(2)]
    a.pT = [s.sbuf("apT%d" % i, [128, 384], BF16) for i in range(2)]
    a.st = [s.sbuf("ast%d" % i, [128, 8], F32) for i in range(2)]
    a.sc4 = [s.sbuf("asc4%d" % i, [128, 4, 384], F32) for i in range(2)]
    a.p4 = [s.sbuf("ap4%d" % i, [128, 4, 384], BF16) for i in range(2)]
    a.pT4 = [s.sbuf("apT4%d" % i, [128, 4, 384], BF16) for i in range(2)]
    a.st4 = [s.sbuf("ast4%d" % i, [128, 24], F32) for i in range(2)]
    a.negsink = s.sbuf("anegsink", [128, NQH], F32)
    s.op("dve", lambda e: e.tensor_scalar(out=a.negsink[:], in0=a.sink[:], scalar1=-1.0, scalar2=None, op0=ALU.mult),
         reads=[a.sink], writes=[a.negsink])
    a.attn = s.sbuf("aattn", [128, D], BF16)
    a.attn_h = [sub(a.attn, "aattn_%d" % h) for h in range(NQH)]
    a.aT = s.sbuf("aaT", [128, DC, 128], BF16)
    a.h3 = s.sbuf("ah3", [128, D], F32)
    a.h3A = sub(a.h3, "ah3A")
    a.h3B = sub(a.h3, "ah3B")
    a.hn4 = s.sbuf("ahn4", [128, D], BF16)
    a.hn4T = s.sbuf("ahn4T", [128, DC, 128], BF16)
    a.rt = s.sbuf("art", [128, 8 * 8], F32)


def swa_produce(cx, t, h2_ap, h2_db, part=None):
    s = cx.s
    a = cx.a
    ps = cx.ps
    x = a.x[t % 3]
    if part in (None, 0):
        s.dma("sp", x[:], h2_ap[t * 128:(t + 1) * 128, :], x.name, reads=[h2_db[t]], writes=[x])
        emit_norm_T(cx, x, a.gain3, a.hn, ps[0], a.hnT, a.ssq, a.rstd, a.junk)
    W = a.W.t
    qkv = a.qkv
    if part in (None, 1):
        for i, bank in enumerate((ps[1], ps[2], ps[3])):
            cs = slice(i * 512, (i + 1) * 512)
            for c in range(DC):
                s.op("pe", lambda e, c=c, bank=bank, cs=cs: e.matmul(
                    bank[:], lhsT=a.hnT[:, c, :], rhs=W[:, c, cs], start=(c == 0), stop=False),
                    reads=[a.hnT, a.W_c[c]], writes=[bank])
            s.op("pe", lambda e, bank=bank, cs=cs: e.matmul(
                bank[:], lhsT=a.ones[0:1, :], rhs=a.brow[0:1, cs], start=False, stop=True),
                reads=[a.brow, a.ones], writes=[bank])
        s.op("act", lambda e: e.activation(out=qkv[:, 0:512], in_=ps[1][:], func=AF.Copy, scale=0.125),
             reads=[ps[1]], writes=[a.qkvA])
        s.op("dve", lambda e: e.tensor_scalar(out=qkv[:, 512:1024], in0=ps[2][:], scalar1=0.125, scalar2=None,
                                              op0=ALU.mult), reads=[ps[2]], writes=[a.qkvB])
        s.op("act", lambda e: e.copy(out=qkv[:, 1024:1536], in_=ps[3][:]), reads=[ps[3]], writes=[a.qkvC])
    if part not in (None, 2):
        return
    qT = a.qT[t % 3]
    kT = a.kT[t % 4]
    v = a.v[t % 4]
    for half, bank, dep in ((0, ps[1], a.qkvA), (1, ps[2], a.qkvB)):
        tv = Bview(bank)
        for h8 in range(8):
            h = half * 8 + h8
            s.op("pe", lambda e, h=h, h8=h8, tv=tv: e.transpose(
                out=tv[0:64, h8 * 128:(h8 + 1) * 128], in_=qkv[:, h * 64:(h + 1) * 64], identity=cx.ident[:]),
                reads=[dep, cx.ident], writes=[bank])
        dst = qT[:, half * 8:(half + 1) * 8, :].rearrange("p h t -> p (h t)")
        if half == 0:
            s.op("act", lambda e, dst=dst, tv=tv: e.copy(out=dst, in_=tv[0:64, :]), reads=[bank], writes=[qT])
        else:
            s.op("dve", lambda e, dst=dst, tv=tv: e.tensor_copy(out=dst, in_=tv[0:64, :]), reads=[bank, qT], writes=[qT])
    tv = Bview(ps[3])
    for kv in range(NKV):
        s.op("pe", lambda e, kv=kv, tv=tv: e.transpose(
            out=tv[0:64, kv * 128:(kv + 1) * 128], in_=qkv[:, 1024 + kv * 64:1024 + (kv + 1) * 64],
            identity=cx.ident[:]), reads=[a.qkvC, cx.ident], writes=[ps[3]])
    s.op("dve", lambda e, tv=tv: e.tensor_copy(out=kT[:].rearrange("p h t -> p (h t)"), in_=tv[0:64, 0:512]),
         reads=[ps[3]], writes=[kT])
    s.op("pool", lambda e: e.tensor_copy(out=v[:], in_=qkv[:, 1280:1536]), reads=[a.qkvC], writes=[v])


def swa_attend(cx, t, NT, h3_ap, h3_db, hnT_ap, hnT_db, cw_all, hn4tm_ap=None, part=None):
    s = cx.s
    a = cx.a
    ps = cx.ps
    x = a.x[t % 3]
    qT = a.qT[t % 3]
    kts = [kt for kt in (t - 1, t, t + 1) if 0 <= kt < NT]
    c0 = (kts[0] - (t - 1)) * 128
    c1 = (kts[-1] - (t - 1) + 1) * 128
    for g4 in (range(NKV) if part is None else ([part] if part < NKV else [])):
        kv = g4
        j = g4 % 2
        p, pT, st = a.p4[j], a.pT4[j], a.st4[j]
        for hh in range(4):
            h = g4 * 4 + hh
            sb_ = ps[4 + hh]
            for i, kt in enumerate(kts):
                cc = (kt - (t - 1)) * 128
                s.op("pe", lambda e, h=h, kt=kt, cc=cc, sb_=sb_, kv=kv: e.matmul(
                    sb_[:, cc:cc + 128], lhsT=qT[:, h, :], rhs=a.kT[kt % 4][:, kv, :], start=True, stop=True),
                    reads=[qT, a.kT[kt % 4]], writes=[sb_])
        sc = a.sc4[j]
        for hh in range(4):
            h = g4 * 4 + hh
            s.op("dve", lambda e, h=h, hh=hh, sc=sc: e.tensor_tensor(
                out=sc[:, hh, c0:c1], in0=ps[4 + hh][:, c0:c1], in1=a.bias[h][:, c0:c1], op=ALU.add),
                reads=[ps[4 + hh], a.bias[h], sc], writes=[sc])
        for hh in range(4):
            s.op("dve", lambda e, hh=hh, st=st, sc=sc: e.reduce_max(out=st[:, hh:hh + 1], in_=sc[:, hh, c0:c1], axis=AX.X),
                 reads=[sc, st], writes=[st])
        s.op("dve", lambda e, st=st, g4=g4: e.scalar_tensor_tensor(
            out=st[:, 4:8], in0=st[:, 0:4], scalar=-1.0, in1=a.negsink[:, g4 * 4:(g4 + 1) * 4], op0=ALU.mult, op1=ALU.min),
            reads=[st, a.negsink], writes=[st])
        for hh in range(4):
            s.op("act", lambda e, hh=hh, p=p, st=st, sc=sc: e.activation(
                out=p[:, hh, c0:c1], in_=sc[:, hh, c0:c1], func=AF.Exp, bias=st[:, 4 + hh:5 + hh],
                accum_out=st[:, 8 + hh:9 + hh]), reads=[sc, st], writes=[p, st])
        s.op("dve", lambda e, st=st, g4=g4: e.tensor_tensor(out=st[:, 12:16], in0=a.sink[:, g4 * 4:(g4 + 1) * 4],
                                                            in1=st[:, 4:8], op=ALU.add), reads=[st, a.sink], writes=[st])
        s.op("act", lambda e, st=st: e.activation(out=st[:, 12:16], in_=st[:, 12:16], func=AF.Exp), reads=[st], writes=[st])
        s.op("dve", lambda e, st=st: e.tensor_tensor(out=st[:, 16:20], in0=st[:, 8:12], in1=st[:, 12:16], op=ALU.add),
             reads=[st], writes=[st])
        s.op("dve", lambda e, st=st: e.reciprocal(out=st[:, 20:24], in_=st[:, 16:20]), reads=[st], writes=[st])
        for half, tb in ((0, ps[0]), (1, ps[3])):
            tv = Bview(tb)
            for h2 in range(2):
                hh = half * 2 + h2
                for kt in kts:
                    cc = (kt - (t - 1)) * 128
                    s.op("pe", lambda e, cc=cc, hh=hh, h2=h2, p=p, tv=tv: e.transpose(
                        out=tv[:, h2 * 384 + cc:h2 * 384 + cc + 128], in_=p[:, hh, cc:cc + 128], identity=cx.ident[:]),
                        reads=[p, cx.ident], writes=[tb])
            full = (c0 == 0 and c1 == 384)
            segs = [(0, 768, None)] if full else [(h2 * 384 + c0, h2 * 384 + c1, h2) for h2 in range(2)]
            for (x0, x1, h2) in segs:
                if h2 is None:
                    dst = pT[:, half * 2:half * 2 + 2, :].rearrange("p h c -> p (h c)")
                else:
                    dst = pT[:, half * 2 + h2, c0:c1]
                if half == 0:
                    s.op("act", lambda e, dst=dst, tv=tv, x0=x0, x1=x1: e.copy(out=dst, in_=tv[:, x0:x1]),
                         reads=[tb], writes=[pT])
                else:
                    s.op("dve", lambda e, dst=dst, tv=tv, x0=x0, x1=x1: e.tensor_copy(out=dst, in_=tv[:, x0:x1]),
                         reads=[tb, pT], writes=[pT])
        for hh in range(4):
            h = g4 * 4 + hh
            ob = ps[1 + h // 8]
            oc = slice((h % 8) * 64, (h % 8) * 64 + 64)
            for i, kt in enumerate(kts):
                cc = (kt - (t - 1)) * 128
                s.op("pe", lambda e, kt=kt, cc=cc, kv=kv, i=i, ob=ob, oc=oc, pT=pT, hh=hh: e.matmul(
                    ob[:, oc], lhsT=pT[:, hh, cc:cc + 128], rhs=a.v[kt % 4][:, kv * 64:(kv + 1) * 64],
                    start=(i == 0), stop=(i == len(kts) - 1)), reads=[pT, a.v[kt % 4]], writes=[ob])
        for hh in range(4):
            h = g4 * 4 + hh
            ob = ps[1 + h // 8]
            oc = slice((h % 8) * 64, (h % 8) * 64 + 64)
            s.op("dve", lambda e, h=h, hh=hh, ob=ob, oc=oc, st=st: e.tensor_scalar(
                out=a.attn[:, h * 64:(h + 1) * 64], in0=ob[:, oc], scalar1=st[:, 20 + hh:21 + hh], scalar2=None,
                op0=ALU.mult), reads=[ob, st], writes=[a.attn_h[h]])
    if part is not None and part < NKV:
        return
    tb = ps[0]
    tv = Bview(tb)
    for c in range(DC):
        s.op("pe", lambda e, c=c, tv=tv: e.transpose(out=tv[:, c * 128:(c + 1) * 128], in_=a.attn[:, c * 128:(c + 1) * 128],
                                              identity=cx.ident[:]), reads=a.attn_h + [cx.ident], writes=[tb])
    s.op("act", lambda e, tv=tv: e.copy(out=a.aT[:].rearrange("p c t -> p (c t)"), in_=tv[:]), reads=[tb], writes=[a.aT])
    for hh in range(2):
        yb = ps[3 + hh]
        for c in range(DC):
            s.op("pe", lambda e, c=c, hh=hh, yb=yb: e.matmul(
                yb[:], lhsT=a.aT[:, c, :], rhs=a.Wo.t[:, c, hh * 512:(hh + 1) * 512],
                start=(c == 0), stop=(c == DC - 1)), reads=[a.aT, a.Wo_c[c]], writes=[yb])
    for hh, hb in ((0, a.h3A), (1, a.h3B)):
        sl = slice(hh * 512, (hh + 1) * 512)
        s.op("dve", lambda e, hh=hh, sl=sl: e.tensor_tensor(out=a.h3[:, sl], in0=ps[3 + hh][:], in1=x[:, sl], op=ALU.add),
             reads=[ps[3 + hh], x], writes=[hb])
        s.op("pool", lambda e, sl=sl: e.tensor_tensor(out=a.h3[:, sl], in0=a.h3[:, sl], in1=a.bout[:, sl], op=ALU.add),
             reads=[hb, a.bout], writes=[hb])
    s.dma("sp", h3_ap[t * 128:(t + 1) * 128, :], a.h3[:], "ah3", reads=[a.h3A, a.h3B], writes=[h3_db[t]])
    emit_norm_T(cx, a.h3, a.gain4, a.hn4, ps[0], a.hn4T, a.ssq, a.rstd, a.junk, xdeps=[a.h3A, a.h3B])
    s.dma("sp", hnT_ap.rearrange("(c p) t -> p c t", p=128)[:, :, t * 128:(t + 1) * 128], a.hn4T[:], "ahn4T",
          reads=[a.hn4T], writes=[hnT_db[t]])
    if hn4tm_ap is not None:
        s.dma("sp", hn4tm_ap[t * 128:(t + 1) * 128, :], a.hn4[:], "ahn4tm", reads=[a.hn4])
    lb = ps[7]
    for c in range(DC):
        s.op("pe", lambda e, c=c: e.matmul(lb[:, 0:NE], lhsT=a.hn4T[:, c, :], rhs=a.Wr[:, c, :],
                                           start=(c == 0), stop=(c == DC - 1)), reads=[a.hn4T, a.Wr], writes=[lb])
    r = a.rt
    R = lambda i: r[:, i * 8:(i + 1) * 8]
    cwt = cw_all[:, t * NE:(t + 1) * NE]

    def dv(fn, rd=(), wr=()):
        s.op("dve", fn, reads=[a.rt] + list(rd), writes=[a.rt] + list(wr))
    dv(lambda e: e.tensor_copy(out=R(0), in_=lb[:, 0:NE]), rd=[lb])
    dv(lambda e: e.reduce_max(out=r[:, 56:57], in_=R(0), axis=AX.X))
    dv(lambda e: e.tensor_scalar(out=R(1), in0=R(0), scalar1=r[:, 56:57], scalar2=None, op0=ALU.is_equal))
    dv(lambda e: e.scalar_tensor_tensor(out=R(2), in0=R(1), scalar=NEG, in1=R(0), op0=ALU.mult, op1=ALU.add))
    dv(lambda e: e.reduce_max(out=r[:, 57:58], in_=R(2), axis=AX.X))
    dv(lambda e: e.tensor_scalar(out=R(3), in0=R(2), scalar1=r[:, 57:58], scalar2=None, op0=ALU.is_equal))
    dv(lambda e: e.tensor_tensor(out=r[:, 58:59], in0=r[:, 57:58], in1=r[:, 56:57], op=ALU.subtract))
    s.op("act", lambda e: e.activation(out=r[:, 59:60], in_=r[:, 58:59], func=AF.Exp), reads=[a.rt], writes=[a.rt])
    dv(lambda e: e.tensor_scalar(out=r[:, 60:61], in0=r[:, 59:60], scalar1=1.0, scalar2=None, op0=ALU.add))
    dv(lambda e: e.reciprocal(out=r[:, 61:62], in_=r[:, 60:61]))
    dv(lambda e: e.tensor_tensor(out=r[:, 62:63], in0=r[:, 59:60], in1=r[:, 61:62], op=ALU.mult))
    dv(lambda e: e.tensor_scalar(out=R(4), in0=R(1), scalar1=r[:, 61:62], scalar2=None, op0=ALU.mult))
    dv(lambda e: e.scalar_tensor_tensor(out=cwt, in0=R(3), scalar=r[:, 62:63], in1=R(4), op0=ALU.mult, op1=ALU.add),
       wr=[cw_all])


def new_phase(cx, nc):
    cx.s = Sched(nc)
    for b in cx.persist:
        b.lw = None
        b.rd = {}
        b.rd_dma = []
    return cx.s


def build_program(S, TB=256, sparse=True):
    NT = S // 128
    nc = bass.Bass("TRN2", target_bir_lowering=False)

    def din(name, shape):
        return nc.dram_tensor(name, list(shape), F32, kind="ExternalInput").ap()

    x = din("x", [S, D])
    P = {
        "mix_norm0": din("mix_norm0", [D]), "mix_norm1": din("mix_norm1", [D]),
        "ffn_norm0": din("ffn_norm0", [D]), "ffn_norm1": din("ffn_norm1", [D]),
        "gla_in": din("gla_in", [D, GW]), "gw_f": din("gw_f", [16, 512]), "gb_f": din("gb_f", [512]),
        "gw_b": din("gw_b", [16, 512]), "gb_b": din("gb_b", [512]), "gla_hn": din("gla_hn", [D]),
        "gla_out": din("gla_out", [D, D]),
        "swa_qkv": din("swa_qkv", [D, 1536]), "swa_qkv_b": din("swa_qkv_b", [1536]), "sinks": din("sinks", [NQH]),
        "swa_out": din("swa_out", [D, D]), "swa_out_b": din("swa_out_b", [D]),
        "dWg": din("dWg", [D, FF]), "dWu": din("dWu", [D, FF]), "dWd": din("dWd", [FF, D]),
        "router": din("router", [D, NE]),
        "mWg": din("mWg", [NE, D, FF]), "mWu": din("mWu", [NE, D, FF]), "mWd": din("mWd", [NE, FF, D]),
        "final_norm": din("final_norm", [D]),
    }
    out = nc.dram_tensor("out", [S, D], F32, kind="ExternalOutput").ap()
    Sb = nc.dram_tensor("scr_Sb", [NT, 128, NH * 256], BF16).ap()
    h1 = nc.dram_tensor("scr_h1", [S, D], F32).ap()
    h3 = nc.dram_tensor("scr_h3", [S + 128, D], F32).ap()
    hnT = nc.dram_tensor("scr_hnT", [D, S], BF16).ap()
    hnT2 = nc.dram_tensor("scr_hnT2", [D, S], BF16).ap()
    hn4tm = nc.dram_tensor("scr_hn4tm", [S + 128, D], BF16).ap()
    lst = nc.dram_tensor("scr_list", [NE * CAPR + NT * NE * 128, 2], I32).ap()

    cx = Ctx()
    pstack = contextlib.ExitStack()
    Sched.sem_pool = {"sw": [], "hw": [], "eng": []}
    Sched.sem_total = {}
    Sched.sem_stack = pstack
    cx.persist = []
    s = cx.s = Sched(nc)
    keep = s.stack
    s.stack = pstack
    emit_consts(cx)
    emit_psum(cx)
    cw_all = s.sbuf("cw_all", [128, NT * NE], F32)
    flag = s.sbuf("flag", [128, 1], I32)
    s.stack = keep
    regs = {}
    for en, eo in (("pe", nc.tensor), ("act", nc.scalar), ("dve", nc.vector), ("pool", nc.gpsimd), ("sp", nc.sync)):
        regs[en] = pstack.enter_context(eo.register("flagreg_" + en))
    cx.persist = [cx.ident, cx.identf, cx.epsc, cw_all, flag] + cx.ps
    cx.cw_buf = cw_all
    alloc_gla(cx, P)
    Sb_db = [s.dbuf("Sbdb%d" % t) for t in range(NT)]
    h1_db = [s.dbuf("h1db%d" % t) for t in range(NT)]
    hn_db = [s.dbuf("hndb%d" % t) for t in range(NT)]
    for t in reversed(range(NT)):
        emit_gla_tile(cx, t, False, x, Sb, Sb_db, h1, h1_db, hnT, hn_db)
    for t in range(NT):
        emit_gla_tile(cx, t, True, x, Sb, Sb_db, h1, h1_db, hnT, hn_db)
    s.emit()
    if _PH == 1:
        pstack.close()
        return nc
    s = new_phase(cx, nc)
    alloc_ffn_bufs(cx, TB)
    ws = [alloc_wset(cx, 0), alloc_wset(cx, 1)]
    db1 = [s.dbuf("p2a%d" % t) for t in range(NT)]
    db2 = [s.dbuf("p2b%d" % t) for t in range(NT)]
    for f in ffn_weight_loads(cx, ws[0], P["dWg"], P["dWu"], P["dWd"], 0):
        f()
    pre = ffn_weight_loads(cx, ws[1], P["dWg"], P["dWu"], P["dWd"], 1)
    ctr = emit_ffn_pass(cx, S, hnT, db1, ws[0], None, None, h1, db2, pre=pre)
    emit_ffn_pass(cx, S, hnT, db1, ws[1], None, None, h1, db2, ctr=ctr)
    s.emit()
    if _PH == 2:
        pstack.close()
        return nc
    s = new_phase(cx, nc)
    alloc_swa(cx, P, NT)
    d2 = [s.dbuf("p3a%d" % t) for t in range(NT)]
    d3 = [s.dbuf("p3b%d" % t) for t in range(NT)]
    d4 = [s.dbuf("p3c%d" % t) for t in range(NT)]
    swa_produce(cx, 0, h1, d2)
    if NT > 1:
        swa_produce(cx, 1, h1, d2)
    for t in range(NT):
        for part in range(NKV):
            swa_attend(cx, t, NT, h3, d3, hnT2, d4, cw_all, part=part)
            if t + 2 < NT and part < 3:
                swa_produce(cx, t + 2, h1, d2, part=part)
        swa_attend(cx, t, NT, h3, d3, hnT2, d4, cw_all, hn4tm_ap=(hn4tm if sparse else None), part=NKV)
    s.emit()
    if _PH == 3:
        pstack.close()
        return nc

    def moe_dense():
        s = cx.s
        db1 = [s.dbuf("p4a%d" % t) for t in range(NT)]
        db2 = [s.dbuf("p4b%d" % t) for t in range(NT)]
        for f in ffn_weight_loads(cx, ws[0], P["mWg"][0], P["mWu"][0], P["mWd"][0], 0):
            f()
        ctr = None
        for he in range(2 * NE):
            e_, half = he // 2, he % 2
            pre = ()
            if he + 1 < 2 * NE:
                e2, h2_ = (he + 1) // 2, (he + 1) % 2
                pre = ffn_weight_loads(cx, ws[(he + 1) % 2], P["mWg"][e2], P["mWu"][e2], P["mWd"][e2], h2_)
            ctr = emit_ffn_pass(cx, S, hnT2, db1, ws[he % 2], cw_all, (lambda t, e_=e_: t * NE + e_), h3, db2,
                                pre=pre, ctr=ctr)
        emit_final_norm(cx, S, h3, out, P["final_norm"], lambda t: [db2[t]])

    def moe_sparse():
        s = cx.s
        h3db = s.dbuf("h3db")
        for f in ffn_weight_loads(cx, ws[0], P["mWg"][0], P["mWu"][0], P["mWd"][0], 0):
            f()
        ctr = None
        for he in range(2 * NE):
            e_, half = he // 2, he % 2
            pre = ()
            if he + 1 < 2 * NE:
                e2, h2_ = (he + 1) // 2, (he + 1) % 2
                pre = ffn_weight_loads(cx, ws[(he + 1) % 2], P["mWg"][e2], P["mWu"][e2], P["mWd"][e2], h2_)
            ctr = emit_moe_sparse_pass(cx, S, e_, ws[he % 2], lst, hn4tm, h3, h3db, pre, ctr)
        emit_final_norm(cx, S, h3, out, P["final_norm"], lambda t: [h3db])

    if not sparse:
        s = new_phase(cx, nc)
        alloc_ffn_bufs(cx, TB)
        ws = [alloc_wset(cx, 0), alloc_wset(cx, 1)]
        cx.fst = [s.sbuf("fst%d" % i, [128, 2], F32) for i in range(3)]
        moe_dense()
        s.emit()
        pstack.close()
        return nc
    s = new_phase(cx, nc)
    emit_route(cx, S, cw_all, lst, flag)
    if _os.environ.get("DBG"):
        dbg = nc.dram_tensor("dbg", [128, NE + 2], F32, kind="ExternalOutput").ap()
        s.dma("sp", dbg[:, 0:NE], cx.r_base[:, NT * NE:NT * NE + NE], "r_dbg", reads=[cx.r_base])
        s.dma("sp", dbg[:, NE:NE + 2], cx.r_fl[:], "r_dbg2", reads=[cx.r_fl])
    zf = s.sbuf("r_zf", [128, D], F32)
    zb = s.sbuf("r_zb", [128, D], BF16)
    s.op("pool", lambda e: e.memset(zf[:], 0.0), writes=[zf])
    s.op("pool", lambda e: e.memset(zb[:], 0.0), writes=[zb])
    s.dma("sp", h3[S:S + 128, :], zf[:], "r_zf", reads=[zf])
    s.dma("sp", hn4tm[S:S + 128, :], zb[:], "r_zb", reads=[zb])
    s.emit()
    sa = new_phase(cx, nc)
    alloc_ffn_bufs(cx, TB)
    ws = [alloc_wset(cx, 0), alloc_wset(cx, 1)]
    g = cx.sp = Ctx()
    g.cnt = 0
    g.tile_idx = {}
    g.tile_gx = {}
    g.idx = [sa.sbuf("sp_idx%d" % i, [128, 2], I32) for i in range(8)]
    g.gx = [sa.sbuf("sp_gx%d" % i, [128, D], BF16) for i in range(6)]
    cx.fst = [sa.sbuf("fst%d" % i, [128, 2], F32) for i in range(3)]
    moe_sparse()
    for b in Buf.registry:
        b.lw = None
        b.rd = {}
        b.rd_dma = []
    sb_ = cx.s = Sched(nc)
    moe_dense()
    emit_either(nc, flag, regs, sa, sb_)
    pstack.close()
    return nc


def emit_final_norm(cx, S, h3, out, gain_ap, h3dep):
    s = cx.s
    NT = S // 128

    def fview(b):
        return b.t[:].rearrange("p a b -> p (a b)").bitcast(F32)[:, 0:D]
    fgB, fjB = cx.hnTb[1], cx.hnTb[0]
    s.dma("sp", fview(fgB), bcast_row(gain_ap), "fgain", writes=[fgB])
    for t in range(NT):
        i = t % 3
        xb, xd = cx.stg[i], [cx.stg[i], cx.stgA[i], cx.stgB[i]]
        ob = cx.hT[t % 2]
        st = cx.fst[i]
        s.dma("sp", xb[:], h3[t * 128:(t + 1) * 128, :], "fx%d" % i, reads=h3dep(t), writes=xd)
        s.op("act", lambda e, xb=xb, st=st: e.activation(out=fview(fjB), in_=xb[:], func=AF.Square, scale=1.0 / 32.0,
                                                         accum_out=st[:, 0:1]), reads=xd, writes=[fjB, st])
        s.op("act", lambda e, st=st: e.activation(out=st[:, 1:2], in_=st[:, 0:1], func=AF.Ln, bias=cx.epsc[:, 0:1]),
             reads=[st, cx.epsc], writes=[st])
        s.op("act", lambda e, st=st: e.activation(out=st[:, 1:2], in_=st[:, 1:2], func=AF.Exp, scale=-0.5),
             reads=[st], writes=[st])
        s.op("dve", lambda e, xb=xb, ob=ob, st=st: e.scalar_tensor_tensor(
            out=fview(ob), in0=xb[:], scalar=st[:, 1:2], in1=fview(fgB), op0=ALU.mult, op1=ALU.mult),
            reads=xd + [st, fgB], writes=[ob])
        s.dma("sp", out[t * 128:(t + 1) * 128, :], fview(ob), "fo%d" % (t % 2), reads=[ob])


PARAM_MAP = [
    ("mix_norm0", "mix_norm", 0), ("mix_norm1", "mix_norm", 1), ("ffn_norm0", "ffn_norm", 0), ("ffn_norm1", "ffn_norm", 1),
    ("gla_in", "gla_in_proj", 0), ("gw_f", "gla_gate_w_fwd", 0), ("gb_f", "gla_gate_b_fwd", 0),
    ("gw_b", "gla_gate_w_bwd", 0), ("gb_b", "gla_gate_b_bwd", 0), ("gla_hn", "gla_head_norm", 0),
    ("gla_out", "gla_out_proj", 0), ("swa_qkv", "swa_qkv_proj", 0), ("swa_qkv_b", "swa_qkv_bias", 0),
    ("sinks", "swa_sinks", 0), ("swa_out", "swa_out_proj", 0), ("swa_out_b", "swa_out_bias", 0),
    ("dWg", "dense_w_gate", 0), ("dWu", "dense_w_up", 0), ("dWd", "dense_w_down", 0), ("router", "moe_router", 0),
    ("mWg", "moe_w_gate", 0), ("mWu", "moe_w_up", 0), ("mWd", "moe_w_down", 0), ("final_norm", "final_norm", None),
]


def make_in_map(inputs, xs):
    m = {"x": np.ascontiguousarray(xs, dtype=np.float32)}
    for dst, src, idx in PARAM_MAP:
        v = np.asarray(inputs[src])
        if idx is not None:
            v = v[idx]
        m[dst] = np.ascontiguousarray(v, dtype=np.float32)
    return m


def kernel(**inputs):
    x = np.asarray(inputs["x"])
    B, S, _ = x.shape
    nc = build_program(S)
    base = make_in_map(inputs, x[0])
    in_maps = []
    for b in range(B):
        m = dict(base)
        m["x"] = np.ascontiguousarray(x[b], dtype=np.float32)
        in_maps.append(m)
    res = run_bass_kernel_spmd(nc, in_maps, core_ids=list(range(B)))
    return np.stack([np.asarray(r["out"]) for r in res.results], axis=0).astype(np.float32)


CAPT = 18
CAPR = CAPT * 128
BIGI = 1.0e6


def emit_route(cx, S, cw_all, list_ap, flag):
    s = cx.s
    NT = S // 128
    NC = NT * NE
    nb = (NC + 511) // 512
    m = s.sbuf("r_m", [128, NC], F32)
    mb = s.sbuf("r_mb", [128, NC], BF16)
    s.op("dve", lambda e: e.tensor_single_scalar(out=m[:], in_=cw_all[:], scalar=0.0, op=ALU.is_gt),
         reads=[cw_all], writes=[m])
    s.op("dve", lambda e: e.tensor_copy(out=mb[:], in_=m[:]), reads=[m], writes=[mb])
    slf = tri(cx, "r_slf", 1.0, 1, -1, ALU.is_gt)
    sl = s.sbuf("r_sl", [128, 128], BF16)
    s.op("dve", lambda e: e.tensor_copy(out=sl[:], in_=slf[:]), reads=[slf], writes=[sl])
    on = s.sbuf("r_on", [128, 128], BF16)
    s.op("pool", lambda e: e.memset(on[:], 1.0), writes=[on])
    within = s.sbuf("r_within", [128, NC], F32)
    tot = s.sbuf("r_tot", [128, NC], F32)
    for k in range(nb):
        cs = slice(k * 512, min(NC, (k + 1) * 512))
        n = cs.stop - cs.start
        s.op("pe", lambda e, cs=cs, n=n: e.matmul(cx.ps[0][:, 0:n], lhsT=sl[:], rhs=mb[:, cs], start=True, stop=True),
             reads=[sl, mb], writes=[cx.ps[0]])
        s.op("pe", lambda e, cs=cs, n=n: e.matmul(cx.ps[1][:, 0:n], lhsT=on[:], rhs=mb[:, cs], start=True, stop=True),
             reads=[on, mb], writes=[cx.ps[1]])
        s.op("act", lambda e, cs=cs, n=n: e.copy(out=within[:, cs], in_=cx.ps[0][:, 0:n]),
             reads=[cx.ps[0], within], writes=[within])
        s.op("dve", lambda e, cs=cs, n=n: e.tensor_copy(out=tot[:, cs], in_=cx.ps[1][:, 0:n]),
             reads=[cx.ps[1], tot], writes=[tot])
    base = s.sbuf("r_base", [128, NC + NE], F32)
    s.op("pool", lambda e: e.memset(base[:, 0:NE], 0.0), writes=[base])
    for t in range(NT):
        s.op("dve", lambda e, t=t: e.tensor_tensor(out=base[:, (t + 1) * NE:(t + 2) * NE], in0=base[:, t * NE:(t + 1) * NE],
                                                   in1=tot[:, t * NE:(t + 1) * NE], op=ALU.add),
             reads=[base, tot], writes=[base])
    fl = cx.r_fl = s.sbuf("r_fl", [128, 2], F32)
    cx.r_base = base
    s.op("dve", lambda e: e.reduce_max(out=fl[:, 0:1], in_=base[:, NC:NC + NE], axis=AX.X), reads=[base], writes=[fl])
    s.op("dve", lambda e: e.tensor_single_scalar(out=fl[:, 1:2], in_=fl[:, 0:1], scalar=(-1.0 if _os.environ.get('FORCEDENSE') else float(CAPR) + 0.5), op=ALU.is_lt),
         reads=[fl], writes=[fl])
    s.op("dve", lambda e: e.tensor_copy(out=flag[:], in_=fl[:, 1:2]), reads=[fl], writes=[flag])
    offi = s.sbuf("r_offi", [128, NC], I32)
    s.op("pool", lambda e: e.iota(offi[:].rearrange("p (t e) -> p t e", e=NE), pattern=[[0, NT], [CAPR, NE]], base=0,
                                  channel_multiplier=0), writes=[offi])
    dest = s.sbuf("r_dest", [128, NC], F32)
    s.op("dve", lambda e: e.tensor_copy(out=dest[:], in_=offi[:]), reads=[offi], writes=[dest])
    s.op("dve", lambda e: e.tensor_tensor(out=dest[:], in0=dest[:], in1=base[:, 0:NC], op=ALU.add),
         reads=[dest, base], writes=[dest])
    s.op("dve", lambda e: e.tensor_tensor(out=dest[:], in0=dest[:], in1=within[:], op=ALU.add),
         reads=[dest, within], writes=[dest])
    s.op("dve", lambda e: e.tensor_tensor(out=dest[:], in0=dest[:], in1=m[:], op=ALU.mult),
         reads=[dest, m], writes=[dest])
    nrow = NE * CAPR
    dumpi = s.sbuf("r_dumpi", [128, NC], I32)
    s.op("pool", lambda e: e.iota(dumpi[:], pattern=[[128, NC]], base=nrow, channel_multiplier=1), writes=[dumpi])
    tmp = s.sbuf("r_tmp", [128, NC], F32)
    s.op("dve", lambda e: e.tensor_copy(out=tmp[:], in_=dumpi[:]), reads=[dumpi], writes=[tmp])
    om = s.sbuf("r_om", [128, NC], F32)
    s.op("dve", lambda e: e.tensor_scalar(out=om[:], in0=m[:], scalar1=-1.0, scalar2=1.0, op0=ALU.mult, op1=ALU.add),
         reads=[m], writes=[om])
    s.op("dve", lambda e: e.tensor_tensor(out=tmp[:], in0=tmp[:], in1=om[:], op=ALU.mult),
         reads=[tmp, om], writes=[tmp])
    s.op("dve", lambda e: e.tensor_tensor(out=dest[:], in0=dest[:], in1=tmp[:], op=ALU.add),
         reads=[dest, tmp], writes=[dest])
    desti = s.sbuf("r_desti", [128, NC], I32)
    s.op("dve", lambda e: e.tensor_copy(out=desti[:], in_=dest[:]), reads=[dest], writes=[desti])
    src = s.sbuf("r_src", [128, NC, 2], I32)
    srcA = sub(src, "r_srcA")
    s.op("pool", lambda e: e.iota(src[:, :, 0].rearrange("p (t e) -> p t e", e=NE), pattern=[[128, NT], [0, NE]], base=0,
                                  channel_multiplier=1), writes=[src])
    s.op("dve", lambda e: e.tensor_copy(out=src[:, :, 1], in_=cw_all[:].bitcast(I32)), reads=[cw_all], writes=[srcA])
    K = nrow // 128
    ini = s.sbuf("r_ini", [128, K, 2], I32)
    iniA = sub(ini, "r_iniA")
    s.op("pool", lambda e: e.iota(ini[:, :, 0], pattern=[[1, K]], base=0, channel_multiplier=K), writes=[ini])
    s.op("dve", lambda e: e.tensor_single_scalar(out=ini[:, :, 0], in_=ini[:, :, 0], scalar=127, op=ALU.bitwise_and),
         reads=[ini], writes=[ini])
    s.op("dve", lambda e: e.tensor_single_scalar(out=ini[:, :, 0], in_=ini[:, :, 0], scalar=S, op=ALU.add),
         reads=[ini], writes=[ini])
    s.op("pool", lambda e: e.memset(ini[:, :, 1], 0), writes=[iniA])
    ldb = s.dbuf("r_listdb")
    s.dma("sp", list_ap[0:nrow, :].rearrange("(p k) c -> p k c", p=128), ini[:], "r_ini", reads=[ini, iniA], writes=[ldb])
    for col in range(NC):
        s.op("pool", lambda e, col=col: e.indirect_dma_start(
            out=list_ap[:, :], out_offset=bass.IndirectOffsetOnAxis(ap=desti[:, col:col + 1], axis=0),
            in_=src[:, col, :], in_offset=None),
            reads=[ldb, desti, src, srcA], writes=[], dma_key="r_scat")


def emit_moe_sparse_pass(cx, S, e_, w, list_ap, hn4tm_ap, h3_ap, h3db, pre, ctr):
    s = cx.s
    TB = cx.TB
    ntt = TB // 128
    g = cx.sp

    def prefetch(b):
        for i in range(ntt):
            j = b * ntt + i
            q = g.cnt
            g.cnt += 1
            idx = g.idx[q % 8]
            gx = g.gx[q % 6]
            g.tile_idx[j] = idx
            g.tile_gx[j] = (gx, q)
            r0 = e_ * CAPR + j * 128
            s.dma("sp", idx[:], list_ap[r0:r0 + 128, :], idx.name, writes=[idx])
            s.op("pool", lambda e, idx=idx, gx=gx: e.indirect_dma_start(
                out=gx[:, :], out_offset=None, in_=hn4tm_ap[:, :],
                in_offset=bass.IndirectOffsetOnAxis(ap=idx[:, 0:1], axis=0)),
                reads=[idx], writes=[gx], dma_key=gx.name)

    def loader(b, hb):
        for i in range(ntt):
            j = b * ntt + i
            gx, q = g.tile_gx[j]
            bank = cx.ps[4 + (q % 2) * 2]
            tv = Bview(bank)
            for c in range(DC):
                s.op("pe", lambda e, c=c, gx=gx, tv=tv: e.transpose(out=tv[:, c * 128:(c + 1) * 128],
                                                                    in_=gx[:, c * 128:(c + 1) * 128], identity=cx.ident[:]),
                     reads=[gx, cx.ident], writes=[bank])
            s.op("act", lambda e, i=i, tv=tv, hb=hb: e.copy(
                out=hb[:, :, i * 128:(i + 1) * 128], in_=tv[:].rearrange("p (c t) -> p c t", c=DC)),
                reads=[bank], writes=[hb])

    def sinker(tile, pd, stg, sa, sb_):
        idx = g.tile_idx[tile]
        cwa = idx[:, 1:2].bitcast(F32)
        s.op("act", lambda e, stg=stg, pd=pd, cwa=cwa: e.activation(
            out=stg[:, 0:512], in_=pd[0][:], func=AF.Copy, scale=cwa), reads=[pd[0], idx], writes=[sa])
        s.op("dve", lambda e, stg=stg, pd=pd, cwa=cwa: e.tensor_scalar(
            out=stg[:, 512:1024], in0=pd[1][:], scalar1=cwa, scalar2=None, op0=ALU.mult),
            reads=[pd[1], idx], writes=[sb_])
        s.op("pool", lambda e, stg=stg, idx=idx: e.indirect_dma_start(
            out=h3_ap[:, :], out_offset=bass.IndirectOffsetOnAxis(ap=idx[:, 0:1], axis=0), in_=stg[:, :],
            in_offset=None, compute_op=ALU.add),
            reads=[sa, sb_, idx, h3db], writes=[h3db], dma_key=stg.name)

    return emit_ffn_pass(cx, CAPR, None, None, w, None, None, None, None, pre=pre, ctr=ctr,
                         loader=loader, sinker=sinker, prefetch=prefetch)
```
